# Optimizing a Trainium2 kernel written in Bass

```python
import math
import jax, jax.numpy as jnp
from jax import lax
import numpy as np

D_MODEL = 2048
BATCH = 2
SEQ = 8192
DEPTH = 1

EPS = 1e-6
F_GROUPS = 4
F_GROUP_DIM = 256
F_WIDTH = F_GROUPS * F_GROUP_DIM
A_HEADS = 8
A_HEAD_DIM = D_MODEL // (2 * A_HEADS)
A_V_DIM = 2 * A_HEAD_DIM
A_QK_WIDTH = A_HEADS * 2 * A_HEAD_DIM
A_V_WIDTH = A_HEADS * A_V_DIM
Q_BLOCK = 128
IN_WIDTH = F_WIDTH + 2 * A_QK_WIDTH + A_V_WIDTH + 2 * D_MODEL
P_HEADS = 8
N_KEYS = 128
N_EXPERTS = N_KEYS * N_KEYS
P_KEY_DIM = 256
P_HALF = P_KEY_DIM // 2
P_TOPK = 16
P_TOKEN_BLOCK = 128

kernel_name = "hybrid_fnet_diffattn_peer_encoder"


def rmsnorm(x, g):
    xf = x.astype(jnp.float32)
    y = xf * lax.rsqrt(jnp.mean(xf * xf, axis=-1, keepdims=True) + EPS)
    return (y * g.astype(jnp.float32)).astype(x.dtype)


def alibi_slopes():
    return 2.0 ** (-8.0 * jnp.arange(1, A_HEADS + 1, dtype=jnp.float32) / A_HEADS)


def lambda_init_for(layer_idx):
    return 0.8 - 0.6 * math.exp(-0.3 * layer_idx)


def fourier_mix(z):
    B, S = z.shape[:2]
    zg = z.astype(jnp.float32).reshape(B, S, F_GROUPS, F_GROUP_DIM).transpose(0, 2, 1, 3)
    y = jnp.fft.fft2(zg, norm="ortho").real
    return y.transpose(0, 2, 1, 3).reshape(B, S, F_WIDTH).astype(z.dtype)


def diff_attention(q, k, v, lam, slopes):
    B, S = q.shape[:2]
    nb = S // Q_BLOCK
    qb = q.reshape(B, nb, Q_BLOCK, A_HEADS, 2, A_HEAD_DIM).swapaxes(0, 1)
    starts = jnp.arange(nb, dtype=jnp.int32) * Q_BLOCK
    kpos = jnp.arange(S, dtype=jnp.int32)
    scale = A_HEAD_DIM ** -0.5

    def block(args):
        qi, s0 = args
        qpos = s0 + jnp.arange(Q_BLOCK, dtype=jnp.int32)
        dist = jnp.abs(qpos[:, None] - kpos[None, :]).astype(jnp.float32)
        bias = -slopes[:, None, None] * dist
        s = jnp.einsum('bqhcd,bkhcd->bhcqk', qi, k,
                       preferred_element_type=jnp.float32) * scale + bias[None, :, None]
        p = jax.nn.softmax(s, axis=-1)
        a = p[:, :, 0] - lam * p[:, :, 1]
        return jnp.einsum('bhqk,bkhe->bqhe', a.astype(v.dtype), v)

    out = lax.map(block, (qb, starts))
    return out.swapaxes(0, 1).reshape(B, S, A_HEADS, A_V_DIM)


def peer(xn, w_query, sub_keys, expert_u, expert_v):
    B, S, D = xn.shape
    nb = (B * S) // P_TOKEN_BLOCK
    xb = xn.reshape(nb, P_TOKEN_BLOCK, D)
    t = P_TOKEN_BLOCK

    def block(xt):
        q = (xt @ w_query).reshape(t, P_HEADS, 2, P_HALF)
        s = jnp.einsum('thcd,hcnd->thcn', q, sub_keys,
                       preferred_element_type=jnp.float32)
        sv, si = lax.top_k(s, P_TOPK)
        cand = sv[:, :, 0, :, None] + sv[:, :, 1, None, :]
        cidx = si[:, :, 0, :, None] * N_KEYS + si[:, :, 1, None, :]
        top_s, top_j = lax.top_k(cand.reshape(t, P_HEADS, P_TOPK * P_TOPK), P_TOPK)
        idx = jnp.take_along_axis(cidx.reshape(t, P_HEADS, P_TOPK * P_TOPK), top_j, axis=-1)
        g = jax.nn.softmax(top_s, axis=-1)
        ue = jnp.take(expert_u, idx, axis=0)
        ve = jnp.take(expert_v, idx, axis=0)
        act = jax.nn.gelu(jnp.einsum('td,thkd->thk', xt, ue,
                                     preferred_element_type=jnp.float32), approximate=False) * g
        return jnp.einsum('thk,thkd->td', act.astype(ve.dtype), ve)

    return lax.map(block, xb).reshape(B, S, D)


def setup_inputs(seed: int = 0) -> dict:
    key = jax.random.key(seed)
    ks = jax.random.split(key, 20)
    f32 = jnp.float32

    def nrm(k, shape, scale):
        return jax.random.normal(k, shape, f32) * scale

    def gain(k, shape):
        return 1.0 + 0.02 * jax.random.normal(k, shape, f32)

    L = DEPTH
    return {
        "x": jax.random.normal(ks[0], (BATCH, SEQ, D_MODEL), f32),
        "norm1_g": gain(ks[1], (L, D_MODEL)),
        "w_in": nrm(ks[2], (L, D_MODEL, IN_WIDTH), D_MODEL ** -0.5),
        "w_fourier": nrm(ks[3], (L, F_WIDTH, D_MODEL), F_WIDTH ** -0.5),
        "w_attn": nrm(ks[4], (L, A_V_WIDTH, D_MODEL), A_V_WIDTH ** -0.5),
        "q_norm_g": gain(ks[5], (L, A_HEAD_DIM)),
        "k_norm_g": gain(ks[6], (L, A_HEAD_DIM)),
        "lambda_q1": nrm(ks[7], (L, A_HEAD_DIM), 0.1),
        "lambda_k1": nrm(ks[8], (L, A_HEAD_DIM), 0.1),
        "lambda_q2": nrm(ks[9], (L, A_HEAD_DIM), 0.1),
        "lambda_k2": nrm(ks[10], (L, A_HEAD_DIM), 0.1),
        "subln_g": gain(ks[11], (L, A_V_DIM)),
        "w_out": nrm(ks[12], (L, D_MODEL, D_MODEL), D_MODEL ** -0.5),
        "norm2_g": gain(ks[13], (L, D_MODEL)),
        "w_query": nrm(ks[14], (L, D_MODEL, P_HEADS * P_KEY_DIM), D_MODEL ** -0.5),
        "sub_keys": nrm(ks[15], (L, P_HEADS, 2, N_KEYS, P_HALF), P_HALF ** -0.5),
        "expert_u": nrm(ks[16], (L, N_EXPERTS, D_MODEL), D_MODEL ** -0.5),
        "expert_v": nrm(ks[17], (L, N_EXPERTS, D_MODEL), P_HEADS ** -0.5),
    }


def reference(x, norm1_g, w_in, w_fourier, w_attn, q_norm_g, k_norm_g, lambda_q1, lambda_k1,
              lambda_q2, lambda_k2, subln_g, w_out, norm2_g, w_query, sub_keys, expert_u, expert_v):
    B, S, _ = x.shape
    slopes = alibi_slopes()
    o1 = F_WIDTH
    o2 = o1 + A_QK_WIDTH
    o3 = o2 + A_QK_WIDTH
    o4 = o3 + A_V_WIDTH
    o5 = o4 + D_MODEL
    h = x
    for i in range(DEPTH):
        lam_init = lambda_init_for(i)
        xn = rmsnorm(h, norm1_g[i])
        proj = xn @ w_in[i]
        z_f, q, k, v, gate_f, gate_a = jnp.split(proj, [o1, o2, o3, o4, o5], axis=-1)

        y_f = fourier_mix(z_f) @ w_fourier[i]

        q = rmsnorm(q.reshape(B, S, A_HEADS, 2, A_HEAD_DIM), q_norm_g[i])
        k = rmsnorm(k.reshape(B, S, A_HEADS, 2, A_HEAD_DIM), k_norm_g[i])
        v = v.reshape(B, S, A_HEADS, A_V_DIM)
        lam = (jnp.exp(jnp.sum(lambda_q1[i].astype(jnp.float32) * lambda_k1[i].astype(jnp.float32)))
               - jnp.exp(jnp.sum(lambda_q2[i].astype(jnp.float32) * lambda_k2[i].astype(jnp.float32)))
               + lam_init)
        o = diff_attention(q, k, v, lam, slopes)
        o = rmsnorm(o, subln_g[i]) * (1.0 - lam_init)
        y_a = o.reshape(B, S, A_V_WIDTH) @ w_attn[i]

        mixed = jax.nn.sigmoid(gate_f) * y_f + jax.nn.sigmoid(gate_a) * y_a
        h = h + mixed @ w_out[i]

        hn = rmsnorm(h, norm2_g[i])
        h = h + peer(hn, w_query[i], sub_keys[i], expert_u[i], expert_v[i])
    return h
```

```python
import math
from contextlib import ExitStack
import numpy as np
import ml_dtypes
import concourse.bass as bass
import concourse.mybir as mybir
from concourse.bass_utils import run_bass_kernel_spmd

F32 = mybir.dt.float32
BF16 = mybir.dt.bfloat16
AF = mybir.ActivationFunctionType
ALU = mybir.AluOpType
AX = mybir.AxisListType

D = 2048
S_LEN = 8192
OWN = 2048
NH = 8
EPS = 1e-6
LAM_INIT = 0.8 - 0.6 * math.exp(-0.3 * 0)
SLOPES = [2.0 ** (-8.0 * (h + 1) / NH) for h in range(NH)]
NEXP = 16384


class Slot:
    __slots__ = ("w", "r")

    def __init__(self):
        self.w = None
        self.r = []


class Sched:
    def __init__(self, nc, es):
        self.nc = nc
        self.eng = {"pe": nc.tensor, "act": nc.scalar, "dve": nc.vector, "pool": nc.gpsimd, "sp": nc.sync}
        self.sem = {}
        self.cnt = {}
        for e in ("pe", "act", "dve", "pool"):
            self.sem[e] = es.enter_context(nc.semaphore("s_" + e))
            self.cnt[e] = 0
        self.seen = {e: {} for e in self.eng}
        self.es = es
        self.ndma = 0

    def dma_sem(self):
        self.ndma += 1
        s = self.es.enter_context(self.nc.semaphore("dq%d" % self.ndma))
        return [s, 0]

    def _wait(self, e, tok):
        if tok is None:
            return
        key, val = tok
        if key == "pe" and e == "pe":
            return
        if isinstance(key, str):
            sem, kid = self.sem[key], key
        else:
            sem, kid = key, id(key)
        if self.seen[e].get(kid, 0) >= val:
            return
        self.eng[e].wait_ge(sem, val)
        self.seen[e][kid] = val

    def _deps(self, e, reads, writes):
        for s in reads:
            self._wait(e, s.w)
        for s in writes:
            self._wait(e, s.w)
            for t in s.r:
                self._wait(e, t)

    def _commit(self, tok, reads, writes):
        for s in reads:
            s.r.append(tok)
            if len(s.r) > 40:
                best = {}
                for k, v in s.r:
                    kk = k if isinstance(k, str) else id(k)
                    if kk not in best or best[kk][1] < v:
                        best[kk] = (k, v)
                s.r = list(best.values())
        for s in writes:
            s.w = tok
            s.r = []

    def op(self, e, fn, reads=(), writes=(), inc=True):
        self._deps(e, reads, writes)
        ins = fn(self.eng[e])
        if inc:
            self.cnt[e] += 1
            ins.then_inc(self.sem[e], 1)
            tok = (e, self.cnt[e])
        else:
            tok = (e, self.cnt[e] + 1)
        self._commit(tok, reads, writes)
        return tok

    def dma(self, q, out, in_, dsem, reads=(), writes=()):
        self._deps(q, reads, writes)
        dsem[1] += 16
        self.eng[q].dma_start(out=out, in_=in_).then_inc(dsem[0], 16)
        tok = (dsem[0], dsem[1])
        self._commit(tok, reads, writes)
        return tok

    def wait_all(self, e, slots):
        for s in slots:
            self._wait(e, s.w)
            for t in s.r:
                self._wait(e, t)


class Ring:
    def __init__(self, items):
        self.items = items
        self.i = 0

    def next(self):
        it = self.items[self.i % len(self.items)]
        self.i += 1
        return it


def build(stage=99):
    nc = bass.Bass("TRN2", target_bir_lowering=False)
    dt_in = lambda n, sh, dt=F32: nc.dram_tensor(n, sh, dt, kind="ExternalInput").ap()
    xT = dt_in("xT", [D, S_LEN])
    import os as _os
    WEXP = bool(_os.environ.get("WBF16"))
    w_in = dt_in("w_in", [D, 11264], BF16 if WEXP else F32)
    vecs = dt_in("vecs", [128, 64])
    csc = dt_in("csc", [256, 512])
    if stage >= 2:
        tabC = dt_in("tabC", [S_LEN, OWN], BF16)
        tabS = dt_in("tabS", [S_LEN, OWN], BF16)
    if stage >= 3:
        mlin_d = dt_in("mlin", [128, 512])
        mdiag_d = dt_in("mdiag", [128, 896])
        atab_d = dt_in("atab", [128, 4096])
    if stage >= 4:
        w_f = dt_in("w_f", [1024, D])
        w_a = dt_in("w_a", [D, D])
        w_o = dt_in("w_o", [D, D])
    if stage >= 5:
        w_q = dt_in("w_q", [D, D])
        skT = dt_in("skT", [128, 16, 128])
        e_uT = dt_in("e_uT", [D, NEXP])
        e_v = dt_in("e_v", [NEXP, D])
    outT = nc.dram_tensor("outT", [D, OWN], F32, kind="ExternalOutput").ap()
    dbg = stage < 99
    dkind = "ExternalOutput" if dbg else "Internal"
    scr = lambda n, sh, dt=BF16: nc.dram_tensor(n, sh, dt, kind=dkind).ap()
    KT = scr("KT", [16, 128, S_LEN])
    QT = scr("QT", [16, 128, OWN])
    Vs = scr("Vs", [NH, S_LEN, 256])
    Zcs = scr("Zcs", [4, S_LEN, 512])
    G = scr("G", [2, D, OWN])
    oT = scr("oT", [D, OWN])
    YfT = scr("YfT", [1024, OWN])
    hTs = scr("hTs", [D, OWN], F32)

    es = ExitStack()
    with es:
        S = Sched(nc, es)
        sbt = lambda n, sh, dt: es.enter_context(nc.sbuf_tensor(n, sh, dt))
        PS = []
        for i in range(8):
            PS.append((es.enter_context(nc.psum_tensor("ps%d" % i, [128, 512], F32)), Slot()))
        pmain = Ring(PS[0:4])
        paux = Ring(PS[4:8])

        vec = sbt("vec", [128, 64], F32)
        s_vec = Slot()
        dq_c = S.dma_sem()
        S.dma("sp", vec[:], vecs[:, :], dq_c, writes=[s_vec])
        ones_b = sbt("ones_b", [128, 128], BF16)
        s_ones = Slot()
        S.op("dve", lambda v: v.memset(ones_b[:], 1.0), writes=[s_ones])
        g1 = vec[:, 0:16]
        gqk = sbt("gqk", [128, 2], F32)
        s_gqk = Slot()
        S.op("dve", lambda v: v.tensor_scalar(gqk[:, 0:1], vec[:, 32:33], 128.0 ** -0.5, None, ALU.mult), reads=[s_vec], writes=[s_gqk])
        S.op("dve", lambda v: v.tensor_copy(gqk[:, 1:2], vec[:, 33:34]), reads=[s_vec], writes=[s_gqk])

        cscb = sbt("cscb", [128, 2, 512], BF16)
        s_csc = Slot()
        dq_p = S.dma_sem()
        S.dma("pool", cscb[:], csc.rearrange("(c p) n -> p c n", p=128), dq_p, writes=[s_csc])

        def phaseA():
            esA = ExitStack()
            with esA:
                sa = lambda n, sh, dt: esA.enter_context(nc.sbuf_tensor(n, sh, dt))
                NT = 256
                xin = Ring([(sa("xin%d" % i, [128, 16, NT], F32), Slot()) for i in range(2)])
                sqb = Ring([(sa("sqb%d" % i, [128, 16, NT], BF16), Slot()) for i in range(1)])
                rst = Ring([(sa("rst%d" % i, [128, NT], F32), Slot()) for i in range(2)])
                xn = sa("xn", [128, 16, 2048], BF16)
                s_xn = [Slot() for _ in range(8)]
                wb = Ring([(sa("wb%d" % i, [128, 16, 512], BF16), Slot()) for i in range(3)])
                wdq = [S.dma_sem() for _ in range(3)]
                xdq = [S.dma_sem() for _ in range(2)]
                zT = Ring([(sa("zT%d" % i, [128, 2, 512], BF16), Slot()) for i in range(2)])
                zst = Ring([(sa("zst%d" % i, [128, 4, 512], BF16), Slot()) for i in range(2)])
                zdq = [S.dma_sem() for _ in range(2)]
                raw = Ring([(sa("raw%d" % i, [128, 512], F32), Slot()) for i in range(2)])
                sq2 = Ring([(sa("sq2%d" % i, [128, 512], BF16), Slot()) for i in range(2)])
                sd2 = Ring([(sa("sd2%d" % i, [128, 512], F32), Slot()) for i in range(2)])
                st4 = Ring([(sa("st4%d" % i, [128, 4, 512], BF16), Slot()) for i in range(3)])
                st4dq = [S.dma_sem() for _ in range(3)]
                vst = Ring([(sa("vst%d" % i, [128, 512], BF16), Slot()) for i in range(3)])
                vdq = [S.dma_sem() for _ in range(3)]
                w_in_v = w_in.rearrange("(c p) n -> p c n", p=128)
                xT_v = xT.rearrange("(c p) t -> p c t", p=128)
                scr_slots = []

                def xn_slots(t0, n):
                    return s_xn[t0 // NT:(t0 + n + NT - 1) // NT]

                import os
                for st in range(int(os.environ.get('PH_A_ST0', '0')), int(os.environ.get('PH_A_ST', '4'))):
                    own = None
                    is_own = (st == 0)
                    tok0 = st * 2048
                    for nt in range(8):
                        xi, s_xi = xin.next()
                        xq = xdq[(xin.i - 1) % 2]
                        S.dma("sp", xi[:], xT_v[:, :, tok0 + nt * NT: tok0 + (nt + 1) * NT], xq, writes=[s_xi])
                        sq, s_sq = sqb.next()
                        S.op("act", lambda a: a.activation(out=sq[:], in_=xi[:], func=AF.Square), reads=[s_xi], writes=[s_sq])
                        pa, s_pa = paux.next()
                        for c in range(16):
                            S.op("pe", lambda t, c=c: t.matmul(pa[:, 0:NT], ones_b[:], sq[:, c, :], start=(c == 0), stop=(c == 15)),
                                 reads=[s_ones, s_sq], writes=[s_pa], inc=(c == 15))
                        rs, s_rs = rst.next()
                        S.op("act", lambda a: a.activation(out=rs[:], in_=pa[:, 0:NT], func=AF.Sqrt, bias=EPS, scale=1.0 / D), reads=[s_pa], writes=[s_rs])
                        S.op("dve", lambda v: v.reciprocal(rs[:], rs[:]), reads=[s_rs], writes=[s_rs])
                        for c in range(16):
                            S.op("dve", lambda v, c=c: v.scalar_tensor_tensor(xn[:, c, nt * NT:(nt + 1) * NT], xi[:, c, :], g1[:, c:c + 1], rs[:], ALU.mult, ALU.mult),
                                 reads=[s_xi, s_rs, s_vec], writes=[s_xn[nt]])
                    blocks = [("z", 0), ("z", 1)]
                    if is_own:
                        blocks += [("q", i) for i in range(4)]
                    blocks += [("k", i) for i in range(4)] + [("v", i) for i in range(4)]
                    if is_own:
                        blocks += [("gf", i) for i in range(4)] + [("ga", i) for i in range(4)]
                    col_base = {"z": 0, "q": 1024, "k": 3072, "v": 5120, "gf": 7168, "ga": 9216}
                    kk = os.environ.get('PH_A_KINDS')
                    if kk is not None:
                        blocks = [b_ for b_ in blocks if b_[0] in kk.split(',')]
                    for kind, bi in blocks:
                        col0 = col_base[kind] + bi * 512
                        w, s_w = wb.next()
                        wq = wdq[(wb.i - 1) % 3]
                        S.dma("sp" if WEXP else "pool", w[:], w_in_v[:, :, col0:col0 + 512], wq, writes=[s_w])
                        if kind == "v":
                            for sub in range(16):
                                pm, s_pm = pmain.next()
                                for kc in range(16):
                                    S.op("pe", lambda t, kc=kc: t.matmul(pm[:], xn[:, kc, sub * 128:(sub + 1) * 128], w[:, kc, :], start=(kc == 0), stop=(kc == 15)),
                                         reads=[s_w] + xn_slots(sub * 128, 128), writes=[s_pm], inc=(kc == 15))
                                vs_, s_vs = vst.next()
                                vq = vdq[(vst.i - 1) % 3]
                                S.op("dve", lambda v: v.tensor_copy(vs_[:], pm[:]), reads=[s_pm], writes=[s_vs])
                                dst = Vs[2 * bi:2 * bi + 2, tok0 + sub * 128: tok0 + (sub + 1) * 128, :].rearrange("h t d -> t h d")
                                sl = Slot()
                                scr_slots.append(sl)
                                S.dma("sp", dst, vs_[:].rearrange("p (h d) -> p h d", h=2), vq, reads=[s_vs], writes=[sl])
                            continue
                        if kind == "z":
                            for gi in range(2):
                                g = 2 * bi + gi
                                for t in range(4):
                                    z, s_z = zT.next()
                                    for c2 in range(2):
                                        cc = gi * 2 + c2
                                        pm, s_pm = pmain.next()
                                        for kc in range(16):
                                            S.op("pe", lambda t_, kc=kc: t_.matmul(pm[:], w[:, kc, cc * 128:(cc + 1) * 128], xn[:, kc, t * 512:(t + 1) * 512], start=(kc == 0), stop=(kc == 15)),
                                                 reads=[s_w] + xn_slots(t * 512, 512), writes=[s_pm], inc=(kc == 15))
                                        S.op("dve", lambda v: v.tensor_copy(z[:, c2, :], pm[:]), reads=[s_pm], writes=[s_z])
                                    zs, s_zs = zst.next()
                                    zq = zdq[(zst.i - 1) % 2]
                                    for sub in range(4):
                                        pa, s_pa = paux.next()
                                        for c2 in range(2):
                                            S.op("pe", lambda t_, c2=c2: t_.matmul(pa[:], z[:, c2, sub * 128:(sub + 1) * 128], cscb[:, c2, :], start=(c2 == 0), stop=(c2 == 1)),
                                                 reads=[s_z, s_csc], writes=[s_pa], inc=(c2 == 1))
                                        S.op("dve", lambda v: v.tensor_copy(zs[:, sub, :], pa[:]), reads=[s_pa], writes=[s_zs])
                                    dst = Zcs[g, tok0 + t * 512: tok0 + (t + 1) * 512, :].rearrange("(s p) n -> p s n", p=128)
                                    sl = Slot()
                                    scr_slots.append(sl)
                                    S.dma("sp", dst, zs[:], zq, reads=[s_zs], writes=[sl])
                            continue
                        for cc in range(4):
                            stg, s_stg = st4.next()
                            sq_ = st4dq[(st4.i - 1) % 3]
                            for t in range(4):
                                pm, s_pm = pmain.next()
                                for kc in range(16):
                                    S.op("pe", lambda t_, kc=kc: t_.matmul(pm[:], w[:, kc, cc * 128:(cc + 1) * 128], xn[:, kc, t * 512:(t + 1) * 512], start=(kc == 0), stop=(kc == 15)),
                                         reads=[s_w] + xn_slots(t * 512, 512), writes=[s_pm], inc=(kc == 15))
                                if kind in ("gf", "ga"):
                                    S.op("act", lambda a: a.activation(out=stg[:, t, :], in_=pm[:], func=AF.Sigmoid), reads=[s_pm], writes=[s_stg])
                                else:
                                    r, s_r = raw.next()
                                    q2, s_q2 = sq2.next()
                                    d2, s_d2 = sd2.next()
                                    S.op("dve", lambda v: v.tensor_copy(r[:], pm[:]), reads=[s_pm], writes=[s_r])
                                    S.op("act", lambda a: a.activation(out=q2[:], in_=r[:], func=AF.Square), reads=[s_r], writes=[s_q2])
                                    pa, s_pa = (pmain if _os.environ.get("QKMAIN") else paux).next()
                                    S.op("pe", lambda t_: t_.matmul(pa[:], ones_b[:], q2[:], start=True, stop=True), reads=[s_ones, s_q2], writes=[s_pa])
                                    S.op("act", lambda a: a.activation(out=d2[:], in_=pa[:], func=AF.Sqrt, bias=EPS, scale=1.0 / 128), reads=[s_pa], writes=[s_d2])
                                    S.op("dve", lambda v: v.reciprocal(d2[:], d2[:]), reads=[s_d2], writes=[s_d2])
                                    gcol = gqk[:, 0:1] if kind == "q" else gqk[:, 1:2]
                                    S.op("dve", lambda v: v.scalar_tensor_tensor(stg[:, t, :], r[:], gcol, d2[:], ALU.mult, ALU.mult),
                                         reads=[s_r, s_d2, s_gqk], writes=[s_stg])
                            ch = bi * 4 + cc
                            if kind == "q":
                                dst = QT[ch, :, :].rearrange("p (t n) -> p t n", t=4)
                            elif kind == "k":
                                dst = KT[ch, :, tok0:tok0 + 2048].rearrange("p (t n) -> p t n", t=4)
                            else:
                                dst = G[0 if kind == "gf" else 1, ch * 128:(ch + 1) * 128, :].rearrange("p (t n) -> p t n", t=4)
                            sl = Slot()
                            scr_slots.append(sl)
                            S.dma("sp", dst, stg[:], sq_, reads=[s_stg], writes=[sl])
                for e in ("sp", "pool"):
                    S.wait_all(e, scr_slots)
                barrier()

        def barrier():
            for e in ("pe", "act", "dve", "pool", "sp"):
                for o in ("pe", "act", "dve", "pool"):
                    if S.cnt[o] > 0:
                        S._wait(e, (o, S.cnt[o]))

        def phaseC():
            esC = ExitStack()
            with esC:
                sa = lambda n, sh, dt: esC.enter_context(nc.sbuf_tensor(n, sh, dt))
                TC = sa("TC", [128, 64, 512], BF16)
                TS = sa("TS", [128, 64, 512], BF16)
                s_TC, s_TS = Slot(), Slot()
                tq = [S.dma_sem(), S.dma_sem()]
                zp = Ring([(sa("zp%d" % i, [128, 8, 512], BF16), Slot()) for i in range(3)])
                zq = [S.dma_sem() for _ in range(3)]
                yst = Ring([(sa("yst%d" % i, [128, 512], BF16), Slot()) for i in range(2)])
                yq = [S.dma_sem() for _ in range(2)]
                tabC_v = tabC.rearrange("(c p) k -> p c k", p=128)
                tabS_v = tabS.rearrange("(c p) k -> p c k", p=128)
                scr_slots = []
                for kb in range(4):
                    S.dma("sp", TC[:], tabC_v[:, :, kb * 512:(kb + 1) * 512], tq[0], writes=[s_TC])
                    S.dma("sp", TS[:], tabS_v[:, :, kb * 512:(kb + 1) * 512], tq[1], writes=[s_TS])
                    for g in range(4):
                        acc = [pmain.next(), pmain.next()]
                        for pi in range(8):
                            z, s_z = zp.next()
                            q_ = zq[(zp.i - 1) % 3]
                            S.dma("sp", z[:], Zcs[g, pi * 1024:(pi + 1) * 1024, :].rearrange("(c p) n -> p c n", p=128), q_, writes=[s_z])
                            for c in range(8):
                                sc = pi * 8 + c
                                for half in range(2):
                                    pa_, s_pa_ = acc[half]
                                    S.op("pe", lambda t_: t_.matmul(pa_[:], z[:, c, half * 128:(half + 1) * 128], TC[:, sc, :], start=(sc == 0), stop=False),
                                         reads=[s_z, s_TC], writes=[s_pa_], inc=False)
                                    S.op("pe", lambda t_: t_.matmul(pa_[:], z[:, c, 256 + half * 128:256 + (half + 1) * 128], TS[:, sc, :], start=False, stop=(sc == 63)),
                                         reads=[s_z, s_TS], writes=[s_pa_], inc=(c == 7))
                        for half in range(2):
                            pa_, s_pa_ = acc[half]
                            ys, s_ys = yst.next()
                            q_ = yq[(yst.i - 1) % 2]
                            S.op("dve", lambda v: v.tensor_copy(ys[:], pa_[:]), reads=[s_pa_], writes=[s_ys])
                            sl = Slot()
                            scr_slots.append(sl)
                            ch = g * 2 + half
                            S.dma("sp", YfT[ch * 128:(ch + 1) * 128, kb * 512:(kb + 1) * 512], ys[:], q_, reads=[s_ys], writes=[sl])
                for e in ("sp", "pool"):
                    S.wait_all(e, scr_slots)
                barrier()

        def phaseB():
            esB = ExitStack()
            with esB:
                sa = lambda n, sh, dt: esB.enter_context(nc.sbuf_tensor(n, sh, dt))
                mlin = sa("mlin_sb", [128, 512], F32)
                mdiag = sa("mdiag_sb", [128, 896], F32)
                at = sa("atab_sb", [128, 4096], F32)
                s_cst = Slot()
                cq = S.dma_sem()
                S.dma("sp", mlin[:], mlin_d[:, :], cq, writes=[s_cst])
                S.dma("sp", mdiag[:], mdiag_d[:, :], cq, writes=[s_cst])
                S.dma("sp", at[:], atab_d[:, :], cq, writes=[s_cst])
                ones_f = sa("ones_f", [128, 128], F32)
                s_of = Slot()
                S.op("dve", lambda v: v.memset(ones_f[:], 1.0), writes=[s_of])
                lam = sa("lam", [128, 8], F32)
                s_lam = Slot()
                S.op("dve", lambda v: v.tensor_tensor(lam[:, 0:1], vec[:, 36:37], vec[:, 37:38], ALU.mult), reads=[s_vec], writes=[s_lam])
                S.op("dve", lambda v: v.tensor_tensor(lam[:, 1:2], vec[:, 38:39], vec[:, 39:40], ALU.mult), reads=[s_vec], writes=[s_lam])
                pmx, s_pmx = PS[4]
                S.op("pe", lambda t_: t_.matmul(pmx[:, 0:2], ones_f[:], lam[:, 0:2], start=True, stop=True), reads=[s_of, s_lam], writes=[s_pmx])
                S.op("act", lambda a: a.activation(out=lam[:, 2:4], in_=pmx[:, 0:2], func=AF.Exp), reads=[s_pmx], writes=[s_lam])
                S.op("dve", lambda v: v.tensor_tensor(lam[:, 4:5], lam[:, 2:3], lam[:, 3:4], ALU.subtract), reads=[s_lam], writes=[s_lam])
                S.op("dve", lambda v: v.tensor_scalar(lam[:, 5:6], lam[:, 4:5], LAM_INIT, -1.0, ALU.add, ALU.mult), reads=[s_lam], writes=[s_lam])
                nlam = lam[:, 5:6]
                gsub = sa("gsub", [128, 2], F32)
                s_gsub = Slot()
                S.op("dve", lambda v: v.tensor_scalar(gsub[:], vec[:, 34:36], 1.0 - LAM_INIT, None, ALU.mult), reads=[s_vec], writes=[s_gsub])

                kt = Ring([(sa("kt%d" % i, [128, 2, S_LEN], BF16), Slot()) for i in range(2)])
                vt = Ring([(sa("vt%d" % i, [128, 64, 256], BF16), Slot()) for i in range(2)])
                qt = Ring([(sa("qt%d" % i, [128, 2, OWN], BF16), Slot()) for i in range(2)])
                hq = [S.dma_sem() for _ in range(2)]
                baseL = sa("baseL", [128, 512], F32)
                baseD = sa("baseD", [128, 896], F32)
                s_base = Slot()
                tt = Ring([(sa("tt%d" % i, [128, 512], F32), Slot()) for i in range(3)])
                pp = Ring([(sa("pp%d" % i, [128, 512], BF16), Slot()) for i in range(3)])
                ev = [sa("ev%d" % i, [128, 512], F32) for i in range(3)]
                s_ev = Slot()
                acc = sa("acc", [128, 2, 512], F32)
                s_acc = Slot()
                sqo = sa("sqo", [128, 2, 512], BF16)
                s_sqo = Slot()
                sdo = sa("sdo", [128, 512], F32)
                s_sdo = Slot()
                ost = Ring([(sa("ost%d" % i, [128, 2, 512], BF16), Slot()) for i in range(2)])
                oq = [S.dma_sem() for _ in range(2)]
                scr_slots = []
                ps_s = Ring(PS[0:4])
                (pO0, s_pO0), (pO1, s_pO1), (pZ, s_pZ) = PS[5], PS[6], PS[7]
                LAG = 2

                def needed_chunks(h, qb):
                    dmin = (2 * 16.0 + 22.0) / SLOPES[h]
                    out = []
                    for kc in range(64):
                        best = 1 << 30
                        for j in range(4):
                            k0 = (kc * 128 + OWN * j) % S_LEN
                            q0 = OWN * j + qb * 512
                            if k0 >= q0 + 512:
                                d = k0 - (q0 + 511)
                            elif k0 + 128 <= q0:
                                d = q0 - (k0 + 127)
                            else:
                                d = 0
                            best = min(best, d)
                        if best < dmin:
                            out.append(kc)
                    return out

                for h in range(NH):
                    k_, s_k = kt.next()
                    v_, s_v = vt.next()
                    q_, s_q = qt.next()
                    dq = hq[h % 2]
                    S.dma("sp", k_[:], KT[2 * h:2 * h + 2, :, :].rearrange("m p t -> p m t"), dq, writes=[s_k])
                    S.dma("sp", v_[:], Vs[h, :, :].rearrange("(c p) d -> p c d", p=128), dq, writes=[s_v])
                    S.dma("sp", q_[:], QT[2 * h:2 * h + 2, :, :].rearrange("m p t -> p m t"), dq, writes=[s_q])
                    S.op("pool", lambda g_: g_.tensor_scalar(baseL[:], mlin[:], -SLOPES[h], None, ALU.mult), reads=[s_cst], writes=[s_base])
                    S.op("pool", lambda g_: g_.tensor_scalar(baseD[:], mdiag[:], -SLOPES[h], None, ALU.mult), reads=[s_cst], writes=[s_base])
                    for qb in range(4):
                        for m in range(2):
                            tiles = {}

                            def issue_s(kc):
                                ps_, s_ps = ps_s.next()
                                S.op("pe", lambda t_: t_.matmul(ps_[:], k_[:, m, kc * 128:(kc + 1) * 128], q_[:, m, qb * 512:(qb + 1) * 512], start=True, stop=True),
                                     reads=[s_k, s_q], writes=[s_ps])
                                t, s_t = tt.next()
                                col = ((h * 4 + qb) * 64 + kc) * 2
                                diag = (qb * 4 <= kc < qb * 4 + 4)
                                if diag:
                                    delta = (kc - qb * 4) * 128
                                    bs = baseD[:, 384 - delta:384 - delta + 512]
                                    S.op("dve", lambda v: v.tensor_tensor(t[:], ps_[:], bs, ALU.add), reads=[s_ps, s_base], writes=[s_t])
                                else:
                                    S.op("dve", lambda v: v.scalar_tensor_tensor(t[:], baseL[:], at[:, col:col + 1], ps_[:], ALU.mult, ALU.add),
                                         reads=[s_ps, s_base, s_cst], writes=[s_t])
                                p, s_p = pp.next()
                                if diag:
                                    S.op("act", lambda a: a.activation(out=p[:], in_=t[:], func=AF.Exp), reads=[s_t], writes=[s_p])
                                else:
                                    S.op("act", lambda a: a.activation(out=p[:], in_=t[:], func=AF.Exp, bias=at[:, col + 1:col + 2]), reads=[s_t, s_cst], writes=[s_p])
                                tiles[kc] = (p, s_p)

                            def issue_pv(kc, st_, sp_):
                                p, s_p = tiles.pop(kc)
                                S.op("pe", lambda t_: t_.matmul(pO0[:], v_[:, kc, 0:128], p[:], start=st_, stop=sp_), reads=[s_v, s_p], writes=[s_pO0], inc=False)
                                S.op("pe", lambda t_: t_.matmul(pO1[:], v_[:, kc, 128:256], p[:], start=st_, stop=sp_), reads=[s_v, s_p], writes=[s_pO1], inc=False)
                                S.op("pe", lambda t_: t_.matmul(pZ[:], ones_b[:], p[:], start=st_, stop=sp_), reads=[s_ones, s_p], writes=[s_pZ], inc=True)

                            chunks = needed_chunks(h, qb)
                            nch = len(chunks)
                            for ix in range(nch + LAG):
                                if ix < nch:
                                    issue_s(chunks[ix])
                                if ix >= LAG:
                                    issue_pv(chunks[ix - LAG], ix - LAG == 0, ix - LAG == nch - 1)
                            S.op("dve", lambda v: v.reciprocal(ev[2][:], pZ[:]), reads=[s_pZ], writes=[s_ev])
                            if m == 0:
                                S.op("dve", lambda v: v.tensor_tensor(acc[:, 0, :], pO0[:], ev[2][:], ALU.mult), reads=[s_pO0, s_ev], writes=[s_acc])
                                S.op("dve", lambda v: v.tensor_tensor(acc[:, 1, :], pO1[:], ev[2][:], ALU.mult), reads=[s_pO1, s_ev], writes=[s_acc])
                            else:
                                S.op("dve", lambda v: v.tensor_tensor(ev[0][:], pO0[:], ev[2][:], ALU.mult), reads=[s_pO0, s_ev], writes=[s_ev])
                                S.op("dve", lambda v: v.tensor_tensor(ev[1][:], pO1[:], ev[2][:], ALU.mult), reads=[s_pO1, s_ev], writes=[s_ev])
                                for dv in range(2):
                                    S.op("dve", lambda v, dv=dv: v.scalar_tensor_tensor(acc[:, dv, :], ev[dv][:], nlam, acc[:, dv, :], ALU.mult, ALU.add),
                                         reads=[s_ev, s_lam, s_acc], writes=[s_acc])
                        S.op("act", lambda a: a.activation(out=sqo[:], in_=acc[:], func=AF.Square), reads=[s_acc], writes=[s_sqo])
                        for dv in range(2):
                            S.op("pe", lambda t_, dv=dv: t_.matmul(pmx[:], ones_b[:], sqo[:, dv, :], start=(dv == 0), stop=(dv == 1)), reads=[s_ones, s_sqo], writes=[s_pmx], inc=(dv == 1))
                        S.op("act", lambda a: a.activation(out=sdo[:], in_=pmx[:], func=AF.Sqrt, bias=EPS, scale=1.0 / 256), reads=[s_pmx], writes=[s_sdo])
                        S.op("dve", lambda v: v.reciprocal(sdo[:], sdo[:]), reads=[s_sdo], writes=[s_sdo])
                        os_, s_os = ost.next()
                        oq_ = oq[(ost.i - 1) % 2]
                        for dv in range(2):
                            S.op("dve", lambda v, dv=dv: v.scalar_tensor_tensor(os_[:, dv, :], acc[:, dv, :], gsub[:, dv:dv + 1], sdo[:], ALU.mult, ALU.mult),
                                 reads=[s_acc, s_gsub, s_sdo], writes=[s_os])
                        sl = Slot()
                        scr_slots.append(sl)
                        S.dma("sp", oT[h * 256:(h + 1) * 256, qb * 512:(qb + 1) * 512].rearrange("(c p) t -> p c t", p=128), os_[:], oq_, reads=[s_os], writes=[sl])
                for e in ("sp", "pool"):
                    S.wait_all(e, scr_slots)
                barrier()

        def phaseD():
            esD = ExitStack()
            with esD:
                sa = lambda n, sh, dt: esD.enter_context(nc.sbuf_tensor(n, sh, dt))
                yf = sa("yf", [128, 8, 512], BF16)
                ot = sa("ot", [128, 16, 512], BF16)
                gf = sa("gf", [128, 16, 512], BF16)
                ga = sa("ga", [128, 16, 512], BF16)
                xt = sa("xt", [128, 16, 512], F32)
                mixed = sa("mixed", [128, 16, 512], BF16)
                s_in, s_xt, s_mixed = Slot(), Slot(), Slot()
                inq = S.dma_sem()
                xq = S.dma_sem()
                hq_ = S.dma_sem()
                wf = Ring([(sa("wf%d" % i, [128, 8, 512], BF16), Slot()) for i in range(2)])
                wa = Ring([(sa("wa%d" % i, [128, 16, 512], BF16), Slot()) for i in range(2)])
                wo = Ring([(sa("wo%d" % i, [128, 16, 512], BF16), Slot()) for i in range(2)])
                wfq = [S.dma_sem() for _ in range(2)]
                waq = [S.dma_sem() for _ in range(2)]
                woq = [S.dma_sem() for _ in range(2)]
                t1 = Ring([(sa("t1_%d" % i, [128, 512], F32), Slot()) for i in range(2)])
                t2 = Ring([(sa("t2_%d" % i, [128, 512], F32), Slot()) for i in range(2)])
                YfT_v = YfT.rearrange("(c p) k -> p c k", p=128)
                oT_v = oT.rearrange("(c p) k -> p c k", p=128)
                Gf_v = G[0].rearrange("(c p) k -> p c k", p=128)
                Ga_v = G[1].rearrange("(c p) k -> p c k", p=128)
                xT_v = xT.rearrange("(c p) t -> p c t", p=128)
                hT_v = hTs.rearrange("(c p) t -> p c t", p=128)
                w_f_v = w_f.rearrange("(c p) n -> p c n", p=128)
                w_a_v = w_a.rearrange("(c p) n -> p c n", p=128)
                w_o_v = w_o.rearrange("(c p) n -> p c n", p=128)
                scr_slots = []
                for t in range(4):
                    ts_ = slice(t * 512, (t + 1) * 512)
                    S.dma("sp", yf[:], YfT_v[:, :, ts_], inq, writes=[s_in])
                    S.dma("sp", ot[:], oT_v[:, :, ts_], inq, writes=[s_in])
                    S.dma("sp", gf[:], Gf_v[:, :, ts_], inq, writes=[s_in])
                    S.dma("sp", ga[:], Ga_v[:, :, ts_], inq, writes=[s_in])
                    S.dma("sp", xt[:], xT_v[:, :, ts_], xq, writes=[s_xt])
                    for ob in range(4):
                        wf_, s_wf = wf.next()
                        wa_, s_wa = wa.next()
                        S.dma("pool", wf_[:], w_f_v[:, :, ob * 512:(ob + 1) * 512], wfq[(wf.i - 1) % 2], writes=[s_wf])
                        S.dma("pool", wa_[:], w_a_v[:, :, ob * 512:(ob + 1) * 512], waq[(wa.i - 1) % 2], writes=[s_wa])
                        for cc in range(4):
                            oc = ob * 4 + cc
                            pf, s_pf = pmain.next()
                            for kc in range(8):
                                S.op("pe", lambda t_, kc=kc: t_.matmul(pf[:], wf_[:, kc, cc * 128:(cc + 1) * 128], yf[:, kc, :], start=(kc == 0), stop=(kc == 7)),
                                     reads=[s_wf, s_in], writes=[s_pf], inc=(kc == 7))
                            pa_, s_pa_ = pmain.next()
                            for kc in range(16):
                                S.op("pe", lambda t_, kc=kc: t_.matmul(pa_[:], wa_[:, kc, cc * 128:(cc + 1) * 128], ot[:, kc, :], start=(kc == 0), stop=(kc == 15)),
                                     reads=[s_wa, s_in], writes=[s_pa_], inc=(kc == 15))
                            a1, s_a1 = t1.next()
                            a2, s_a2 = t2.next()
                            S.op("dve", lambda v: v.tensor_tensor(a1[:], pf[:], gf[:, oc, :], ALU.mult), reads=[s_pf, s_in], writes=[s_a1])
                            S.op("dve", lambda v: v.tensor_tensor(a2[:], pa_[:], ga[:, oc, :], ALU.mult), reads=[s_pa_, s_in], writes=[s_a2])
                            S.op("pool", lambda g_: g_.tensor_tensor(mixed[:, oc, :], a1[:], a2[:], ALU.add), reads=[s_a1, s_a2], writes=[s_mixed])
                    for ob in range(4):
                        wo_, s_wo = wo.next()
                        S.dma("pool", wo_[:], w_o_v[:, :, ob * 512:(ob + 1) * 512], woq[(wo.i - 1) % 2], writes=[s_wo])
                        for cc in range(4):
                            oc = ob * 4 + cc
                            ph, s_ph = pmain.next()
                            for kc in range(16):
                                S.op("pe", lambda t_, kc=kc: t_.matmul(ph[:], wo_[:, kc, cc * 128:(cc + 1) * 128], mixed[:, kc, :], start=(kc == 0), stop=(kc == 15)),
                                     reads=[s_wo, s_mixed], writes=[s_ph], inc=(kc == 15))
                            S.op("dve", lambda v: v.tensor_tensor(xt[:, oc, :], ph[:], xt[:, oc, :], ALU.add), reads=[s_ph, s_xt], writes=[s_xt])
                    sl = Slot()
                    scr_slots.append(sl)
                    S.dma("sp", hT_v[:, :, ts_], xt[:], hq_, reads=[s_xt], writes=[sl])
                for e in ("sp", "pool"):
                    S.wait_all(e, scr_slots)
                barrier()

        def phaseE():
            esE = ExitStack()
            with esE:
                sa = lambda n, sh, dt: esE.enter_context(nc.sbuf_tensor(n, sh, dt))
                ident = sa("ident", [128, 128], BF16)
                s_id = Slot()
                S.op("pool", lambda g_: g_.memset(ident[:], 0.0), writes=[s_id])
                S.op("pool", lambda g_: g_.affine_select(out=ident[:], in_=ident[:], pattern=[[-1, 128]], compare_op=ALU.not_equal, fill=1.0, base=0, channel_multiplier=1), reads=[s_id], writes=[s_id])
                skb = sa("skb", [128, 16, 128], BF16)
                s_skb = Slot()
                kq_ = S.dma_sem()
                S.dma("pool", skb[:], skT[:, :, :], kq_, writes=[s_skb])
                acc = sa("pacc", [128, 16, 512], F32)
                s_acc = Slot()
                hn = sa("hn", [128, 16, 512], BF16)
                s_hn = Slot()
                ssb = sa("ssb", [128, 4, 16, 128], F32)
                s_ssb = Slot()
                thr = sa("thr", [128, 4, 8], F32)
                s_thr = Slot()
                s1b = sa("s1b", [128, 4, 8, 128], F32)
                s_e12 = Slot()
                accq = S.dma_sem()
                outq = S.dma_sem()
                ub = Ring([(sa("ub%d" % i, [128, 16, 512], BF16), Slot()) for i in range(2)])
                ubq = [S.dma_sem() for _ in range(2)]
                hT_v = hTs.rearrange("(c p) t -> p c t", p=128)
                outT_v = outT.rearrange("(c p) t -> p c t", p=128)
                w_q_v = w_q.rearrange("(c p) n -> p c n", p=128)
                e_uT_v = e_uT.rearrange("(c p) e -> p c e", p=128)
                g2 = vec[:, 16:32]
                out_slots = []
                pq = Ring(PS[0:3])
                pT = [PS[3], PS[4]]
                pv = Ring(PS[5:8])
                for t in range(4):
                    ts_ = slice(t * 512, (t + 1) * 512)
                    S.dma("sp", acc[:], hT_v[:, :, ts_], accq, writes=[s_acc])
                    es1 = ExitStack()
                    with es1:
                        s1a = lambda n, sh, dt: es1.enter_context(nc.sbuf_tensor(n + '_%d' % t, sh, dt))
                        sq = s1a("esq", [128, 16, 512], BF16)
                        s_sq = Slot()
                        rs = s1a("ers", [128, 512], F32)
                        s_rs = Slot()
                        qT_ = s1a("eqT", [128, 16, 512], BF16)
                        s_qT = Slot()
                        m16 = s1a("m16", [128, 16, 16], F32)
                        s_m16 = Slot()
                        tmp = s1a("etmp", [128, 256], F32)
                        s_tmp = Slot()
                        cand = s1a("cand", [128, 8, 16, 16], F32)
                        s_cand = Slot()
                        t16 = s1a("t16", [128, 8, 16], F32)
                        s_t16 = Slot()
                        sm = s1a("sm", [128, 8, 8], F32)
                        s_sm = Slot()
                        S.op("act", lambda a: a.activation(out=sq[:], in_=acc[:], func=AF.Square), reads=[s_acc], writes=[s_sq])
                        pa_, s_pa_ = pq.next()
                        for c in range(16):
                            S.op("pe", lambda t_, c=c: t_.matmul(pa_[:], ones_b[:], sq[:, c, :], start=(c == 0), stop=(c == 15)), reads=[s_ones, s_sq], writes=[s_pa_], inc=(c == 15))
                        S.op("act", lambda a: a.activation(out=rs[:], in_=pa_[:], func=AF.Sqrt, bias=EPS, scale=1.0 / D), reads=[s_pa_], writes=[s_rs])
                        S.op("dve", lambda v: v.reciprocal(rs[:], rs[:]), reads=[s_rs], writes=[s_rs])
                        for c in range(16):
                            S.op("dve", lambda v, c=c: v.scalar_tensor_tensor(hn[:, c, :], acc[:, c, :], g2[:, c:c + 1], rs[:], ALU.mult, ALU.mult),
                                 reads=[s_acc, s_rs, s_vec], writes=[s_hn])
                        for ob in range(4):
                            u_, s_u = ub.next()
                            S.dma("pool", u_[:], w_q_v[:, :, ob * 512:(ob + 1) * 512], ubq[(ub.i - 1) % 2], writes=[s_u])
                            for cc in range(4):
                                pm, s_pm = pq.next()
                                for kc in range(16):
                                    S.op("pe", lambda t_, kc=kc: t_.matmul(pm[:], u_[:, kc, cc * 128:(cc + 1) * 128], hn[:, kc, :], start=(kc == 0), stop=(kc == 15)),
                                         reads=[s_u, s_hn], writes=[s_pm], inc=(kc == 15))
                                S.op("dve", lambda v: v.tensor_copy(qT_[:, ob * 4 + cc, :], pm[:]), reads=[s_pm], writes=[s_qT])
                        for sub in range(4):
                            for q4 in range(4):
                                pm, s_pm = pq.next()
                                for i4 in range(4):
                                    hc = q4 * 4 + i4
                                    S.op("pe", lambda t_, hc=hc, i4=i4: t_.matmul(pm[:, i4 * 128:(i4 + 1) * 128], qT_[:, hc, sub * 128:(sub + 1) * 128], skb[:, hc, :], start=True, stop=True),
                                         reads=[s_qT, s_skb], writes=[s_pm], inc=(i4 == 3))
                                S.op("dve", lambda v: v.tensor_copy(ssb[:, sub, q4 * 4:(q4 + 1) * 4, :], pm[:].rearrange("p (a n) -> p a n", a=4)), reads=[s_pm], writes=[s_ssb])
                            for hc in range(16):
                                S.op("dve", lambda v, hc=hc: v.max(out=m16[:, hc, 0:8], in_=ssb[:, sub, hc, :]), reads=[s_ssb], writes=[s_m16])
                                S.op("dve", lambda v, hc=hc: v.match_replace(out=tmp[:, 0:128], in_to_replace=m16[:, hc, 0:8], in_values=ssb[:, sub, hc, :], imm_value=-1e30), reads=[s_ssb, s_m16], writes=[s_tmp])
                                S.op("dve", lambda v, hc=hc: v.max(out=m16[:, hc, 8:16], in_=tmp[:, 0:128]), reads=[s_tmp], writes=[s_m16])
                            m16v = m16[:].rearrange("p (h c) k -> p h c k", c=2)
                            S.op("dve", lambda v: v.tensor_tensor(cand[:], m16v[:, :, 0, :].unsqueeze(3).to_broadcast([128, 8, 16, 16]),
                                                                  m16v[:, :, 1, :].unsqueeze(2).to_broadcast([128, 8, 16, 16]), ALU.add), reads=[s_m16], writes=[s_cand])
                            for h in range(8):
                                cv = cand[:, h].rearrange("p a b -> p (a b)")
                                S.op("dve", lambda v, h=h, cv=cv: v.max(out=t16[:, h, 0:8], in_=cv), reads=[s_cand], writes=[s_t16])
                                S.op("dve", lambda v, h=h, cv=cv: v.match_replace(out=tmp[:], in_to_replace=t16[:, h, 0:8], in_values=cv, imm_value=-1e30), reads=[s_cand, s_t16], writes=[s_tmp])
                                S.op("dve", lambda v, h=h: v.max(out=t16[:, h, 8:16], in_=tmp[:]), reads=[s_tmp], writes=[s_t16])
                            S.op("dve", lambda v: v.tensor_tensor(cand[:, 0:8, 0, :], t16[:], t16[:, :, 0:1].to_broadcast([128, 8, 16]), ALU.subtract), reads=[s_t16], writes=[s_cand])
                            S.op("act", lambda a: a.activation(out=cand[:, 0:8, 1, :], in_=cand[:, 0:8, 0, :], func=AF.Exp), reads=[s_cand], writes=[s_cand])
                            S.op("dve", lambda v: v.reduce_sum(sm[:, :, 0], cand[:, 0:8, 1, :], axis=AX.X), reads=[s_cand], writes=[s_sm])
                            S.op("act", lambda a: a.activation(out=sm[:, :, 1], in_=sm[:, :, 0], func=AF.Ln), reads=[s_sm], writes=[s_sm])
                            S.op("dve", lambda v: v.tensor_tensor(sm[:, :, 2], t16[:, :, 0], sm[:, :, 1], ALU.add), reads=[s_sm, s_t16], writes=[s_sm])
                            S.op("dve", lambda v: v.tensor_scalar(sm[:, :, 2], sm[:, :, 2], -1.0, None, ALU.mult), reads=[s_sm], writes=[s_sm])
                            S.op("dve", lambda v: v.scalar_tensor_tensor(thr[:, sub, :], t16[:, :, 15], -1e-4, sm[:, :, 2], ALU.add, ALU.add), reads=[s_sm, s_t16], writes=[s_thr])
                            s1v = ssb[:, sub].rearrange("p (h c) n -> p h c n", c=2)[:, :, 0, :]
                            S.op("dve", lambda v: v.tensor_tensor(s1v, s1v, sm[:, :, 2:3].to_broadcast([128, 8, 128]), ALU.add), reads=[s_ssb, s_sm], writes=[s_ssb])
                            s2v = ssb[:, sub].rearrange("p (h c) n -> p h c n", c=2)[:, :, 1, :]
                            S.op("act", lambda a: a.copy(out=s1b[:, sub], in_=s1v), reads=[s_ssb], writes=[s_e12])
                            S.op("dve", lambda v: v.tensor_tensor(s1v, thr[:, sub, :].unsqueeze(2).to_broadcast([128, 8, 128]), s1v, ALU.subtract), reads=[s_ssb, s_thr, s_e12], writes=[s_ssb])
                        barrier()
                    es2 = ExitStack()
                    with es2:
                        s2a = lambda n, sh, dt: es2.enter_context(nc.sbuf_tensor(n + '_%d' % t, sh, dt))
                        vbr = Ring([(s2a("vb%d" % i, [128, 4, 2048], BF16), Slot()) for i in range(2)])
                        vbq = [S.dma_sem() for _ in range(2)]
                        mkr = Ring([(s2a("mk%d" % i, [128, 2048], BF16), Slot()) for i in range(2)])
                        ewr = Ring([(s2a("ew%d" % i, [128, 2048], BF16), Slot()) for i in range(3)])
                        whr = Ring([(s2a("wh%d" % i, [128, 2, 512], BF16), Slot()) for i in range(2)])
                        Wb = s2a("Wb", [128, 4, 512], BF16)
                        s_Wb = [Slot() for _ in range(4)]
                        glr = Ring([(s2a("gl%d" % i, [128, 4, 512], BF16), Slot()) for i in range(2)])
                        AT = s2a("AT", [128, 4, 512], BF16)
                        s_AT = Slot()
                        stt_ = {}

                        def S1_load(eb):
                            u_, s_u = ub.next()
                            S.dma("pool", u_[:], e_uT_v[:, :, eb * 512:(eb + 1) * 512], ubq[(ub.i - 1) % 2], writes=[s_u])
                            vb, s_vb = vbr.next()
                            S.dma("pool", vb[:], e_v[eb * 512:(eb + 1) * 512, :].rearrange("(c p) d -> p c d", p=128), vbq[(vbr.i - 1) % 2], writes=[s_vb])
                            gl, s_gl = glr.next()
                            stt_[eb] = (vb, s_vb, gl, s_gl, u_, s_u)

                        def S1_score(eb, ecs):
                            vb, s_vb, gl, s_gl, u_, s_u = stt_[eb]
                            for ec in ecs:
                                pm, s_pm = pq.next()
                                for kc in range(16):
                                    S.op("pe", lambda t_, kc=kc: t_.matmul(pm[:], u_[:, kc, ec * 128:(ec + 1) * 128], hn[:, kc, :], start=(kc == 0), stop=(kc == 15)),
                                         reads=[s_u, s_hn], writes=[s_pm], inc=(kc == 15))
                                S.op("act", lambda a: a.activation(out=gl[:, ec, :], in_=pm[:], func=AF.Gelu), reads=[s_pm], writes=[s_gl])

                        cur_wh = {}

                        bst = {}

                        def step_of(g):
                            return g // 8, (g % 8) // 2, g % 2

                        def Sa(g):
                            eb, sub, hh = step_of(g)
                            mk, s_mk = mkr.next()
                            ew, s_ew = ewr.next()
                            bst[g] = (mk, s_mk, ew, s_ew)
                            sv = ssb[:, sub].rearrange("p (h c) n -> p h c n", c=2)
                            hs = slice(4 * hh, 4 * hh + 4)
                            isl = slice(4 * eb, 4 * eb + 4)
                            mk4 = mk[:].rearrange("p (h i j) -> p h i j", h=4, i=4)
                            ew4 = ew[:].rearrange("p (h i j) -> p h i j", h=4, i=4)
                            B4 = [128, 4, 4, 128]
                            for h_ in range(4):
                                for i_ in range(4):
                                    hh_, ii_ = 4 * hh + h_, 4 * eb + i_
                                    S.op("act", lambda a, h_=h_, i_=i_, hh_=hh_, ii_=ii_: a.activation(out=ew4[:, h_, i_, :], in_=sv[:, hh_, 1, :], func=AF.Exp, bias=s1b[:, sub, hh_, ii_:ii_ + 1]),
                                         reads=[s_ssb, s_e12], writes=[s_ew])
                            S.op("dve", lambda v: v.tensor_tensor(mk4, sv[:, hs, 1, :].unsqueeze(2).to_broadcast(B4), sv[:, hs, 0, isl].unsqueeze(3).to_broadcast(B4), ALU.is_ge),
                                 reads=[s_ssb], writes=[s_mk])

                        def Sc(g):
                            eb, sub, hh = step_of(g)
                            mk, s_mk, ew, s_ew = bst.pop(g)
                            if hh == 0:
                                cur_wh[sub] = whr.next()
                            wh, s_wh = cur_wh[sub]
                            S.op("dve", lambda g_: g_.tensor_tensor(mk[:], mk[:], ew[:], ALU.mult), reads=[s_mk, s_ew], writes=[s_mk])
                            mk2 = mk[:].rearrange("p (a e) -> p a e", a=2)
                            S.op("dve", lambda v: v.tensor_tensor(mk2[:, 0, :], mk2[:, 0, :], mk2[:, 1, :], ALU.add), reads=[s_mk], writes=[s_mk])
                            S.op("dve", lambda v: v.tensor_tensor(wh[:, hh, :], mk[:, 0:512], mk[:, 512:1024], ALU.add), reads=[s_mk], writes=[s_wh])
                            if hh == 1:
                                S2_fin(eb, sub)

                        GMAX = 32 * 8

                        def build_step(n):
                            if 0 <= n + 1 < GMAX:
                                Sa(n + 1)
                            if 0 <= n < GMAX:
                                Sc(n)

                        def S2_fin(eb, sub):
                            wh, s_wh = cur_wh[sub]
                            S.op("dve", lambda g_: g_.tensor_tensor(Wb[:, sub, :], wh[:, 0, :], wh[:, 1, :], ALU.add), reads=[s_wh], writes=[s_Wb[sub]])
                            for ec in range(4):
                                pt_, s_pt = pT[ec // 2]
                                dst = pt_[:].bitcast(BF16)[:, (ec % 2) * 512 + sub * 128:(ec % 2) * 512 + (sub + 1) * 128]
                                S.op("pe", lambda t_, dst=dst, ec=ec: t_.transpose(dst, Wb[:, sub, ec * 128:(ec + 1) * 128], ident[:]), reads=[s_Wb[sub], s_id], writes=[s_pt])

                        def S3_at(eb):
                            vb, s_vb, gl, s_gl, u_, s_u = stt_[eb]
                            for ec in range(4):
                                pt_, s_pt = pT[ec // 2]
                                src = pt_[:].bitcast(BF16)[:, (ec % 2) * 512:(ec % 2 + 1) * 512]
                                S.op("dve", lambda v, src=src, ec=ec: v.tensor_tensor(AT[:, ec, :], src, gl[:, ec, :], ALU.mult), reads=[s_pt, s_gl], writes=[s_AT])

                        def S3_v(eb, dcs):
                            vb, s_vb, gl, s_gl, u_, s_u = stt_[eb]
                            for dc in dcs:
                                po, s_po = pv.next()
                                for ec in range(4):
                                    S.op("pe", lambda t_, ec=ec: t_.matmul(po[:], vb[:, ec, dc * 128:(dc + 1) * 128], AT[:, ec, :], start=(ec == 0), stop=(ec == 3)),
                                         reads=[s_vb, s_AT], writes=[s_po], inc=(ec == 3))
                                S.op("dve", lambda v: v.tensor_tensor(acc[:, dc, :], po[:], acc[:, dc, :], ALU.add), reads=[s_po, s_acc], writes=[s_acc])

                        NEB = 32
                        S1_load(0)
                        S1_score(0, range(4))
                        for n in range(-1, 8):
                            build_step(n)
                        for eb in range(NEB):
                            nxt = eb + 1 < NEB
                            if nxt:
                                S1_load(eb + 1)
                            S3_at(eb)
                            for k2 in range(8):
                                if nxt:
                                    build_step((eb + 1) * 8 + k2)
                                S3_v(eb, [2 * k2, 2 * k2 + 1])
                                if nxt and k2 == 2:
                                    S1_score(eb + 1, [0, 1])
                                if nxt and k2 == 5:
                                    S1_score(eb + 1, [2, 3])
                            del stt_[eb]
                        sl = Slot()
                        out_slots.append(sl)
                        S.dma("sp", outT_v[:, :, ts_], acc[:], outq, reads=[s_acc], writes=[sl])
                        barrier()
                S.wait_all("sp", out_slots)
                barrier()

        if stage >= 1:
            phaseA()
        if stage >= 2:
            phaseC()
        if stage >= 3:
            phaseB()
        if stage >= 4:
            phaseD()
        if stage >= 5:
            phaseE()
        if stage < 5:
            fin = sbt("fin", [128, 16], F32)
            s_fin = Slot()
            S.op("dve", lambda v: v.memset(fin[:], 0.0), writes=[s_fin])
            dqo = S.dma_sem()
            so = Slot()
            S.dma("sp", outT[0:128, 0:16], fin[:], dqo, reads=[s_fin], writes=[so])
            S.wait_all("sp", [so])
    return nc


_CONST = {}


def _const_tables(j):
    key = ("t", j)
    if key in _CONST:
        return _CONST[key]
    r = np.arange(S_LEN)
    s_act = (r + OWN * j) % S_LEN
    k_act = (np.arange(OWN) + OWN * j)
    prod = (s_act[:, None].astype(np.int64) * k_act[None, :].astype(np.int64)) % S_LEN
    ang = prod.astype(np.float64) * (2.0 * np.pi / S_LEN)
    tabC = (np.cos(ang) / math.sqrt(S_LEN)).astype(np.float32).astype(ml_dtypes.bfloat16)
    tabS = (-np.sin(ang) / math.sqrt(S_LEN)).astype(np.float32).astype(ml_dtypes.bfloat16)
    _CONST[key] = (tabC, tabS)
    return _CONST[key]


def _shared_consts():
    if "s" in _CONST:
        return _CONST["s"]
    jj = np.arange(256)
    ang = (jj[:, None] * jj[None, :] % 256).astype(np.float64) * (2.0 * np.pi / 256)
    csc = np.concatenate([np.cos(ang), np.sin(ang)], axis=1).astype(np.float32) / 16.0
    p = np.arange(128)[:, None]
    mlin = (np.arange(512)[None, :] - p).astype(np.float32)
    mdiag = np.abs(np.arange(896)[None, :] - p - 384).astype(np.float32)
    _CONST["s"] = (csc, mlin, mdiag)
    return _CONST["s"]


def _prep_inputs(inp):
    x = np.asarray(inp["x"], np.float32)
    csc, mlin, mdiag = _shared_consts()
    vecs = np.zeros((128, 64), np.float32)
    vecs[:, 0:16] = np.asarray(inp["norm1_g"], np.float32).reshape(16, 128).T
    vecs[:, 16:32] = np.asarray(inp["norm2_g"], np.float32).reshape(16, 128).T
    vecs[:, 32] = np.asarray(inp["q_norm_g"], np.float32).reshape(128)
    vecs[:, 33] = np.asarray(inp["k_norm_g"], np.float32).reshape(128)
    vecs[:, 34:36] = np.asarray(inp["subln_g"], np.float32).reshape(2, 128).T
    vecs[:, 36] = np.asarray(inp["lambda_q1"], np.float32).reshape(128)
    vecs[:, 37] = np.asarray(inp["lambda_k1"], np.float32).reshape(128)
    vecs[:, 38] = np.asarray(inp["lambda_q2"], np.float32).reshape(128)
    vecs[:, 39] = np.asarray(inp["lambda_k2"], np.float32).reshape(128)
    w_in = np.ascontiguousarray(np.asarray(inp["w_in"], np.float32)[0])
    w_f = np.ascontiguousarray(np.asarray(inp["w_fourier"], np.float32)[0])
    w_a = np.ascontiguousarray(np.asarray(inp["w_attn"], np.float32)[0])
    w_o = np.ascontiguousarray(np.asarray(inp["w_out"], np.float32)[0])
    w_q = np.ascontiguousarray(np.asarray(inp["w_query"], np.float32)[0])
    sk = np.asarray(inp["sub_keys"], np.float32)[0]
    skT = np.ascontiguousarray(sk.reshape(16, 128, 128).transpose(2, 0, 1))
    e_uT = np.ascontiguousarray(np.asarray(inp["expert_u"], np.float32)[0].T)
    e_v = np.ascontiguousarray(np.asarray(inp["expert_v"], np.float32)[0])
    xTb = [np.ascontiguousarray(x[b].T) for b in range(2)]
    maps = []
    for c in range(8):
        b, j = c // 4, c % 4
        tabC, tabS = _const_tables(j)
        xT = np.ascontiguousarray(np.roll(xTb[b], -OWN * j, axis=1))
        atab = np.zeros((NH, 4, 64, 2), np.float32)
        for qb in range(4):
            q0 = OWN * j + qb * 512
            for kc in range(64):
                k0 = (kc * 128 + OWN * j) % S_LEN
                A = q0 - k0
                for h in range(NH):
                    atab[h, qb, kc, 0] = 1.0 if A > 0 else -1.0
                    atab[h, qb, kc, 1] = -SLOPES[h] * abs(A)
        atab = np.ascontiguousarray(np.broadcast_to(atab.reshape(1, 4096), (128, 4096)))
        maps.append({
            "atab": atab,
            "xT": xT, "w_in": w_in, "vecs": vecs, "csc": csc, "tabC": tabC, "tabS": tabS,
            "mlin": mlin, "mdiag": mdiag, "w_f": w_f, "w_a": w_a, "w_o": w_o, "w_q": w_q,
            "skT": skT, "e_uT": e_uT, "e_v": e_v,
        })
    return maps


def kernel(**inputs):
    maps = _prep_inputs(inputs)
    nc = build()
    res = run_bass_kernel_spmd(nc, maps, core_ids=list(range(8)), trace=True)
    out = np.empty((2, S_LEN, D), np.float32)
    for c in range(8):
        b, j = c // 4, c % 4
        out[b, j * OWN:(j + 1) * OWN, :] = res.results[c]["outT"].T
    return out
```

```python
import math
from contextlib import ExitStack
import numpy as np
import ml_dtypes
import concourse.bass as bass
import concourse.mybir as mybir
from concourse.bass_utils import run_bass_kernel_spmd

F32 = mybir.dt.float32
BF16 = mybir.dt.bfloat16
AF = mybir.ActivationFunctionType
ALU = mybir.AluOpType
AX = mybir.AxisListType

D = 2048
S_LEN = 8192
OWN = 2048
NH = 8
EPS = 1e-6
LAM_INIT = 0.8 - 0.6 * math.exp(-0.3 * 0)
SLOPES = [2.0 ** (-8.0 * (h + 1) / NH) for h in range(NH)]
NEXP = 16384


class Slot:
    __slots__ = ("w", "r")

    def __init__(self):
        self.w = None
        self.r = []


class Sched:
    def __init__(self, nc, es):
        self.nc = nc
        self.eng = {"pe": nc.tensor, "act": nc.scalar, "dve": nc.vector, "pool": nc.gpsimd, "sp": nc.sync}
        self.sem = {}
        self.cnt = {}
        for e in ("pe", "act", "dve", "pool"):
            self.sem[e] = es.enter_context(nc.semaphore("s_" + e))
            self.cnt[e] = 0
        self.seen = {e: {} for e in self.eng}
        self.es = es
        self.ndma = 0

    def dma_sem(self):
        self.ndma += 1
        s = self.es.enter_context(self.nc.semaphore("dq%d" % self.ndma))
        return [s, 0]

    def _wait(self, e, tok):
        if tok is None:
            return
        key, val = tok
        if key == "pe" and e == "pe":
            return
        if isinstance(key, str):
            sem, kid = self.sem[key], key
        else:
            sem, kid = key, id(key)
        if self.seen[e].get(kid, 0) >= val:
            return
        self.eng[e].wait_ge(sem, val)
        self.seen[e][kid] = val

    def _deps(self, e, reads, writes):
        for s in reads:
            self._wait(e, s.w)
        for s in writes:
            self._wait(e, s.w)
            for t in s.r:
                self._wait(e, t)

    def _commit(self, tok, reads, writes):
        for s in reads:
            s.r.append(tok)
            if len(s.r) > 40:
                best = {}
                for k, v in s.r:
                    kk = k if isinstance(k, str) else id(k)
                    if kk not in best or best[kk][1] < v:
                        best[kk] = (k, v)
                s.r = list(best.values())
        for s in writes:
            s.w = tok
            s.r = []

    def op(self, e, fn, reads=(), writes=(), inc=True):
        self._deps(e, reads, writes)
        ins = fn(self.eng[e])
        if inc:
            self.cnt[e] += 1
            ins.then_inc(self.sem[e], 1)
            tok = (e, self.cnt[e])
        else:
            tok = (e, self.cnt[e] + 1)
        self._commit(tok, reads, writes)
        return tok

    def dma(self, q, out, in_, dsem, reads=(), writes=()):
        self._deps(q, reads, writes)
        dsem[1] += 16
        self.eng[q].dma_start(out=out, in_=in_).then_inc(dsem[0], 16)
        tok = (dsem[0], dsem[1])
        self._commit(tok, reads, writes)
        return tok

    def wait_all(self, e, slots):
        for s in slots:
            self._wait(e, s.w)
            for t in s.r:
                self._wait(e, t)


class Ring:
    def __init__(self, items):
        self.items = items
        self.i = 0

    def next(self):
        it = self.items[self.i % len(self.items)]
        self.i += 1
        return it


def build(stage=99):
    nc = bass.Bass("TRN2", target_bir_lowering=False)
    dt_in = lambda n, sh, dt=F32: nc.dram_tensor(n, sh, dt, kind="ExternalInput").ap()
    xT = dt_in("xT", [D, S_LEN])
    import os as _os
    WEXP = bool(_os.environ.get("WBF16"))
    w_in = dt_in("w_in", [D, 11264], BF16 if WEXP else F32)
    vecs = dt_in("vecs", [128, 64])
    csc = dt_in("csc", [256, 512])
    if stage >= 2:
        tabC = dt_in("tabC", [S_LEN, OWN], BF16)
        tabS = dt_in("tabS", [S_LEN, OWN], BF16)
    if stage >= 3:
        mlin_d = dt_in("mlin", [128, 512])
        mdiag_d = dt_in("mdiag", [128, 896])
        atab_d = dt_in("atab", [128, 4096])
    if stage >= 4:
        w_f = dt_in("w_f", [1024, D])
        w_a = dt_in("w_a", [D, D])
        w_o = dt_in("w_o", [D, D])
    if stage >= 5:
        w_q = dt_in("w_q", [D, D])
        skT = dt_in("skT", [128, 16, 128])
        e_uT = dt_in("e_uT", [D, NEXP])
        e_v = dt_in("e_v", [NEXP, D])
    outT = nc.dram_tensor("outT", [D, OWN], F32, kind="ExternalOutput").ap()
    dbg = stage < 99
    dkind = "ExternalOutput" if dbg else "Internal"
    scr = lambda n, sh, dt=BF16: nc.dram_tensor(n, sh, dt, kind=dkind).ap()
    KT = scr("KT", [16, 128, S_LEN])
    QT = scr("QT", [16, 128, OWN])
    Vs = scr("Vs", [NH, S_LEN, 256])
    Zcs = scr("Zcs", [4, S_LEN, 512])
    G = scr("G", [2, D, OWN])
    oT = scr("oT", [D, OWN])
    YfT = scr("YfT", [1024, OWN])
    hTs = scr("hTs", [D, OWN], F32)

    es = ExitStack()
    with es:
        S = Sched(nc, es)
        sbt = lambda n, sh, dt: es.enter_context(nc.sbuf_tensor(n, sh, dt))
        PS = []
        for i in range(8):
            PS.append((es.enter_context(nc.psum_tensor("ps%d" % i, [128, 512], F32)), Slot()))
        pmain = Ring(PS[0:4])
        paux = Ring(PS[4:8])

        vec = sbt("vec", [128, 64], F32)
        s_vec = Slot()
        dq_c = S.dma_sem()
        S.dma("sp", vec[:], vecs[:, :], dq_c, writes=[s_vec])
        ones_b = sbt("ones_b", [128, 128], BF16)
        s_ones = Slot()
        S.op("dve", lambda v: v.memset(ones_b[:], 1.0), writes=[s_ones])
        g1 = vec[:, 0:16]
        gqk = sbt("gqk", [128, 2], F32)
        s_gqk = Slot()
        S.op("dve", lambda v: v.tensor_scalar(gqk[:, 0:1], vec[:, 32:33], 128.0 ** -0.5, None, ALU.mult), reads=[s_vec], writes=[s_gqk])
        S.op("dve", lambda v: v.tensor_copy(gqk[:, 1:2], vec[:, 33:34]), reads=[s_vec], writes=[s_gqk])

        cscb = sbt("cscb", [128, 2, 512], BF16)
        s_csc = Slot()
        dq_p = S.dma_sem()
        S.dma("pool", cscb[:], csc.rearrange("(c p) n -> p c n", p=128), dq_p, writes=[s_csc])

        def phaseA():
            esA = ExitStack()
            with esA:
                sa = lambda n, sh, dt: esA.enter_context(nc.sbuf_tensor(n, sh, dt))
                NT = 256
                xin = Ring([(sa("xin%d" % i, [128, 16, NT], F32), Slot()) for i in range(2)])
                sqb = Ring([(sa("sqb%d" % i, [128, 16, NT], BF16), Slot()) for i in range(1)])
                rst = Ring([(sa("rst%d" % i, [128, NT], F32), Slot()) for i in range(2)])
                xn = sa("xn", [128, 16, 2048], BF16)
                s_xn = [Slot() for _ in range(8)]
                wb = Ring([(sa("wb%d" % i, [128, 16, 512], BF16), Slot()) for i in range(3)])
                wdq = [S.dma_sem() for _ in range(3)]
                xdq = [S.dma_sem() for _ in range(2)]
                zT = Ring([(sa("zT%d" % i, [128, 2, 512], BF16), Slot()) for i in range(2)])
                zst = Ring([(sa("zst%d" % i, [128, 4, 512], BF16), Slot()) for i in range(2)])
                zdq = [S.dma_sem() for _ in range(2)]
                raw = Ring([(sa("raw%d" % i, [128, 512], F32), Slot()) for i in range(2)])
                sq2 = Ring([(sa("sq2%d" % i, [128, 512], BF16), Slot()) for i in range(2)])
                sd2 = Ring([(sa("sd2%d" % i, [128, 512], F32), Slot()) for i in range(2)])
                st4 = Ring([(sa("st4%d" % i, [128, 4, 512], BF16), Slot()) for i in range(3)])
                st4dq = [S.dma_sem() for _ in range(3)]
                vst = Ring([(sa("vst%d" % i, [128, 512], BF16), Slot()) for i in range(3)])
                vdq = [S.dma_sem() for _ in range(3)]
                w_in_v = w_in.rearrange("(c p) n -> p c n", p=128)
                xT_v = xT.rearrange("(c p) t -> p c t", p=128)
                scr_slots = []

                def xn_slots(t0, n):
                    return s_xn[t0 // NT:(t0 + n + NT - 1) // NT]

                import os
                for st in range(int(os.environ.get('PH_A_ST0', '0')), int(os.environ.get('PH_A_ST', '4'))):
                    own = None
                    is_own = (st == 0)
                    tok0 = st * 2048
                    for nt in range(8):
                        xi, s_xi = xin.next()
                        xq = xdq[(xin.i - 1) % 2]
                        S.dma("sp", xi[:], xT_v[:, :, tok0 + nt * NT: tok0 + (nt + 1) * NT], xq, writes=[s_xi])
                        sq, s_sq = sqb.next()
                        S.op("act", lambda a: a.activation(out=sq[:], in_=xi[:], func=AF.Square), reads=[s_xi], writes=[s_sq])
                        pa, s_pa = paux.next()
                        for c in range(16):
                            S.op("pe", lambda t, c=c: t.matmul(pa[:, 0:NT], ones_b[:], sq[:, c, :], start=(c == 0), stop=(c == 15)),
                                 reads=[s_ones, s_sq], writes=[s_pa], inc=(c == 15))
                        rs, s_rs = rst.next()
                        S.op("act", lambda a: a.activation(out=rs[:], in_=pa[:, 0:NT], func=AF.Sqrt, bias=EPS, scale=1.0 / D), reads=[s_pa], writes=[s_rs])
                        S.op("dve", lambda v: v.reciprocal(rs[:], rs[:]), reads=[s_rs], writes=[s_rs])
                        for c in range(16):
                            S.op("dve", lambda v, c=c: v.scalar_tensor_tensor(xn[:, c, nt * NT:(nt + 1) * NT], xi[:, c, :], g1[:, c:c + 1], rs[:], ALU.mult, ALU.mult),
                                 reads=[s_xi, s_rs, s_vec], writes=[s_xn[nt]])
                    blocks = [("z", 0), ("z", 1)]
                    if is_own:
                        blocks += [("q", i) for i in range(4)]
                    blocks += [("k", i) for i in range(4)] + [("v", i) for i in range(4)]
                    if is_own:
                        blocks += [("gf", i) for i in range(4)] + [("ga", i) for i in range(4)]
                    col_base = {"z": 0, "q": 1024, "k": 3072, "v": 5120, "gf": 7168, "ga": 9216}
                    kk = os.environ.get('PH_A_KINDS')
                    if kk is not None:
                        blocks = [b_ for b_ in blocks if b_[0] in kk.split(',')]
                    for kind, bi in blocks:
                        col0 = col_base[kind] + bi * 512
                        w, s_w = wb.next()
                        wq = wdq[(wb.i - 1) % 3]
                        S.dma("sp" if WEXP else "pool", w[:], w_in_v[:, :, col0:col0 + 512], wq, writes=[s_w])
                        if kind == "v":
                            for sub in range(16):
                                pm, s_pm = pmain.next()
                                for kc in range(16):
                                    S.op("pe", lambda t, kc=kc: t.matmul(pm[:], xn[:, kc, sub * 128:(sub + 1) * 128], w[:, kc, :], start=(kc == 0), stop=(kc == 15)),
                                         reads=[s_w] + xn_slots(sub * 128, 128), writes=[s_pm], inc=(kc == 15))
                                vs_, s_vs = vst.next()
                                vq = vdq[(vst.i - 1) % 3]
                                S.op("dve", lambda v: v.tensor_copy(vs_[:], pm[:]), reads=[s_pm], writes=[s_vs])
                                dst = Vs[2 * bi:2 * bi + 2, tok0 + sub * 128: tok0 + (sub + 1) * 128, :].rearrange("h t d -> t h d")
                                sl = Slot()
                                scr_slots.append(sl)
                                S.dma("sp", dst, vs_[:].rearrange("p (h d) -> p h d", h=2), vq, reads=[s_vs], writes=[sl])
                            continue
                        if kind == "z":
                            for gi in range(2):
                                g = 2 * bi + gi
                                for t in range(4):
                                    z, s_z = zT.next()
                                    for c2 in range(2):
                                        cc = gi * 2 + c2
                                        pm, s_pm = pmain.next()
                                        for kc in range(16):
                                            S.op("pe", lambda t_, kc=kc: t_.matmul(pm[:], w[:, kc, cc * 128:(cc + 1) * 128], xn[:, kc, t * 512:(t + 1) * 512], start=(kc == 0), stop=(kc == 15)),
                                                 reads=[s_w] + xn_slots(t * 512, 512), writes=[s_pm], inc=(kc == 15))
                                        S.op("dve", lambda v: v.tensor_copy(z[:, c2, :], pm[:]), reads=[s_pm], writes=[s_z])
                                    zs, s_zs = zst.next()
                                    zq = zdq[(zst.i - 1) % 2]
                                    for sub in range(4):
                                        pa, s_pa = paux.next()
                                        for c2 in range(2):
                                            S.op("pe", lambda t_, c2=c2: t_.matmul(pa[:], z[:, c2, sub * 128:(sub + 1) * 128], cscb[:, c2, :], start=(c2 == 0), stop=(c2 == 1)),
                                                 reads=[s_z, s_csc], writes=[s_pa], inc=(c2 == 1))
                                        S.op("dve", lambda v: v.tensor_copy(zs[:, sub, :], pa[:]), reads=[s_pa], writes=[s_zs])
                                    dst = Zcs[g, tok0 + t * 512: tok0 + (t + 1) * 512, :].rearrange("(s p) n -> p s n", p=128)
                                    sl = Slot()
                                    scr_slots.append(sl)
                                    S.dma("sp", dst, zs[:], zq, reads=[s_zs], writes=[sl])
                            continue
                        for cc in range(4):
                            stg, s_stg = st4.next()
                            sq_ = st4dq[(st4.i - 1) % 3]
                            for t in range(4):
                                pm, s_pm = pmain.next()
                                for kc in range(16):
                                    S.op("pe", lambda t_, kc=kc: t_.matmul(pm[:], w[:, kc, cc * 128:(cc + 1) * 128], xn[:, kc, t * 512:(t + 1) * 512], start=(kc == 0), stop=(kc == 15)),
                                         reads=[s_w] + xn_slots(t * 512, 512), writes=[s_pm], inc=(kc == 15))
                                if kind in ("gf", "ga"):
                                    S.op("act", lambda a: a.activation(out=stg[:, t, :], in_=pm[:], func=AF.Sigmoid), reads=[s_pm], writes=[s_stg])
                                else:
                                    r, s_r = raw.next()
                                    q2, s_q2 = sq2.next()
                                    d2, s_d2 = sd2.next()
                                    S.op("dve", lambda v: v.tensor_copy(r[:], pm[:]), reads=[s_pm], writes=[s_r])
                                    S.op("act", lambda a: a.activation(out=q2[:], in_=r[:], func=AF.Square), reads=[s_r], writes=[s_q2])
                                    pa, s_pa = (pmain if _os.environ.get("QKMAIN") else paux).next()
                                    S.op("pe", lambda t_: t_.matmul(pa[:], ones_b[:], q2[:], start=True, stop=True), reads=[s_ones, s_q2], writes=[s_pa])
                                    S.op("act", lambda a: a.activation(out=d2[:], in_=pa[:], func=AF.Sqrt, bias=EPS, scale=1.0 / 128), reads=[s_pa], writes=[s_d2])
                                    S.op("dve", lambda v: v.reciprocal(d2[:], d2[:]), reads=[s_d2], writes=[s_d2])
                                    gcol = gqk[:, 0:1] if kind == "q" else gqk[:, 1:2]
                                    S.op("dve", lambda v: v.scalar_tensor_tensor(stg[:, t, :], r[:], gcol, d2[:], ALU.mult, ALU.mult),
                                         reads=[s_r, s_d2, s_gqk], writes=[s_stg])
                            ch = bi * 4 + cc
                            if kind == "q":
                                dst = QT[ch, :, :].rearrange("p (t n) -> p t n", t=4)
                            elif kind == "k":
                                dst = KT[ch, :, tok0:tok0 + 2048].rearrange("p (t n) -> p t n", t=4)
                            else:
                                dst = G[0 if kind == "gf" else 1, ch * 128:(ch + 1) * 128, :].rearrange("p (t n) -> p t n", t=4)
                            sl = Slot()
                            scr_slots.append(sl)
                            S.dma("sp", dst, stg[:], sq_, reads=[s_stg], writes=[sl])
                for e in ("sp", "pool"):
                    S.wait_all(e, scr_slots)
                barrier()

        def barrier():
            for e in ("pe", "act", "dve", "pool", "sp"):
                for o in ("pe", "act", "dve", "pool"):
                    if S.cnt[o] > 0:
                        S._wait(e, (o, S.cnt[o]))

        def phaseC():
            esC = ExitStack()
            with esC:
                sa = lambda n, sh, dt: esC.enter_context(nc.sbuf_tensor(n, sh, dt))
                TC = sa("TC", [128, 64, 512], BF16)
                TS = sa("TS", [128, 64, 512], BF16)
                s_TC, s_TS = Slot(), Slot()
                tq = [S.dma_sem(), S.dma_sem()]
                zp = Ring([(sa("zp%d" % i, [128, 8, 512], BF16), Slot()) for i in range(3)])
                zq = [S.dma_sem() for _ in range(3)]
                yst = Ring([(sa("yst%d" % i, [128, 512], BF16), Slot()) for i in range(2)])
                yq = [S.dma_sem() for _ in range(2)]
                tabC_v = tabC.rearrange("(c p) k -> p c k", p=128)
                tabS_v = tabS.rearrange("(c p) k -> p c k", p=128)
                scr_slots = []
                for kb in range(4):
                    S.dma("sp", TC[:], tabC_v[:, :, kb * 512:(kb + 1) * 512], tq[0], writes=[s_TC])
                    S.dma("sp", TS[:], tabS_v[:, :, kb * 512:(kb + 1) * 512], tq[1], writes=[s_TS])
                    for g in range(4):
                        acc = [pmain.next(), pmain.next()]
                        for pi in range(8):
                            z, s_z = zp.next()
                            q_ = zq[(zp.i - 1) % 3]
                            S.dma("sp", z[:], Zcs[g, pi * 1024:(pi + 1) * 1024, :].rearrange("(c p) n -> p c n", p=128), q_, writes=[s_z])
                            for c in range(8):
                                sc = pi * 8 + c
                                for half in range(2):
                                    pa_, s_pa_ = acc[half]
                                    S.op("pe", lambda t_: t_.matmul(pa_[:], z[:, c, half * 128:(half + 1) * 128], TC[:, sc, :], start=(sc == 0), stop=False),
                                         reads=[s_z, s_TC], writes=[s_pa_], inc=False)
                                    S.op("pe", lambda t_: t_.matmul(pa_[:], z[:, c, 256 + half * 128:256 + (half + 1) * 128], TS[:, sc, :], start=False, stop=(sc == 63)),
                                         reads=[s_z, s_TS], writes=[s_pa_], inc=(c == 7))
                        for half in range(2):
                            pa_, s_pa_ = acc[half]
                            ys, s_ys = yst.next()
                            q_ = yq[(yst.i - 1) % 2]
                            S.op("dve", lambda v: v.tensor_copy(ys[:], pa_[:]), reads=[s_pa_], writes=[s_ys])
                            sl = Slot()
                            scr_slots.append(sl)
                            ch = g * 2 + half
                            S.dma("sp", YfT[ch * 128:(ch + 1) * 128, kb * 512:(kb + 1) * 512], ys[:], q_, reads=[s_ys], writes=[sl])
                for e in ("sp", "pool"):
                    S.wait_all(e, scr_slots)
                barrier()

        def phaseB():
            esB = ExitStack()
            with esB:
                sa = lambda n, sh, dt: esB.enter_context(nc.sbuf_tensor(n, sh, dt))
                mlin = sa("mlin_sb", [128, 512], F32)
                mdiag = sa("mdiag_sb", [128, 896], F32)
                at = sa("atab_sb", [128, 4096], F32)
                s_cst = Slot()
                cq = S.dma_sem()
                S.dma("sp", mlin[:], mlin_d[:, :], cq, writes=[s_cst])
                S.dma("sp", mdiag[:], mdiag_d[:, :], cq, writes=[s_cst])
                S.dma("sp", at[:], atab_d[:, :], cq, writes=[s_cst])
                ones_f = sa("ones_f", [128, 128], F32)
                s_of = Slot()
                S.op("dve", lambda v: v.memset(ones_f[:], 1.0), writes=[s_of])
                lam = sa("lam", [128, 8], F32)
                s_lam = Slot()
                S.op("dve", lambda v: v.tensor_tensor(lam[:, 0:1], vec[:, 36:37], vec[:, 37:38], ALU.mult), reads=[s_vec], writes=[s_lam])
                S.op("dve", lambda v: v.tensor_tensor(lam[:, 1:2], vec[:, 38:39], vec[:, 39:40], ALU.mult), reads=[s_vec], writes=[s_lam])
                pmx, s_pmx = PS[4]
                S.op("pe", lambda t_: t_.matmul(pmx[:, 0:2], ones_f[:], lam[:, 0:2], start=True, stop=True), reads=[s_of, s_lam], writes=[s_pmx])
                S.op("act", lambda a: a.activation(out=lam[:, 2:4], in_=pmx[:, 0:2], func=AF.Exp), reads=[s_pmx], writes=[s_lam])
                S.op("dve", lambda v: v.tensor_tensor(lam[:, 4:5], lam[:, 2:3], lam[:, 3:4], ALU.subtract), reads=[s_lam], writes=[s_lam])
                S.op("dve", lambda v: v.tensor_scalar(lam[:, 5:6], lam[:, 4:5], LAM_INIT, -1.0, ALU.add, ALU.mult), reads=[s_lam], writes=[s_lam])
                nlam = lam[:, 5:6]
                gsub = sa("gsub", [128, 2], F32)
                s_gsub = Slot()
                S.op("dve", lambda v: v.tensor_scalar(gsub[:], vec[:, 34:36], 1.0 - LAM_INIT, None, ALU.mult), reads=[s_vec], writes=[s_gsub])

                kt = Ring([(sa("kt%d" % i, [128, 2, S_LEN], BF16), Slot()) for i in range(2)])
                vt = Ring([(sa("vt%d" % i, [128, 64, 256], BF16), Slot()) for i in range(2)])
                qt = Ring([(sa("qt%d" % i, [128, 2, OWN], BF16), Slot()) for i in range(2)])
                hq = [S.dma_sem() for _ in range(2)]
                baseL = sa("baseL", [128, 512], F32)
                baseD = sa("baseD", [128, 896], F32)
                s_base = Slot()
                tt = Ring([(sa("tt%d" % i, [128, 512], F32), Slot()) for i in range(4)])
                pp = Ring([(sa("pp%d" % i, [128, 512], BF16), Slot()) for i in range(4)])
                ev = [sa("ev%d" % i, [128, 512], F32) for i in range(3)]
                s_ev = Slot()
                acc = sa("acc", [128, 2, 512], F32)
                s_acc = Slot()
                sqo = sa("sqo", [128, 2, 512], BF16)
                s_sqo = Slot()
                sdo = sa("sdo", [128, 512], F32)
                s_sdo = Slot()
                ost = Ring([(sa("ost%d" % i, [128, 2, 512], BF16), Slot()) for i in range(2)])
                oq = [S.dma_sem() for _ in range(2)]
                scr_slots = []
                ps_s = Ring(PS[0:4])
                (pO0, s_pO0), (pO1, s_pO1), (pZ, s_pZ) = PS[5], PS[6], PS[7]
                LAG = 3

                def needed_chunks(h, qb):
                    dmin = (2 * 16.0 + 22.0) / SLOPES[h]
                    out = []
                    for kc in range(64):
                        best = 1 << 30
                        for j in range(4):
                            k0 = (kc * 128 + OWN * j) % S_LEN
                            q0 = OWN * j + qb * 512
                            if k0 >= q0 + 512:
                                d = k0 - (q0 + 511)
                            elif k0 + 128 <= q0:
                                d = q0 - (k0 + 127)
                            else:
                                d = 0
                            best = min(best, d)
                        if best < dmin:
                            out.append(kc)
                    return out

                hbuf = {}

                def load_head(h):
                    k_, s_k = kt.next()
                    v_, s_v = vt.next()
                    q_, s_q = qt.next()
                    dq = hq[h % 2]
                    S.dma("sp", k_[:], KT[2 * h:2 * h + 2, :, :].rearrange("m p t -> p m t"), dq, writes=[s_k])
                    S.dma("sp", v_[:], Vs[h, :, :].rearrange("(c p) d -> p c d", p=128), dq, writes=[s_v])
                    S.dma("sp", q_[:], QT[2 * h:2 * h + 2, :, :].rearrange("m p t -> p m t"), dq, writes=[s_q])
                    hbuf[h] = (k_, s_k, v_, s_v, q_, s_q)

                load_head(0)
                for h in range(NH):
                    if h + 1 < NH:
                        load_head(h + 1)
                    k_, s_k, v_, s_v, q_, s_q = hbuf.pop(h)
                    S.op("pool", lambda g_: g_.tensor_scalar(baseL[:], mlin[:], -SLOPES[h], None, ALU.mult), reads=[s_cst], writes=[s_base])
                    S.op("pool", lambda g_: g_.tensor_scalar(baseD[:], mdiag[:], -SLOPES[h], None, ALU.mult), reads=[s_cst], writes=[s_base])
                    for qb in range(4):
                        for m in range(2):
                            tiles = {}

                            def issue_s(kc):
                                ps_, s_ps = ps_s.next()
                                S.op("pe", lambda t_: t_.matmul(ps_[:], k_[:, m, kc * 128:(kc + 1) * 128], q_[:, m, qb * 512:(qb + 1) * 512], start=True, stop=True),
                                     reads=[s_k, s_q], writes=[s_ps])
                                t, s_t = tt.next()
                                col = ((h * 4 + qb) * 64 + kc) * 2
                                diag = (qb * 4 <= kc < qb * 4 + 4)
                                if diag:
                                    delta = (kc - qb * 4) * 128
                                    bs = baseD[:, 384 - delta:384 - delta + 512]
                                    S.op("dve", lambda v: v.tensor_tensor(t[:], ps_[:], bs, ALU.add), reads=[s_ps, s_base], writes=[s_t])
                                else:
                                    S.op("dve", lambda v: v.scalar_tensor_tensor(t[:], baseL[:], at[:, col:col + 1], ps_[:], ALU.mult, ALU.add),
                                         reads=[s_ps, s_base, s_cst], writes=[s_t])
                                p, s_p = pp.next()
                                if diag:
                                    S.op("act", lambda a: a.activation(out=p[:], in_=t[:], func=AF.Exp), reads=[s_t], writes=[s_p])
                                else:
                                    S.op("act", lambda a: a.activation(out=p[:], in_=t[:], func=AF.Exp, bias=at[:, col + 1:col + 2]), reads=[s_t, s_cst], writes=[s_p])
                                tiles[kc] = (p, s_p)

                            def issue_pv(kc, st_, sp_):
                                p, s_p = tiles.pop(kc)
                                S.op("pe", lambda t_: t_.matmul(pO0[:], v_[:, kc, 0:128], p[:], start=st_, stop=sp_), reads=[s_v, s_p], writes=[s_pO0], inc=False)
                                S.op("pe", lambda t_: t_.matmul(pO1[:], v_[:, kc, 128:256], p[:], start=st_, stop=sp_), reads=[s_v, s_p], writes=[s_pO1], inc=False)
                                S.op("pe", lambda t_: t_.matmul(pZ[:], ones_b[:], p[:], start=st_, stop=sp_), reads=[s_ones, s_p], writes=[s_pZ], inc=True)

                            chunks = needed_chunks(h, qb)
                            nch = len(chunks)
                            for ix in range(nch + LAG):
                                if ix < nch:
                                    issue_s(chunks[ix])
                                if ix >= LAG:
                                    issue_pv(chunks[ix - LAG], ix - LAG == 0, ix - LAG == nch - 1)
                            S.op("dve", lambda v: v.reciprocal(ev[2][:], pZ[:]), reads=[s_pZ], writes=[s_ev])
                            if m == 0:
                                S.op("dve", lambda v: v.tensor_tensor(acc[:, 0, :], pO0[:], ev[2][:], ALU.mult), reads=[s_pO0, s_ev], writes=[s_acc])
                                S.op("dve", lambda v: v.tensor_tensor(acc[:, 1, :], pO1[:], ev[2][:], ALU.mult), reads=[s_pO1, s_ev], writes=[s_acc])
                            else:
                                S.op("dve", lambda v: v.tensor_tensor(ev[0][:], pO0[:], ev[2][:], ALU.mult), reads=[s_pO0, s_ev], writes=[s_ev])
                                S.op("dve", lambda v: v.tensor_tensor(ev[1][:], pO1[:], ev[2][:], ALU.mult), reads=[s_pO1, s_ev], writes=[s_ev])
                                for dv in range(2):
                                    S.op("dve", lambda v, dv=dv: v.scalar_tensor_tensor(acc[:, dv, :], ev[dv][:], nlam, acc[:, dv, :], ALU.mult, ALU.add),
                                         reads=[s_ev, s_lam, s_acc], writes=[s_acc])
                        S.op("act", lambda a: a.activation(out=sqo[:], in_=acc[:], func=AF.Square), reads=[s_acc], writes=[s_sqo])
                        for dv in range(2):
                            S.op("pe", lambda t_, dv=dv: t_.matmul(pmx[:], ones_b[:], sqo[:, dv, :], start=(dv == 0), stop=(dv == 1)), reads=[s_ones, s_sqo], writes=[s_pmx], inc=(dv == 1))
                        S.op("act", lambda a: a.activation(out=sdo[:], in_=pmx[:], func=AF.Sqrt, bias=EPS, scale=1.0 / 256), reads=[s_pmx], writes=[s_sdo])
                        S.op("dve", lambda v: v.reciprocal(sdo[:], sdo[:]), reads=[s_sdo], writes=[s_sdo])
                        os_, s_os = ost.next()
                        oq_ = oq[(ost.i - 1) % 2]
                        for dv in range(2):
                            S.op("dve", lambda v, dv=dv: v.scalar_tensor_tensor(os_[:, dv, :], acc[:, dv, :], gsub[:, dv:dv + 1], sdo[:], ALU.mult, ALU.mult),
                                 reads=[s_acc, s_gsub, s_sdo], writes=[s_os])
                        sl = Slot()
                        scr_slots.append(sl)
                        S.dma("sp", oT[h * 256:(h + 1) * 256, qb * 512:(qb + 1) * 512].rearrange("(c p) t -> p c t", p=128), os_[:], oq_, reads=[s_os], writes=[sl])
                for e in ("sp", "pool"):
                    S.wait_all(e, scr_slots)
                barrier()

        def phaseD():
            esD = ExitStack()
            with esD:
                sa = lambda n, sh, dt: esD.enter_context(nc.sbuf_tensor(n, sh, dt))
                yf = sa("yf", [128, 8, 512], BF16)
                ot = sa("ot", [128, 16, 512], BF16)
                gf = sa("gf", [128, 16, 512], BF16)
                ga = sa("ga", [128, 16, 512], BF16)
                xt = sa("xt", [128, 16, 512], F32)
                mixed = sa("mixed", [128, 16, 512], BF16)
                s_in, s_xt, s_mixed = Slot(), Slot(), Slot()
                inq = S.dma_sem()
                xq = S.dma_sem()
                hq_ = S.dma_sem()
                wf = Ring([(sa("wf%d" % i, [128, 8, 512], BF16), Slot()) for i in range(2)])
                wa = Ring([(sa("wa%d" % i, [128, 16, 512], BF16), Slot()) for i in range(2)])
                wo = Ring([(sa("wo%d" % i, [128, 16, 512], BF16), Slot()) for i in range(2)])
                wfq = [S.dma_sem() for _ in range(2)]
                waq = [S.dma_sem() for _ in range(2)]
                woq = [S.dma_sem() for _ in range(2)]
                t1 = Ring([(sa("t1_%d" % i, [128, 512], F32), Slot()) for i in range(2)])
                t2 = Ring([(sa("t2_%d" % i, [128, 512], F32), Slot()) for i in range(2)])
                YfT_v = YfT.rearrange("(c p) k -> p c k", p=128)
                oT_v = oT.rearrange("(c p) k -> p c k", p=128)
                Gf_v = G[0].rearrange("(c p) k -> p c k", p=128)
                Ga_v = G[1].rearrange("(c p) k -> p c k", p=128)
                xT_v = xT.rearrange("(c p) t -> p c t", p=128)
                hT_v = hTs.rearrange("(c p) t -> p c t", p=128)
                w_f_v = w_f.rearrange("(c p) n -> p c n", p=128)
                w_a_v = w_a.rearrange("(c p) n -> p c n", p=128)
                w_o_v = w_o.rearrange("(c p) n -> p c n", p=128)
                scr_slots = []
                for t in range(4):
                    ts_ = slice(t * 512, (t + 1) * 512)
                    S.dma("sp", yf[:], YfT_v[:, :, ts_], inq, writes=[s_in])
                    S.dma("sp", ot[:], oT_v[:, :, ts_], inq, writes=[s_in])
                    S.dma("sp", gf[:], Gf_v[:, :, ts_], inq, writes=[s_in])
                    S.dma("sp", ga[:], Ga_v[:, :, ts_], inq, writes=[s_in])
                    S.dma("sp", xt[:], xT_v[:, :, ts_], xq, writes=[s_xt])
                    for ob in range(4):
                        wf_, s_wf = wf.next()
                        wa_, s_wa = wa.next()
                        S.dma("pool", wf_[:], w_f_v[:, :, ob * 512:(ob + 1) * 512], wfq[(wf.i - 1) % 2], writes=[s_wf])
                        S.dma("pool", wa_[:], w_a_v[:, :, ob * 512:(ob + 1) * 512], waq[(wa.i - 1) % 2], writes=[s_wa])
                        for cc in range(4):
                            oc = ob * 4 + cc
                            pf, s_pf = pmain.next()
                            for kc in range(8):
                                S.op("pe", lambda t_, kc=kc: t_.matmul(pf[:], wf_[:, kc, cc * 128:(cc + 1) * 128], yf[:, kc, :], start=(kc == 0), stop=(kc == 7)),
                                     reads=[s_wf, s_in], writes=[s_pf], inc=(kc == 7))
                            pa_, s_pa_ = pmain.next()
                            for kc in range(16):
                                S.op("pe", lambda t_, kc=kc: t_.matmul(pa_[:], wa_[:, kc, cc * 128:(cc + 1) * 128], ot[:, kc, :], start=(kc == 0), stop=(kc == 15)),
                                     reads=[s_wa, s_in], writes=[s_pa_], inc=(kc == 15))
                            a1, s_a1 = t1.next()
                            a2, s_a2 = t2.next()
                            S.op("dve", lambda v: v.tensor_tensor(a1[:], pf[:], gf[:, oc, :], ALU.mult), reads=[s_pf, s_in], writes=[s_a1])
                            S.op("dve", lambda v: v.tensor_tensor(a2[:], pa_[:], ga[:, oc, :], ALU.mult), reads=[s_pa_, s_in], writes=[s_a2])
                            S.op("pool", lambda g_: g_.tensor_tensor(mixed[:, oc, :], a1[:], a2[:], ALU.add), reads=[s_a1, s_a2], writes=[s_mixed])
                    for ob in range(4):
                        wo_, s_wo = wo.next()
                        S.dma("pool", wo_[:], w_o_v[:, :, ob * 512:(ob + 1) * 512], woq[(wo.i - 1) % 2], writes=[s_wo])
                        for cc in range(4):
                            oc = ob * 4 + cc
                            ph, s_ph = pmain.next()
                            for kc in range(16):
                                S.op("pe", lambda t_, kc=kc: t_.matmul(ph[:], wo_[:, kc, cc * 128:(cc + 1) * 128], mixed[:, kc, :], start=(kc == 0), stop=(kc == 15)),
                                     reads=[s_wo, s_mixed], writes=[s_ph], inc=(kc == 15))
                            S.op("dve", lambda v: v.tensor_tensor(xt[:, oc, :], ph[:], xt[:, oc, :], ALU.add), reads=[s_ph, s_xt], writes=[s_xt])
                    sl = Slot()
                    scr_slots.append(sl)
                    S.dma("sp", hT_v[:, :, ts_], xt[:], hq_, reads=[s_xt], writes=[sl])
                for e in ("sp", "pool"):
                    S.wait_all(e, scr_slots)
                barrier()

        def phaseE():
            esE = ExitStack()
            with esE:
                sa = lambda n, sh, dt: esE.enter_context(nc.sbuf_tensor(n, sh, dt))
                ident = sa("ident", [128, 128], BF16)
                s_id = Slot()
                S.op("pool", lambda g_: g_.memset(ident[:], 0.0), writes=[s_id])
                S.op("pool", lambda g_: g_.affine_select(out=ident[:], in_=ident[:], pattern=[[-1, 128]], compare_op=ALU.not_equal, fill=1.0, base=0, channel_multiplier=1), reads=[s_id], writes=[s_id])
                skb = sa("skb", [128, 16, 128], BF16)
                s_skb = Slot()
                kq_ = S.dma_sem()
                S.dma("pool", skb[:], skT[:, :, :], kq_, writes=[s_skb])
                acc = sa("pacc", [128, 16, 512], F32)
                s_acc = Slot()
                hn = sa("hn", [128, 16, 512], BF16)
                s_hn = Slot()
                ssb = sa("ssb", [128, 4, 16, 128], F32)
                s_ssb = Slot()
                thr = sa("thr", [128, 4, 8], F32)
                s_thr = Slot()
                s1b = sa("s1b", [128, 4, 8, 128], F32)
                s_e12 = Slot()
                e1s = sa("e1s", [128, 4, 2, 128], BF16)
                e2s = sa("e2s", [128, 4, 2, 128], BF16)
                s_e67 = Slot()
                accq = S.dma_sem()
                outq = S.dma_sem()
                ub = Ring([(sa("ub%d" % i, [128, 16, 512], BF16), Slot()) for i in range(2)])
                ubq = [S.dma_sem() for _ in range(2)]
                hT_v = hTs.rearrange("(c p) t -> p c t", p=128)
                outT_v = outT.rearrange("(c p) t -> p c t", p=128)
                w_q_v = w_q.rearrange("(c p) n -> p c n", p=128)
                e_uT_v = e_uT.rearrange("(c p) e -> p c e", p=128)
                g2 = vec[:, 16:32]
                out_slots = []
                pq = Ring(PS[0:3])
                pT = [PS[3], PS[4]]
                pv = Ring(PS[5:8])
                for t in range(4):
                    ts_ = slice(t * 512, (t + 1) * 512)
                    S.dma("sp", acc[:], hT_v[:, :, ts_], accq, writes=[s_acc])
                    es1 = ExitStack()
                    with es1:
                        s1a = lambda n, sh, dt: es1.enter_context(nc.sbuf_tensor(n + '_%d' % t, sh, dt))
                        sq = s1a("esq", [128, 16, 512], BF16)
                        s_sq = Slot()
                        rs = s1a("ers", [128, 512], F32)
                        s_rs = Slot()
                        qT_ = s1a("eqT", [128, 16, 512], BF16)
                        s_qT = Slot()
                        m16 = s1a("m16", [128, 16, 16], F32)
                        s_m16 = Slot()
                        tmp = s1a("etmp", [128, 256], F32)
                        s_tmp = Slot()
                        cand = s1a("cand", [128, 8, 16, 16], F32)
                        s_cand = Slot()
                        t16 = s1a("t16", [128, 8, 16], F32)
                        s_t16 = Slot()
                        sm = s1a("sm", [128, 8, 8], F32)
                        s_sm = Slot()
                        S.op("act", lambda a: a.activation(out=sq[:], in_=acc[:], func=AF.Square), reads=[s_acc], writes=[s_sq])
                        pa_, s_pa_ = pq.next()
                        for c in range(16):
                            S.op("pe", lambda t_, c=c: t_.matmul(pa_[:], ones_b[:], sq[:, c, :], start=(c == 0), stop=(c == 15)), reads=[s_ones, s_sq], writes=[s_pa_], inc=(c == 15))
                        S.op("act", lambda a: a.activation(out=rs[:], in_=pa_[:], func=AF.Sqrt, bias=EPS, scale=1.0 / D), reads=[s_pa_], writes=[s_rs])
                        S.op("dve", lambda v: v.reciprocal(rs[:], rs[:]), reads=[s_rs], writes=[s_rs])
                        for c in range(16):
                            S.op("dve", lambda v, c=c: v.scalar_tensor_tensor(hn[:, c, :], acc[:, c, :], g2[:, c:c + 1], rs[:], ALU.mult, ALU.mult),
                                 reads=[s_acc, s_rs, s_vec], writes=[s_hn])
                        for ob in range(4):
                            u_, s_u = ub.next()
                            S.dma("pool", u_[:], w_q_v[:, :, ob * 512:(ob + 1) * 512], ubq[(ub.i - 1) % 2], writes=[s_u])
                            for cc in range(4):
                                pm, s_pm = pq.next()
                                for kc in range(16):
                                    S.op("pe", lambda t_, kc=kc: t_.matmul(pm[:], u_[:, kc, cc * 128:(cc + 1) * 128], hn[:, kc, :], start=(kc == 0), stop=(kc == 15)),
                                         reads=[s_u, s_hn], writes=[s_pm], inc=(kc == 15))
                                S.op("dve", lambda v: v.tensor_copy(qT_[:, ob * 4 + cc, :], pm[:]), reads=[s_pm], writes=[s_qT])
                        for sub in range(4):
                            for q4 in range(4):
                                pm, s_pm = pq.next()
                                for i4 in range(4):
                                    hc = q4 * 4 + i4
                                    S.op("pe", lambda t_, hc=hc, i4=i4: t_.matmul(pm[:, i4 * 128:(i4 + 1) * 128], qT_[:, hc, sub * 128:(sub + 1) * 128], skb[:, hc, :], start=True, stop=True),
                                         reads=[s_qT, s_skb], writes=[s_pm], inc=(i4 == 3))
                                S.op("dve", lambda v: v.tensor_copy(ssb[:, sub, q4 * 4:(q4 + 1) * 4, :], pm[:].rearrange("p (a n) -> p a n", a=4)), reads=[s_pm], writes=[s_ssb])
                            for hc in range(16):
                                S.op("dve", lambda v, hc=hc: v.max(out=m16[:, hc, 0:8], in_=ssb[:, sub, hc, :]), reads=[s_ssb], writes=[s_m16])
                                S.op("dve", lambda v, hc=hc: v.match_replace(out=tmp[:, 0:128], in_to_replace=m16[:, hc, 0:8], in_values=ssb[:, sub, hc, :], imm_value=-1e30), reads=[s_ssb, s_m16], writes=[s_tmp])
                                S.op("dve", lambda v, hc=hc: v.max(out=m16[:, hc, 8:16], in_=tmp[:, 0:128]), reads=[s_tmp], writes=[s_m16])
                            m16v = m16[:].rearrange("p (h c) k -> p h c k", c=2)
                            S.op("dve", lambda v: v.tensor_tensor(cand[:], m16v[:, :, 0, :].unsqueeze(3).to_broadcast([128, 8, 16, 16]),
                                                                  m16v[:, :, 1, :].unsqueeze(2).to_broadcast([128, 8, 16, 16]), ALU.add), reads=[s_m16], writes=[s_cand])
                            for h in range(8):
                                cv = cand[:, h].rearrange("p a b -> p (a b)")
                                S.op("dve", lambda v, h=h, cv=cv: v.max(out=t16[:, h, 0:8], in_=cv), reads=[s_cand], writes=[s_t16])
                                S.op("dve", lambda v, h=h, cv=cv: v.match_replace(out=tmp[:], in_to_replace=t16[:, h, 0:8], in_values=cv, imm_value=-1e30), reads=[s_cand, s_t16], writes=[s_tmp])
                                S.op("dve", lambda v, h=h: v.max(out=t16[:, h, 8:16], in_=tmp[:]), reads=[s_tmp], writes=[s_t16])
                            S.op("dve", lambda v: v.tensor_tensor(cand[:, 0:8, 0, :], t16[:], t16[:, :, 0:1].to_broadcast([128, 8, 16]), ALU.subtract), reads=[s_t16], writes=[s_cand])
                            S.op("act", lambda a: a.activation(out=cand[:, 0:8, 1, :], in_=cand[:, 0:8, 0, :], func=AF.Exp), reads=[s_cand], writes=[s_cand])
                            S.op("dve", lambda v: v.reduce_sum(sm[:, :, 0], cand[:, 0:8, 1, :], axis=AX.X), reads=[s_cand], writes=[s_sm])
                            S.op("act", lambda a: a.activation(out=sm[:, :, 1], in_=sm[:, :, 0], func=AF.Ln), reads=[s_sm], writes=[s_sm])
                            S.op("dve", lambda v: v.tensor_tensor(sm[:, :, 2], t16[:, :, 0], sm[:, :, 1], ALU.add), reads=[s_sm, s_t16], writes=[s_sm])
                            S.op("dve", lambda v: v.tensor_scalar(sm[:, :, 2], sm[:, :, 2], -1.0, None, ALU.mult), reads=[s_sm], writes=[s_sm])
                            S.op("dve", lambda v: v.scalar_tensor_tensor(thr[:, sub, :], t16[:, :, 15], -1e-4, sm[:, :, 2], ALU.add, ALU.add), reads=[s_sm, s_t16], writes=[s_thr])
                            s1v = ssb[:, sub].rearrange("p (h c) n -> p h c n", c=2)[:, :, 0, :]
                            S.op("dve", lambda v: v.tensor_tensor(s1v, s1v, sm[:, :, 2:3].to_broadcast([128, 8, 128]), ALU.add), reads=[s_ssb, s_sm], writes=[s_ssb])
                            s2v = ssb[:, sub].rearrange("p (h c) n -> p h c n", c=2)[:, :, 1, :]
                            S.op("act", lambda a: a.copy(out=s1b[:, sub], in_=s1v), reads=[s_ssb], writes=[s_e12])
                            S.op("act", lambda a: a.activation(out=e1s[:, sub], in_=s1v[:, 6:8, :], func=AF.Exp), reads=[s_ssb], writes=[s_e67])
                            S.op("act", lambda a: a.activation(out=e2s[:, sub], in_=s2v[:, 6:8, :], func=AF.Exp), reads=[s_ssb], writes=[s_e67])
                            S.op("dve", lambda v: v.tensor_tensor(s1v, thr[:, sub, :].unsqueeze(2).to_broadcast([128, 8, 128]), s1v, ALU.subtract), reads=[s_ssb, s_thr, s_e12], writes=[s_ssb])
                        barrier()
                    es2 = ExitStack()
                    with es2:
                        s2a = lambda n, sh, dt: es2.enter_context(nc.sbuf_tensor(n + '_%d' % t, sh, dt))
                        vbr = Ring([(s2a("vb%d" % i, [128, 4, 2048], BF16), Slot()) for i in range(2)])
                        vbq = [S.dma_sem() for _ in range(2)]
                        mkr = Ring([(s2a("mk%d" % i, [128, 2048], BF16), Slot()) for i in range(2)])
                        ewr = Ring([(s2a("ew%d" % i, [128, 2048], BF16), Slot()) for i in range(2)])
                        whr = Ring([(s2a("wh%d" % i, [128, 2, 512], BF16), Slot()) for i in range(2)])
                        Wb = s2a("Wb", [128, 4, 512], BF16)
                        s_Wb = [Slot() for _ in range(4)]
                        glr = Ring([(s2a("gl%d" % i, [128, 4, 512], BF16), Slot()) for i in range(2)])
                        AT = s2a("AT", [128, 4, 512], BF16)
                        s_AT = Slot()
                        stt_ = {}

                        def S1_load(eb):
                            u_, s_u = ub.next()
                            S.dma("pool", u_[:], e_uT_v[:, :, eb * 512:(eb + 1) * 512], ubq[(ub.i - 1) % 2], writes=[s_u])
                            vb, s_vb = vbr.next()
                            S.dma("pool", vb[:], e_v[eb * 512:(eb + 1) * 512, :].rearrange("(c p) d -> p c d", p=128), vbq[(vbr.i - 1) % 2], writes=[s_vb])
                            gl, s_gl = glr.next()
                            stt_[eb] = (vb, s_vb, gl, s_gl, u_, s_u)

                        def S1_score(eb, ecs):
                            vb, s_vb, gl, s_gl, u_, s_u = stt_[eb]
                            for ec in ecs:
                                pm, s_pm = pq.next()
                                for kc in range(16):
                                    S.op("pe", lambda t_, kc=kc: t_.matmul(pm[:], u_[:, kc, ec * 128:(ec + 1) * 128], hn[:, kc, :], start=(kc == 0), stop=(kc == 15)),
                                         reads=[s_u, s_hn], writes=[s_pm], inc=(kc == 15))
                                S.op("act", lambda a: a.activation(out=gl[:, ec, :], in_=pm[:], func=AF.Gelu), reads=[s_pm], writes=[s_gl])

                        cur_wh = {}

                        bst = {}

                        def step_of(g):
                            return g // 8, (g % 8) // 2, g % 2

                        def Sa(g):
                            eb, sub, hh = step_of(g)
                            mk, s_mk = mkr.next()
                            ew, s_ew = ewr.next()
                            bst[g] = (mk, s_mk, ew, s_ew)
                            sv = ssb[:, sub].rearrange("p (h c) n -> p h c n", c=2)
                            hs = slice(4 * hh, 4 * hh + 4)
                            isl = slice(4 * eb, 4 * eb + 4)
                            mk4 = mk[:].rearrange("p (h i j) -> p h i j", h=4, i=4)
                            ew4 = ew[:].rearrange("p (h i j) -> p h i j", h=4, i=4)
                            B4 = [128, 4, 4, 128]
                            for h_ in range(4 if hh == 0 else 2):
                                for i_ in range(4):
                                    hh_, ii_ = 4 * hh + h_, 4 * eb + i_
                                    S.op("act", lambda a, h_=h_, i_=i_, hh_=hh_, ii_=ii_: a.activation(out=ew4[:, h_, i_, :], in_=sv[:, hh_, 1, :], func=AF.Exp, bias=s1b[:, sub, hh_, ii_:ii_ + 1]),
                                         reads=[s_ssb, s_e12], writes=[s_ew])
                            S.op("dve", lambda v: v.tensor_tensor(mk4, sv[:, hs, 1, :].unsqueeze(2).to_broadcast(B4), sv[:, hs, 0, isl].unsqueeze(3).to_broadcast(B4), ALU.is_ge),
                                 reads=[s_ssb], writes=[s_mk])

                        def Sc(g):
                            eb, sub, hh = step_of(g)
                            mk, s_mk, ew, s_ew = bst.pop(g)
                            if hh == 0:
                                cur_wh[sub] = whr.next()
                            wh, s_wh = cur_wh[sub]
                            if hh == 0:
                                S.op("dve", lambda g_: g_.tensor_tensor(mk[:], mk[:], ew[:], ALU.mult), reads=[s_mk, s_ew], writes=[s_mk])
                            else:
                                isl = slice(4 * eb, 4 * eb + 4)
                                mk4 = mk[:].rearrange("p (h i j) -> p h i j", h=4, i=4)
                                B2 = [128, 2, 4, 128]
                                S.op("dve", lambda g_: g_.tensor_tensor(mk[:, 0:1024], mk[:, 0:1024], ew[:, 0:1024], ALU.mult), reads=[s_mk, s_ew], writes=[s_mk])
                                S.op("dve", lambda g_: g_.tensor_tensor(mk4[:, 2:4], mk4[:, 2:4], e2s[:, sub].unsqueeze(2).to_broadcast(B2), ALU.mult), reads=[s_mk, s_e67], writes=[s_mk])
                                S.op("dve", lambda g_: g_.tensor_tensor(mk4[:, 2:4], mk4[:, 2:4], e1s[:, sub, :, isl].unsqueeze(3).to_broadcast(B2), ALU.mult), reads=[s_mk, s_e67], writes=[s_mk])
                            mk2 = mk[:].rearrange("p (a e) -> p a e", a=2)
                            S.op("dve", lambda v: v.tensor_tensor(mk2[:, 0, :], mk2[:, 0, :], mk2[:, 1, :], ALU.add), reads=[s_mk], writes=[s_mk])
                            S.op("dve", lambda v: v.tensor_tensor(wh[:, hh, :], mk[:, 0:512], mk[:, 512:1024], ALU.add), reads=[s_mk], writes=[s_wh])
                            if hh == 1:
                                S2_fin(eb, sub)

                        GMAX = 32 * 8

                        def build_step(n):
                            if 0 <= n + 1 < GMAX:
                                Sa(n + 1)
                            if 0 <= n < GMAX:
                                Sc(n)

                        def S2_fin(eb, sub):
                            wh, s_wh = cur_wh[sub]
                            S.op("dve", lambda g_: g_.tensor_tensor(Wb[:, sub, :], wh[:, 0, :], wh[:, 1, :], ALU.add), reads=[s_wh], writes=[s_Wb[sub]])
                            for ec in range(4):
                                pt_, s_pt = pT[ec // 2]
                                dst = pt_[:].bitcast(BF16)[:, (ec % 2) * 512 + sub * 128:(ec % 2) * 512 + (sub + 1) * 128]
                                S.op("pe", lambda t_, dst=dst, ec=ec: t_.transpose(dst, Wb[:, sub, ec * 128:(ec + 1) * 128], ident[:]), reads=[s_Wb[sub], s_id], writes=[s_pt])

                        def S3_at(eb):
                            vb, s_vb, gl, s_gl, u_, s_u = stt_[eb]
                            for ec in range(4):
                                pt_, s_pt = pT[ec // 2]
                                src = pt_[:].bitcast(BF16)[:, (ec % 2) * 512:(ec % 2 + 1) * 512]
                                S.op("dve", lambda v, src=src, ec=ec: v.tensor_tensor(AT[:, ec, :], src, gl[:, ec, :], ALU.mult), reads=[s_pt, s_gl], writes=[s_AT])

                        def S3_v(eb, dcs):
                            vb, s_vb, gl, s_gl, u_, s_u = stt_[eb]
                            for dc in dcs:
                                po, s_po = pv.next()
                                for ec in range(4):
                                    S.op("pe", lambda t_, ec=ec: t_.matmul(po[:], vb[:, ec, dc * 128:(dc + 1) * 128], AT[:, ec, :], start=(ec == 0), stop=(ec == 3)),
                                         reads=[s_vb, s_AT], writes=[s_po], inc=(ec == 3))
                                S.op("dve", lambda v: v.tensor_tensor(acc[:, dc, :], po[:], acc[:, dc, :], ALU.add), reads=[s_po, s_acc], writes=[s_acc])

                        NEB = 32
                        S1_load(0)
                        S1_score(0, range(4))
                        for n in range(-1, 8):
                            build_step(n)
                        for eb in range(NEB):
                            nxt = eb + 1 < NEB
                            if nxt:
                                S1_load(eb + 1)
                            S3_at(eb)
                            for k2 in range(8):
                                if nxt:
                                    build_step((eb + 1) * 8 + k2)
                                S3_v(eb, [2 * k2, 2 * k2 + 1])
                                if nxt and k2 == 2:
                                    S1_score(eb + 1, [0, 1])
                                if nxt and k2 == 5:
                                    S1_score(eb + 1, [2, 3])
                            del stt_[eb]
                        sl = Slot()
                        out_slots.append(sl)
                        S.dma("sp", outT_v[:, :, ts_], acc[:], outq, reads=[s_acc], writes=[sl])
                        barrier()
                S.wait_all("sp", out_slots)
                barrier()

        if stage >= 1:
            phaseA()
        if stage >= 2:
            phaseC()
        if stage >= 3:
            phaseB()
        if stage >= 4:
            phaseD()
        if stage >= 5:
            phaseE()
        if stage < 5:
            fin = sbt("fin", [128, 16], F32)
            s_fin = Slot()
            S.op("dve", lambda v: v.memset(fin[:], 0.0), writes=[s_fin])
            dqo = S.dma_sem()
            so = Slot()
            S.dma("sp", outT[0:128, 0:16], fin[:], dqo, reads=[s_fin], writes=[so])
            S.wait_all("sp", [so])
    return nc


_CONST = {}


def _const_tables(j):
    key = ("t", j)
    if key in _CONST:
        return _CONST[key]
    r = np.arange(S_LEN)
    s_act = (r + OWN * j) % S_LEN
    k_act = (np.arange(OWN) + OWN * j)
    prod = (s_act[:, None].astype(np.int64) * k_act[None, :].astype(np.int64)) % S_LEN
    ang = prod.astype(np.float64) * (2.0 * np.pi / S_LEN)
    tabC = (np.cos(ang) / math.sqrt(S_LEN)).astype(np.float32).astype(ml_dtypes.bfloat16)
    tabS = (-np.sin(ang) / math.sqrt(S_LEN)).astype(np.float32).astype(ml_dtypes.bfloat16)
    _CONST[key] = (tabC, tabS)
    return _CONST[key]


def _shared_consts():
    if "s" in _CONST:
        return _CONST["s"]
    jj = np.arange(256)
    ang = (jj[:, None] * jj[None, :] % 256).astype(np.float64) * (2.0 * np.pi / 256)
    csc = np.concatenate([np.cos(ang), np.sin(ang)], axis=1).astype(np.float32) / 16.0
    p = np.arange(128)[:, None]
    mlin = (np.arange(512)[None, :] - p).astype(np.float32)
    mdiag = np.abs(np.arange(896)[None, :] - p - 384).astype(np.float32)
    _CONST["s"] = (csc, mlin, mdiag)
    return _CONST["s"]


def _prep_inputs(inp):
    x = np.asarray(inp["x"], np.float32)
    csc, mlin, mdiag = _shared_consts()
    vecs = np.zeros((128, 64), np.float32)
    vecs[:, 0:16] = np.asarray(inp["norm1_g"], np.float32).reshape(16, 128).T
    vecs[:, 16:32] = np.asarray(inp["norm2_g"], np.float32).reshape(16, 128).T
    vecs[:, 32] = np.asarray(inp["q_norm_g"], np.float32).reshape(128)
    vecs[:, 33] = np.asarray(inp["k_norm_g"], np.float32).reshape(128)
    vecs[:, 34:36] = np.asarray(inp["subln_g"], np.float32).reshape(2, 128).T
    vecs[:, 36] = np.asarray(inp["lambda_q1"], np.float32).reshape(128)
    vecs[:, 37] = np.asarray(inp["lambda_k1"], np.float32).reshape(128)
    vecs[:, 38] = np.asarray(inp["lambda_q2"], np.float32).reshape(128)
    vecs[:, 39] = np.asarray(inp["lambda_k2"], np.float32).reshape(128)
    w_in = np.ascontiguousarray(np.asarray(inp["w_in"], np.float32)[0])
    w_f = np.ascontiguousarray(np.asarray(inp["w_fourier"], np.float32)[0])
    w_a = np.ascontiguousarray(np.asarray(inp["w_attn"], np.float32)[0])
    w_o = np.ascontiguousarray(np.asarray(inp["w_out"], np.float32)[0])
    w_q = np.ascontiguousarray(np.asarray(inp["w_query"], np.float32)[0])
    sk = np.asarray(inp["sub_keys"], np.float32)[0]
    skT = np.ascontiguousarray(sk.reshape(16, 128, 128).transpose(2, 0, 1))
    e_uT = np.ascontiguousarray(np.asarray(inp["expert_u"], np.float32)[0].T)
    e_v = np.ascontiguousarray(np.asarray(inp["expert_v"], np.float32)[0])
    xTb = [np.ascontiguousarray(x[b].T) for b in range(2)]
    maps = []
    for c in range(8):
        b, j = c // 4, c % 4
        tabC, tabS = _const_tables(j)
        xT = np.ascontiguousarray(np.roll(xTb[b], -OWN * j, axis=1))
        atab = np.zeros((NH, 4, 64, 2), np.float32)
        for qb in range(4):
            q0 = OWN * j + qb * 512
            for kc in range(64):
                k0 = (kc * 128 + OWN * j) % S_LEN
                A = q0 - k0
                for h in range(NH):
                    atab[h, qb, kc, 0] = 1.0 if A > 0 else -1.0
                    atab[h, qb, kc, 1] = -SLOPES[h] * abs(A)
        atab = np.ascontiguousarray(np.broadcast_to(atab.reshape(1, 4096), (128, 4096)))
        maps.append({
            "atab": atab,
            "xT": xT, "w_in": w_in, "vecs": vecs, "csc": csc, "tabC": tabC, "tabS": tabS,
            "mlin": mlin, "mdiag": mdiag, "w_f": w_f, "w_a": w_a, "w_o": w_o, "w_q": w_q,
            "skT": skT, "e_uT": e_uT, "e_v": e_v,
        })
    return maps


def kernel(**inputs):
    maps = _prep_inputs(inputs)
    nc = build()
    res = run_bass_kernel_spmd(nc, maps, core_ids=list(range(8)), trace=True)
    out = np.empty((2, S_LEN, D), np.float32)
    for c in range(8):
        b, j = c // 4, c % 4
        out[b, j * OWN:(j + 1) * OWN, :] = res.results[c]["outT"].T
    return out
```

```python
import math
from contextlib import ExitStack
import numpy as np
import ml_dtypes
import concourse.bass as bass
import concourse.mybir as mybir
from concourse.bass_utils import run_bass_kernel_spmd

F32 = mybir.dt.float32
BF16 = mybir.dt.bfloat16
AF = mybir.ActivationFunctionType
ALU = mybir.AluOpType
AX = mybir.AxisListType

D = 2048
S_LEN = 8192
OWN = 2048
NH = 8
EPS = 1e-6
LAM_INIT = 0.8 - 0.6 * math.exp(-0.3 * 0)
SLOPES = [2.0 ** (-8.0 * (h + 1) / NH) for h in range(NH)]
NEXP = 16384


class Slot:
    __slots__ = ("w", "r")

    def __init__(self):
        self.w = None
        self.r = []


class Sched:
    def __init__(self, nc, es):
        self.nc = nc
        self.eng = {"pe": nc.tensor, "act": nc.scalar, "dve": nc.vector, "pool": nc.gpsimd, "sp": nc.sync}
        self.sem = {}
        self.cnt = {}
        for e in ("pe", "act", "dve", "pool"):
            self.sem[e] = es.enter_context(nc.semaphore("s_" + e))
            self.cnt[e] = 0
        self.seen = {e: {} for e in self.eng}
        self.es = es
        self.ndma = 0

    def dma_sem(self):
        self.ndma += 1
        s = self.es.enter_context(self.nc.semaphore("dq%d" % self.ndma))
        return [s, 0]

    def _wait(self, e, tok):
        if tok is None:
            return
        key, val = tok
        if key == "pe" and e == "pe":
            return
        if isinstance(key, str):
            sem, kid = self.sem[key], key
        else:
            sem, kid = key, id(key)
        if self.seen[e].get(kid, 0) >= val:
            return
        self.eng[e].wait_ge(sem, val)
        self.seen[e][kid] = val

    def _deps(self, e, reads, writes):
        for s in reads:
            self._wait(e, s.w)
        for s in writes:
            self._wait(e, s.w)
            for t in s.r:
                self._wait(e, t)

    def _commit(self, tok, reads, writes):
        for s in reads:
            s.r.append(tok)
            if len(s.r) > 40:
                best = {}
                for k, v in s.r:
                    kk = k if isinstance(k, str) else id(k)
                    if kk not in best or best[kk][1] < v:
                        best[kk] = (k, v)
                s.r = list(best.values())
        for s in writes:
            s.w = tok
            s.r = []

    def op(self, e, fn, reads=(), writes=(), inc=True):
        self._deps(e, reads, writes)
        ins = fn(self.eng[e])
        if inc:
            self.cnt[e] += 1
            ins.then_inc(self.sem[e], 1)
            tok = (e, self.cnt[e])
        else:
            tok = (e, self.cnt[e] + 1)
        self._commit(tok, reads, writes)
        return tok

    def dma(self, q, out, in_, dsem, reads=(), writes=()):
        self._deps(q, reads, writes)
        dsem[1] += 16
        self.eng[q].dma_start(out=out, in_=in_).then_inc(dsem[0], 16)
        tok = (dsem[0], dsem[1])
        self._commit(tok, reads, writes)
        return tok

    def wait_all(self, e, slots):
        for s in slots:
            self._wait(e, s.w)
            for t in s.r:
                self._wait(e, t)


class Ring:
    def __init__(self, items):
        self.items = items
        self.i = 0

    def next(self):
        it = self.items[self.i % len(self.items)]
        self.i += 1
        return it


def build(stage=99):
    nc = bass.Bass("TRN2", target_bir_lowering=False)
    dt_in = lambda n, sh, dt=F32: nc.dram_tensor(n, sh, dt, kind="ExternalInput").ap()
    xT = dt_in("xT", [D, S_LEN])
    import os as _os
    WEXP = bool(_os.environ.get("WBF16"))
    w_in = dt_in("w_in", [D, 11264], BF16 if WEXP else F32)
    vecs = dt_in("vecs", [128, 64])
    csc = dt_in("csc", [256, 512])
    if stage >= 2:
        tabC = dt_in("tabC", [S_LEN, OWN], BF16)
        tabS = dt_in("tabS", [S_LEN, OWN], BF16)
    if stage >= 3:
        mlin_d = dt_in("mlin", [128, 512])
        mdiag_d = dt_in("mdiag", [128, 896])
        atab_d = dt_in("atab", [128, 4096])
    if stage >= 4:
        w_f = dt_in("w_f", [1024, D])
        w_a = dt_in("w_a", [D, D])
        w_o = dt_in("w_o", [D, D])
    if stage >= 5:
        w_q = dt_in("w_q", [D, D])
        skT = dt_in("skT", [128, 16, 128])
        e_uT = dt_in("e_uT", [D, NEXP])
        e_v = dt_in("e_v", [NEXP, D])
    outT = nc.dram_tensor("outT", [D, OWN], F32, kind="ExternalOutput").ap()
    dbg = stage < 99
    dkind = "ExternalOutput" if dbg else "Internal"
    scr = lambda n, sh, dt=BF16: nc.dram_tensor(n, sh, dt, kind=dkind).ap()
    KT = scr("KT", [16, 128, S_LEN])
    QT = scr("QT", [16, 128, OWN])
    Vs = scr("Vs", [NH, S_LEN, 256])
    Zcs = scr("Zcs", [4, S_LEN, 512])
    G = scr("G", [2, D, OWN])
    oT = scr("oT", [D, OWN])
    YfT = scr("YfT", [1024, OWN])
    hTs = scr("hTs", [D, OWN], F32)

    es = ExitStack()
    with es:
        S = Sched(nc, es)
        sbt = lambda n, sh, dt: es.enter_context(nc.sbuf_tensor(n, sh, dt))
        PS = []
        for i in range(8):
            PS.append((es.enter_context(nc.psum_tensor("ps%d" % i, [128, 512], F32)), Slot()))
        pmain = Ring(PS[0:4])
        paux = Ring(PS[4:8])

        vec = sbt("vec", [128, 64], F32)
        s_vec = Slot()
        dq_c = S.dma_sem()
        S.dma("sp", vec[:], vecs[:, :], dq_c, writes=[s_vec])
        ones_b = sbt("ones_b", [128, 128], BF16)
        s_ones = Slot()
        S.op("dve", lambda v: v.memset(ones_b[:], 1.0), writes=[s_ones])
        g1 = vec[:, 0:16]
        gqk = sbt("gqk", [128, 2], F32)
        s_gqk = Slot()
        S.op("dve", lambda v: v.tensor_scalar(gqk[:, 0:1], vec[:, 32:33], 128.0 ** -0.5, None, ALU.mult), reads=[s_vec], writes=[s_gqk])
        S.op("dve", lambda v: v.tensor_copy(gqk[:, 1:2], vec[:, 33:34]), reads=[s_vec], writes=[s_gqk])

        cscb = sbt("cscb", [128, 2, 512], BF16)
        s_csc = Slot()
        dq_p = S.dma_sem()
        S.dma("pool", cscb[:], csc.rearrange("(c p) n -> p c n", p=128), dq_p, writes=[s_csc])

        def phaseA():
            esA = ExitStack()
            with esA:
                sa = lambda n, sh, dt: esA.enter_context(nc.sbuf_tensor(n, sh, dt))
                NT = 256
                xin = Ring([(sa("xin%d" % i, [128, 16, NT], F32), Slot()) for i in range(2)])
                sqb = Ring([(sa("sqb%d" % i, [128, 16, NT], BF16), Slot()) for i in range(1)])
                rst = Ring([(sa("rst%d" % i, [128, NT], F32), Slot()) for i in range(2)])
                xn = sa("xn", [128, 16, 2048], BF16)
                s_xn = [Slot() for _ in range(8)]
                wb = Ring([(sa("wb%d" % i, [128, 16, 512], BF16), Slot()) for i in range(3)])
                wdq = [S.dma_sem() for _ in range(3)]
                xdq = [S.dma_sem() for _ in range(2)]
                zT = Ring([(sa("zT%d" % i, [128, 2, 512], BF16), Slot()) for i in range(2)])
                zst = Ring([(sa("zst%d" % i, [128, 4, 512], BF16), Slot()) for i in range(2)])
                zdq = [S.dma_sem() for _ in range(2)]
                raw = Ring([(sa("raw%d" % i, [128, 512], F32), Slot()) for i in range(2)])
                sq2 = Ring([(sa("sq2%d" % i, [128, 512], BF16), Slot()) for i in range(2)])
                sd2 = Ring([(sa("sd2%d" % i, [128, 512], F32), Slot()) for i in range(2)])
                st4 = Ring([(sa("st4%d" % i, [128, 4, 512], BF16), Slot()) for i in range(3)])
                st4dq = [S.dma_sem() for _ in range(3)]
                vst = Ring([(sa("vst%d" % i, [128, 512], BF16), Slot()) for i in range(3)])
                vdq = [S.dma_sem() for _ in range(3)]
                w_in_v = w_in.rearrange("(c p) n -> p c n", p=128)
                xT_v = xT.rearrange("(c p) t -> p c t", p=128)
                scr_slots = []

                def xn_slots(t0, n):
                    return s_xn[t0 // NT:(t0 + n + NT - 1) // NT]

                import os
                for st in range(int(os.environ.get('PH_A_ST0', '0')), int(os.environ.get('PH_A_ST', '4'))):
                    own = None
                    is_own = (st == 0)
                    tok0 = st * 2048
                    for nt in range(8):
                        xi, s_xi = xin.next()
                        xq = xdq[(xin.i - 1) % 2]
                        S.dma("sp", xi[:], xT_v[:, :, tok0 + nt * NT: tok0 + (nt + 1) * NT], xq, writes=[s_xi])
                        sq, s_sq = sqb.next()
                        S.op("act", lambda a: a.activation(out=sq[:], in_=xi[:], func=AF.Square), reads=[s_xi], writes=[s_sq])
                        pa, s_pa = paux.next()
                        for c in range(16):
                            S.op("pe", lambda t, c=c: t.matmul(pa[:, 0:NT], ones_b[:], sq[:, c, :], start=(c == 0), stop=(c == 15)),
                                 reads=[s_ones, s_sq], writes=[s_pa], inc=(c == 15))
                        rs, s_rs = rst.next()
                        S.op("act", lambda a: a.activation(out=rs[:], in_=pa[:, 0:NT], func=AF.Sqrt, bias=EPS, scale=1.0 / D), reads=[s_pa], writes=[s_rs])
                        S.op("dve", lambda v: v.reciprocal(rs[:], rs[:]), reads=[s_rs], writes=[s_rs])
                        for c in range(16):
                            S.op("dve", lambda v, c=c: v.scalar_tensor_tensor(xn[:, c, nt * NT:(nt + 1) * NT], xi[:, c, :], g1[:, c:c + 1], rs[:], ALU.mult, ALU.mult),
                                 reads=[s_xi, s_rs, s_vec], writes=[s_xn[nt]])
                    blocks = [("z", 0), ("z", 1)]
                    if is_own:
                        blocks += [("q", i) for i in range(4)]
                    blocks += [("k", i) for i in range(4)] + [("v", i) for i in range(4)]
                    if is_own:
                        blocks += [("gf", i) for i in range(4)] + [("ga", i) for i in range(4)]
                    col_base = {"z": 0, "q": 1024, "k": 3072, "v": 5120, "gf": 7168, "ga": 9216}
                    kk = os.environ.get('PH_A_KINDS')
                    if kk is not None:
                        blocks = [b_ for b_ in blocks if b_[0] in kk.split(',')]
                    for kind, bi in blocks:
                        col0 = col_base[kind] + bi * 512
                        w, s_w = wb.next()
                        wq = wdq[(wb.i - 1) % 3]
                        S.dma("sp" if WEXP else "pool", w[:], w_in_v[:, :, col0:col0 + 512], wq, writes=[s_w])
                        if kind == "v":
                            for sub in range(16):
                                pm, s_pm = pmain.next()
                                for kc in range(16):
                                    S.op("pe", lambda t, kc=kc: t.matmul(pm[:], xn[:, kc, sub * 128:(sub + 1) * 128], w[:, kc, :], start=(kc == 0), stop=(kc == 15)),
                                         reads=[s_w] + xn_slots(sub * 128, 128), writes=[s_pm], inc=(kc == 15))
                                vs_, s_vs = vst.next()
                                vq = vdq[(vst.i - 1) % 3]
                                S.op("dve", lambda v: v.tensor_copy(vs_[:], pm[:]), reads=[s_pm], writes=[s_vs])
                                dst = Vs[2 * bi:2 * bi + 2, tok0 + sub * 128: tok0 + (sub + 1) * 128, :].rearrange("h t d -> t h d")
                                sl = Slot()
                                scr_slots.append(sl)
                                S.dma("sp", dst, vs_[:].rearrange("p (h d) -> p h d", h=2), vq, reads=[s_vs], writes=[sl])
                            continue
                        if kind == "z":
                            for gi in range(2):
                                g = 2 * bi + gi
                                for t in range(4):
                                    z, s_z = zT.next()
                                    for c2 in range(2):
                                        cc = gi * 2 + c2
                                        pm, s_pm = pmain.next()
                                        for kc in range(16):
                                            S.op("pe", lambda t_, kc=kc: t_.matmul(pm[:], w[:, kc, cc * 128:(cc + 1) * 128], xn[:, kc, t * 512:(t + 1) * 512], start=(kc == 0), stop=(kc == 15)),
                                                 reads=[s_w] + xn_slots(t * 512, 512), writes=[s_pm], inc=(kc == 15))
                                        S.op("dve", lambda v: v.tensor_copy(z[:, c2, :], pm[:]), reads=[s_pm], writes=[s_z])
                                    zs, s_zs = zst.next()
                                    zq = zdq[(zst.i - 1) % 2]
                                    for sub in range(4):
                                        pa, s_pa = paux.next()
                                        for c2 in range(2):
                                            S.op("pe", lambda t_, c2=c2: t_.matmul(pa[:], z[:, c2, sub * 128:(sub + 1) * 128], cscb[:, c2, :], start=(c2 == 0), stop=(c2 == 1)),
                                                 reads=[s_z, s_csc], writes=[s_pa], inc=(c2 == 1))
                                        S.op("dve", lambda v: v.tensor_copy(zs[:, sub, :], pa[:]), reads=[s_pa], writes=[s_zs])
                                    dst = Zcs[g, tok0 + t * 512: tok0 + (t + 1) * 512, :].rearrange("(s p) n -> p s n", p=128)
                                    sl = Slot()
                                    scr_slots.append(sl)
                                    S.dma("sp", dst, zs[:], zq, reads=[s_zs], writes=[sl])
                            continue
                        for cc in range(4):
                            stg, s_stg = st4.next()
                            sq_ = st4dq[(st4.i - 1) % 3]
                            for t in range(4):
                                pm, s_pm = pmain.next()
                                for kc in range(16):
                                    S.op("pe", lambda t_, kc=kc: t_.matmul(pm[:], w[:, kc, cc * 128:(cc + 1) * 128], xn[:, kc, t * 512:(t + 1) * 512], start=(kc == 0), stop=(kc == 15)),
                                         reads=[s_w] + xn_slots(t * 512, 512), writes=[s_pm], inc=(kc == 15))
                                if kind in ("gf", "ga"):
                                    S.op("act", lambda a: a.activation(out=stg[:, t, :], in_=pm[:], func=AF.Sigmoid), reads=[s_pm], writes=[s_stg])
                                else:
                                    r, s_r = raw.next()
                                    q2, s_q2 = sq2.next()
                                    d2, s_d2 = sd2.next()
                                    S.op("dve", lambda v: v.tensor_copy(r[:], pm[:]), reads=[s_pm], writes=[s_r])
                                    S.op("act", lambda a: a.activation(out=q2[:], in_=r[:], func=AF.Square), reads=[s_r], writes=[s_q2])
                                    pa, s_pa = (pmain if _os.environ.get("QKMAIN") else paux).next()
                                    S.op("pe", lambda t_: t_.matmul(pa[:], ones_b[:], q2[:], start=True, stop=True), reads=[s_ones, s_q2], writes=[s_pa])
                                    S.op("act", lambda a: a.activation(out=d2[:], in_=pa[:], func=AF.Sqrt, bias=EPS, scale=1.0 / 128), reads=[s_pa], writes=[s_d2])
                                    S.op("dve", lambda v: v.reciprocal(d2[:], d2[:]), reads=[s_d2], writes=[s_d2])
                                    gcol = gqk[:, 0:1] if kind == "q" else gqk[:, 1:2]
                                    S.op("dve", lambda v: v.scalar_tensor_tensor(stg[:, t, :], r[:], gcol, d2[:], ALU.mult, ALU.mult),
                                         reads=[s_r, s_d2, s_gqk], writes=[s_stg])
                            ch = bi * 4 + cc
                            if kind == "q":
                                dst = QT[ch, :, :].rearrange("p (t n) -> p t n", t=4)
                            elif kind == "k":
                                dst = KT[ch, :, tok0:tok0 + 2048].rearrange("p (t n) -> p t n", t=4)
                            else:
                                dst = G[0 if kind == "gf" else 1, ch * 128:(ch + 1) * 128, :].rearrange("p (t n) -> p t n", t=4)
                            sl = Slot()
                            scr_slots.append(sl)
                            S.dma("sp", dst, stg[:], sq_, reads=[s_stg], writes=[sl])
                for e in ("sp", "pool"):
                    S.wait_all(e, scr_slots)
                barrier()

        def barrier():
            for e in ("pe", "act", "dve", "pool", "sp"):
                for o in ("pe", "act", "dve", "pool"):
                    if S.cnt[o] > 0:
                        S._wait(e, (o, S.cnt[o]))

        def phaseC():
            esC = ExitStack()
            with esC:
                sa = lambda n, sh, dt: esC.enter_context(nc.sbuf_tensor(n, sh, dt))
                TC = sa("TC", [128, 64, 512], BF16)
                TS = sa("TS", [128, 64, 512], BF16)
                s_TC, s_TS = Slot(), Slot()
                tq = [S.dma_sem(), S.dma_sem()]
                zp = Ring([(sa("zp%d" % i, [128, 8, 512], BF16), Slot()) for i in range(3)])
                zq = [S.dma_sem() for _ in range(3)]
                yst = Ring([(sa("yst%d" % i, [128, 512], BF16), Slot()) for i in range(2)])
                yq = [S.dma_sem() for _ in range(2)]
                tabC_v = tabC.rearrange("(c p) k -> p c k", p=128)
                tabS_v = tabS.rearrange("(c p) k -> p c k", p=128)
                scr_slots = []
                for kb in range(4):
                    S.dma("sp", TC[:], tabC_v[:, :, kb * 512:(kb + 1) * 512], tq[0], writes=[s_TC])
                    S.dma("sp", TS[:], tabS_v[:, :, kb * 512:(kb + 1) * 512], tq[1], writes=[s_TS])
                    for g in range(4):
                        acc = [pmain.next(), pmain.next()]
                        for pi in range(8):
                            z, s_z = zp.next()
                            q_ = zq[(zp.i - 1) % 3]
                            S.dma("sp", z[:], Zcs[g, pi * 1024:(pi + 1) * 1024, :].rearrange("(c p) n -> p c n", p=128), q_, writes=[s_z])
                            for c in range(8):
                                sc = pi * 8 + c
                                for half in range(2):
                                    pa_, s_pa_ = acc[half]
                                    S.op("pe", lambda t_: t_.matmul(pa_[:], z[:, c, half * 128:(half + 1) * 128], TC[:, sc, :], start=(sc == 0), stop=False),
                                         reads=[s_z, s_TC], writes=[s_pa_], inc=False)
                                    S.op("pe", lambda t_: t_.matmul(pa_[:], z[:, c, 256 + half * 128:256 + (half + 1) * 128], TS[:, sc, :], start=False, stop=(sc == 63)),
                                         reads=[s_z, s_TS], writes=[s_pa_], inc=(c == 7))
                        for half in range(2):
                            pa_, s_pa_ = acc[half]
                            ys, s_ys = yst.next()
                            q_ = yq[(yst.i - 1) % 2]
                            S.op("dve", lambda v: v.tensor_copy(ys[:], pa_[:]), reads=[s_pa_], writes=[s_ys])
                            sl = Slot()
                            scr_slots.append(sl)
                            ch = g * 2 + half
                            S.dma("sp", YfT[ch * 128:(ch + 1) * 128, kb * 512:(kb + 1) * 512], ys[:], q_, reads=[s_ys], writes=[sl])
                for e in ("sp", "pool"):
                    S.wait_all(e, scr_slots)
                barrier()

        def phaseB():
            esB = ExitStack()
            with esB:
                sa = lambda n, sh, dt: esB.enter_context(nc.sbuf_tensor(n, sh, dt))
                mlin = sa("mlin_sb", [128, 512], F32)
                mdiag = sa("mdiag_sb", [128, 896], F32)
                at = sa("atab_sb", [128, 4096], F32)
                s_cst = Slot()
                cq = S.dma_sem()
                S.dma("sp", mlin[:], mlin_d[:, :], cq, writes=[s_cst])
                S.dma("sp", mdiag[:], mdiag_d[:, :], cq, writes=[s_cst])
                S.dma("sp", at[:], atab_d[:, :], cq, writes=[s_cst])
                ones_f = sa("ones_f", [128, 128], F32)
                s_of = Slot()
                S.op("dve", lambda v: v.memset(ones_f[:], 1.0), writes=[s_of])
                lam = sa("lam", [128, 8], F32)
                s_lam = Slot()
                S.op("dve", lambda v: v.tensor_tensor(lam[:, 0:1], vec[:, 36:37], vec[:, 37:38], ALU.mult), reads=[s_vec], writes=[s_lam])
                S.op("dve", lambda v: v.tensor_tensor(lam[:, 1:2], vec[:, 38:39], vec[:, 39:40], ALU.mult), reads=[s_vec], writes=[s_lam])
                pmx, s_pmx = PS[4]
                S.op("pe", lambda t_: t_.matmul(pmx[:, 0:2], ones_f[:], lam[:, 0:2], start=True, stop=True), reads=[s_of, s_lam], writes=[s_pmx])
                S.op("act", lambda a: a.activation(out=lam[:, 2:4], in_=pmx[:, 0:2], func=AF.Exp), reads=[s_pmx], writes=[s_lam])
                S.op("dve", lambda v: v.tensor_tensor(lam[:, 4:5], lam[:, 2:3], lam[:, 3:4], ALU.subtract), reads=[s_lam], writes=[s_lam])
                S.op("dve", lambda v: v.tensor_scalar(lam[:, 5:6], lam[:, 4:5], LAM_INIT, -1.0, ALU.add, ALU.mult), reads=[s_lam], writes=[s_lam])
                nlam = lam[:, 5:6]
                gsub = sa("gsub", [128, 2], F32)
                s_gsub = Slot()
                S.op("dve", lambda v: v.tensor_scalar(gsub[:], vec[:, 34:36], 1.0 - LAM_INIT, None, ALU.mult), reads=[s_vec], writes=[s_gsub])

                kt = Ring([(sa("kt%d" % i, [128, 2, S_LEN], BF16), Slot()) for i in range(2)])
                vt = Ring([(sa("vt%d" % i, [128, 64, 256], BF16), Slot()) for i in range(2)])
                qt = Ring([(sa("qt%d" % i, [128, 2, OWN], BF16), Slot()) for i in range(2)])
                hq = [S.dma_sem() for _ in range(2)]
                baseL = sa("baseL", [128, 512], F32)
                baseD = sa("baseD", [128, 896], F32)
                s_base = Slot()
                tt = Ring([(sa("tt%d" % i, [128, 512], F32), Slot()) for i in range(4)])
                pp = Ring([(sa("pp%d" % i, [128, 512], BF16), Slot()) for i in range(4)])
                ev = [sa("ev%d" % i, [128, 512], F32) for i in range(3)]
                s_ev = Slot()
                acc = sa("acc", [128, 2, 512], F32)
                s_acc = Slot()
                sqo = sa("sqo", [128, 2, 512], BF16)
                s_sqo = Slot()
                sdo = sa("sdo", [128, 512], F32)
                s_sdo = Slot()
                ost = Ring([(sa("ost%d" % i, [128, 2, 512], BF16), Slot()) for i in range(2)])
                oq = [S.dma_sem() for _ in range(2)]
                scr_slots = []
                ps_s = Ring(PS[0:4])
                (pO0, s_pO0), (pO1, s_pO1), (pZ, s_pZ) = PS[5], PS[6], PS[7]
                LAG = 3

                def needed_chunks(h, qb):
                    dmin = (2 * 16.0 + 22.0) / SLOPES[h]
                    out = []
                    for kc in range(64):
                        best = 1 << 30
                        for j in range(4):
                            k0 = (kc * 128 + OWN * j) % S_LEN
                            q0 = OWN * j + qb * 512
                            if k0 >= q0 + 512:
                                d = k0 - (q0 + 511)
                            elif k0 + 128 <= q0:
                                d = q0 - (k0 + 127)
                            else:
                                d = 0
                            best = min(best, d)
                        if best < dmin:
                            out.append(kc)
                    return out

                hbuf = {}

                def load_head(h):
                    k_, s_k = kt.next()
                    v_, s_v = vt.next()
                    q_, s_q = qt.next()
                    dq = hq[h % 2]
                    S.dma("sp", k_[:], KT[2 * h:2 * h + 2, :, :].rearrange("m p t -> p m t"), dq, writes=[s_k])
                    S.dma("sp", v_[:], Vs[h, :, :].rearrange("(c p) d -> p c d", p=128), dq, writes=[s_v])
                    S.dma("sp", q_[:], QT[2 * h:2 * h + 2, :, :].rearrange("m p t -> p m t"), dq, writes=[s_q])
                    hbuf[h] = (k_, s_k, v_, s_v, q_, s_q)

                load_head(0)
                for h in range(NH):
                    if h + 1 < NH:
                        load_head(h + 1)
                    k_, s_k, v_, s_v, q_, s_q = hbuf.pop(h)
                    S.op("pool", lambda g_: g_.tensor_scalar(baseL[:], mlin[:], -SLOPES[h], None, ALU.mult), reads=[s_cst], writes=[s_base])
                    S.op("pool", lambda g_: g_.tensor_scalar(baseD[:], mdiag[:], -SLOPES[h], None, ALU.mult), reads=[s_cst], writes=[s_base])
                    for qb in range(4):
                        for m in range(2):
                            tiles = {}

                            def issue_s(kc):
                                ps_, s_ps = ps_s.next()
                                S.op("pe", lambda t_: t_.matmul(ps_[:], k_[:, m, kc * 128:(kc + 1) * 128], q_[:, m, qb * 512:(qb + 1) * 512], start=True, stop=True),
                                     reads=[s_k, s_q], writes=[s_ps])
                                t, s_t = tt.next()
                                col = ((h * 4 + qb) * 64 + kc) * 2
                                diag = (qb * 4 <= kc < qb * 4 + 4)
                                if diag:
                                    delta = (kc - qb * 4) * 128
                                    bs = baseD[:, 384 - delta:384 - delta + 512]
                                    S.op("dve", lambda v: v.tensor_tensor(t[:], ps_[:], bs, ALU.add), reads=[s_ps, s_base], writes=[s_t])
                                else:
                                    S.op("dve", lambda v: v.scalar_tensor_tensor(t[:], baseL[:], at[:, col:col + 1], ps_[:], ALU.mult, ALU.add),
                                         reads=[s_ps, s_base, s_cst], writes=[s_t])
                                p, s_p = pp.next()
                                if diag:
                                    S.op("act", lambda a: a.activation(out=p[:], in_=t[:], func=AF.Exp), reads=[s_t], writes=[s_p])
                                else:
                                    S.op("act", lambda a: a.activation(out=p[:], in_=t[:], func=AF.Exp, bias=at[:, col + 1:col + 2]), reads=[s_t, s_cst], writes=[s_p])
                                tiles[kc] = (p, s_p)

                            def issue_pv(kc, st_, sp_):
                                p, s_p = tiles.pop(kc)
                                S.op("pe", lambda t_: t_.matmul(pO0[:], v_[:, kc, 0:128], p[:], start=st_, stop=sp_), reads=[s_v, s_p], writes=[s_pO0], inc=False)
                                S.op("pe", lambda t_: t_.matmul(pO1[:], v_[:, kc, 128:256], p[:], start=st_, stop=sp_), reads=[s_v, s_p], writes=[s_pO1], inc=False)
                                S.op("pe", lambda t_: t_.matmul(pZ[:], ones_b[:], p[:], start=st_, stop=sp_), reads=[s_ones, s_p], writes=[s_pZ], inc=True)

                            chunks = needed_chunks(h, qb)
                            nch = len(chunks)
                            for ix in range(nch + LAG):
                                if ix < nch:
                                    issue_s(chunks[ix])
                                if ix >= LAG:
                                    issue_pv(chunks[ix - LAG], ix - LAG == 0, ix - LAG == nch - 1)
                            S.op("dve", lambda v: v.reciprocal(ev[2][:], pZ[:]), reads=[s_pZ], writes=[s_ev])
                            if m == 0:
                                S.op("dve", lambda v: v.tensor_tensor(acc[:, 0, :], pO0[:], ev[2][:], ALU.mult), reads=[s_pO0, s_ev], writes=[s_acc])
                                S.op("dve", lambda v: v.tensor_tensor(acc[:, 1, :], pO1[:], ev[2][:], ALU.mult), reads=[s_pO1, s_ev], writes=[s_acc])
                            else:
                                S.op("dve", lambda v: v.tensor_tensor(ev[0][:], pO0[:], ev[2][:], ALU.mult), reads=[s_pO0, s_ev], writes=[s_ev])
                                S.op("dve", lambda v: v.tensor_tensor(ev[1][:], pO1[:], ev[2][:], ALU.mult), reads=[s_pO1, s_ev], writes=[s_ev])
                                for dv in range(2):
                                    S.op("dve", lambda v, dv=dv: v.scalar_tensor_tensor(acc[:, dv, :], ev[dv][:], nlam, acc[:, dv, :], ALU.mult, ALU.add),
                                         reads=[s_ev, s_lam, s_acc], writes=[s_acc])
                        S.op("act", lambda a: a.activation(out=sqo[:], in_=acc[:], func=AF.Square), reads=[s_acc], writes=[s_sqo])
                        for dv in range(2):
                            S.op("pe", lambda t_, dv=dv: t_.matmul(pmx[:], ones_b[:], sqo[:, dv, :], start=(dv == 0), stop=(dv == 1)), reads=[s_ones, s_sqo], writes=[s_pmx], inc=(dv == 1))
                        S.op("act", lambda a: a.activation(out=sdo[:], in_=pmx[:], func=AF.Sqrt, bias=EPS, scale=1.0 / 256), reads=[s_pmx], writes=[s_sdo])
                        S.op("dve", lambda v: v.reciprocal(sdo[:], sdo[:]), reads=[s_sdo], writes=[s_sdo])
                        os_, s_os = ost.next()
                        oq_ = oq[(ost.i - 1) % 2]
                        for dv in range(2):
                            S.op("dve", lambda v, dv=dv: v.scalar_tensor_tensor(os_[:, dv, :], acc[:, dv, :], gsub[:, dv:dv + 1], sdo[:], ALU.mult, ALU.mult),
                                 reads=[s_acc, s_gsub, s_sdo], writes=[s_os])
                        sl = Slot()
                        scr_slots.append(sl)
                        S.dma("sp", oT[h * 256:(h + 1) * 256, qb * 512:(qb + 1) * 512].rearrange("(c p) t -> p c t", p=128), os_[:], oq_, reads=[s_os], writes=[sl])
                for e in ("sp", "pool"):
                    S.wait_all(e, scr_slots)
                barrier()

        def phaseD():
            esD = ExitStack()
            with esD:
                sa = lambda n, sh, dt: esD.enter_context(nc.sbuf_tensor(n, sh, dt))
                yf = sa("yf", [128, 8, 512], BF16)
                ot = sa("ot", [128, 16, 512], BF16)
                gf = sa("gf", [128, 16, 512], BF16)
                ga = sa("ga", [128, 16, 512], BF16)
                xt = sa("xt", [128, 16, 512], F32)
                mixed = sa("mixed", [128, 16, 512], BF16)
                s_in, s_xt, s_mixed = Slot(), Slot(), Slot()
                inq = S.dma_sem()
                xq = S.dma_sem()
                hq_ = S.dma_sem()
                wf = Ring([(sa("wf%d" % i, [128, 8, 512], BF16), Slot()) for i in range(2)])
                wa = Ring([(sa("wa%d" % i, [128, 16, 512], BF16), Slot()) for i in range(2)])
                wo = Ring([(sa("wo%d" % i, [128, 16, 512], BF16), Slot()) for i in range(2)])
                wfq = [S.dma_sem() for _ in range(2)]
                waq = [S.dma_sem() for _ in range(2)]
                woq = [S.dma_sem() for _ in range(2)]
                t1 = Ring([(sa("t1_%d" % i, [128, 512], F32), Slot()) for i in range(2)])
                t2 = Ring([(sa("t2_%d" % i, [128, 512], F32), Slot()) for i in range(2)])
                YfT_v = YfT.rearrange("(c p) k -> p c k", p=128)
                oT_v = oT.rearrange("(c p) k -> p c k", p=128)
                Gf_v = G[0].rearrange("(c p) k -> p c k", p=128)
                Ga_v = G[1].rearrange("(c p) k -> p c k", p=128)
                xT_v = xT.rearrange("(c p) t -> p c t", p=128)
                hT_v = hTs.rearrange("(c p) t -> p c t", p=128)
                w_f_v = w_f.rearrange("(c p) n -> p c n", p=128)
                w_a_v = w_a.rearrange("(c p) n -> p c n", p=128)
                w_o_v = w_o.rearrange("(c p) n -> p c n", p=128)
                scr_slots = []
                for t in range(4):
                    ts_ = slice(t * 512, (t + 1) * 512)
                    S.dma("sp", yf[:], YfT_v[:, :, ts_], inq, writes=[s_in])
                    S.dma("sp", ot[:], oT_v[:, :, ts_], inq, writes=[s_in])
                    S.dma("sp", gf[:], Gf_v[:, :, ts_], inq, writes=[s_in])
                    S.dma("sp", ga[:], Ga_v[:, :, ts_], inq, writes=[s_in])
                    S.dma("sp", xt[:], xT_v[:, :, ts_], xq, writes=[s_xt])
                    for ob in range(4):
                        wf_, s_wf = wf.next()
                        wa_, s_wa = wa.next()
                        S.dma("pool", wf_[:], w_f_v[:, :, ob * 512:(ob + 1) * 512], wfq[(wf.i - 1) % 2], writes=[s_wf])
                        S.dma("pool", wa_[:], w_a_v[:, :, ob * 512:(ob + 1) * 512], waq[(wa.i - 1) % 2], writes=[s_wa])
                        for cc in range(4):
                            oc = ob * 4 + cc
                            pf, s_pf = pmain.next()
                            for kc in range(8):
                                S.op("pe", lambda t_, kc=kc: t_.matmul(pf[:], wf_[:, kc, cc * 128:(cc + 1) * 128], yf[:, kc, :], start=(kc == 0), stop=(kc == 7)),
                                     reads=[s_wf, s_in], writes=[s_pf], inc=(kc == 7))
                            pa_, s_pa_ = pmain.next()
                            for kc in range(16):
                                S.op("pe", lambda t_, kc=kc: t_.matmul(pa_[:], wa_[:, kc, cc * 128:(cc + 1) * 128], ot[:, kc, :], start=(kc == 0), stop=(kc == 15)),
                                     reads=[s_wa, s_in], writes=[s_pa_], inc=(kc == 15))
                            a1, s_a1 = t1.next()
                            a2, s_a2 = t2.next()
                            S.op("dve", lambda v: v.tensor_tensor(a1[:], pf[:], gf[:, oc, :], ALU.mult), reads=[s_pf, s_in], writes=[s_a1])
                            S.op("dve", lambda v: v.tensor_tensor(a2[:], pa_[:], ga[:, oc, :], ALU.mult), reads=[s_pa_, s_in], writes=[s_a2])
                            S.op("pool", lambda g_: g_.tensor_tensor(mixed[:, oc, :], a1[:], a2[:], ALU.add), reads=[s_a1, s_a2], writes=[s_mixed])
                    for ob in range(4):
                        wo_, s_wo = wo.next()
                        S.dma("pool", wo_[:], w_o_v[:, :, ob * 512:(ob + 1) * 512], woq[(wo.i - 1) % 2], writes=[s_wo])
                        for cc in range(4):
                            oc = ob * 4 + cc
                            ph, s_ph = pmain.next()
                            for kc in range(16):
                                S.op("pe", lambda t_, kc=kc: t_.matmul(ph[:], wo_[:, kc, cc * 128:(cc + 1) * 128], mixed[:, kc, :], start=(kc == 0), stop=(kc == 15)),
                                     reads=[s_wo, s_mixed], writes=[s_ph], inc=(kc == 15))
                            S.op("dve", lambda v: v.tensor_tensor(xt[:, oc, :], ph[:], xt[:, oc, :], ALU.add), reads=[s_ph, s_xt], writes=[s_xt])
                    sl = Slot()
                    scr_slots.append(sl)
                    S.dma("sp", hT_v[:, :, ts_], xt[:], hq_, reads=[s_xt], writes=[sl])
                for e in ("sp", "pool"):
                    S.wait_all(e, scr_slots)
                barrier()

        def phaseE():
            esE = ExitStack()
            with esE:
                sa = lambda n, sh, dt: esE.enter_context(nc.sbuf_tensor(n, sh, dt))
                ident = sa("ident", [128, 128], BF16)
                s_id = Slot()
                S.op("pool", lambda g_: g_.memset(ident[:], 0.0), writes=[s_id])
                S.op("pool", lambda g_: g_.affine_select(out=ident[:], in_=ident[:], pattern=[[-1, 128]], compare_op=ALU.not_equal, fill=1.0, base=0, channel_multiplier=1), reads=[s_id], writes=[s_id])
                skb = sa("skb", [128, 16, 128], BF16)
                s_skb = Slot()
                kq_ = S.dma_sem()
                S.dma("pool", skb[:], skT[:, :, :], kq_, writes=[s_skb])
                acc = sa("pacc", [128, 16, 512], F32)
                s_acc = Slot()
                hn = sa("hn", [128, 16, 512], BF16)
                s_hn = Slot()
                ssb = sa("ssb", [128, 4, 16, 128], F32)
                s_ssb = Slot()
                thr = sa("thr", [128, 4, 8], F32)
                s_thr = Slot()
                s1b = sa("s1b", [128, 4, 8, 128], F32)
                s_e12 = Slot()
                e1s = sa("e1s", [128, 4, 2, 128], BF16)
                e2s = sa("e2s", [128, 4, 2, 128], BF16)
                s_e67 = Slot()
                accq = S.dma_sem()
                outq = S.dma_sem()
                ub = Ring([(sa("ub%d" % i, [128, 16, 512], BF16), Slot()) for i in range(2)])
                ubq = [S.dma_sem() for _ in range(2)]
                hT_v = hTs.rearrange("(c p) t -> p c t", p=128)
                outT_v = outT.rearrange("(c p) t -> p c t", p=128)
                w_q_v = w_q.rearrange("(c p) n -> p c n", p=128)
                e_uT_v = e_uT.rearrange("(c p) e -> p c e", p=128)
                g2 = vec[:, 16:32]
                out_slots = []
                pq = Ring(PS[0:3])
                pT = [PS[3], PS[4]]
                pv = Ring(PS[5:8])
                for t in range(4):
                    ts_ = slice(t * 512, (t + 1) * 512)
                    S.dma("sp", acc[:], hT_v[:, :, ts_], accq, writes=[s_acc])
                    es1 = ExitStack()
                    with es1:
                        s1a = lambda n, sh, dt: es1.enter_context(nc.sbuf_tensor(n + '_%d' % t, sh, dt))
                        sq = s1a("esq", [128, 16, 512], BF16)
                        s_sq = Slot()
                        rs = s1a("ers", [128, 512], F32)
                        s_rs = Slot()
                        qT_ = s1a("eqT", [128, 16, 512], BF16)
                        s_qT = Slot()
                        m16 = s1a("m16", [128, 16, 16], F32)
                        s_m16 = Slot()
                        tmp = s1a("etmp", [128, 256], F32)
                        s_tmp = Slot()
                        cand = s1a("cand", [128, 8, 16, 16], F32)
                        s_cand = Slot()
                        t16 = s1a("t16", [128, 8, 16], F32)
                        s_t16 = Slot()
                        sm = s1a("sm", [128, 8, 8], F32)
                        s_sm = Slot()
                        S.op("act", lambda a: a.activation(out=sq[:], in_=acc[:], func=AF.Square), reads=[s_acc], writes=[s_sq])
                        pa_, s_pa_ = pq.next()
                        for c in range(16):
                            S.op("pe", lambda t_, c=c: t_.matmul(pa_[:], ones_b[:], sq[:, c, :], start=(c == 0), stop=(c == 15)), reads=[s_ones, s_sq], writes=[s_pa_], inc=(c == 15))
                        S.op("act", lambda a: a.activation(out=rs[:], in_=pa_[:], func=AF.Sqrt, bias=EPS, scale=1.0 / D), reads=[s_pa_], writes=[s_rs])
                        S.op("dve", lambda v: v.reciprocal(rs[:], rs[:]), reads=[s_rs], writes=[s_rs])
                        for c in range(16):
                            S.op("dve", lambda v, c=c: v.scalar_tensor_tensor(hn[:, c, :], acc[:, c, :], g2[:, c:c + 1], rs[:], ALU.mult, ALU.mult),
                                 reads=[s_acc, s_rs, s_vec], writes=[s_hn])
                        for ob in range(4):
                            u_, s_u = ub.next()
                            S.dma("pool", u_[:], w_q_v[:, :, ob * 512:(ob + 1) * 512], ubq[(ub.i - 1) % 2], writes=[s_u])
                            for cc in range(4):
                                pm, s_pm = pq.next()
                                for kc in range(16):
                                    S.op("pe", lambda t_, kc=kc: t_.matmul(pm[:], u_[:, kc, cc * 128:(cc + 1) * 128], hn[:, kc, :], start=(kc == 0), stop=(kc == 15)),
                                         reads=[s_u, s_hn], writes=[s_pm], inc=(kc == 15))
                                S.op("dve", lambda v: v.tensor_copy(qT_[:, ob * 4 + cc, :], pm[:]), reads=[s_pm], writes=[s_qT])
                        for sub in range(4):
                            for q4 in range(4):
                                pm, s_pm = pq.next()
                                for i4 in range(4):
                                    hc = q4 * 4 + i4
                                    S.op("pe", lambda t_, hc=hc, i4=i4: t_.matmul(pm[:, i4 * 128:(i4 + 1) * 128], qT_[:, hc, sub * 128:(sub + 1) * 128], skb[:, hc, :], start=True, stop=True),
                                         reads=[s_qT, s_skb], writes=[s_pm], inc=(i4 == 3))
                                S.op("dve", lambda v: v.tensor_copy(ssb[:, sub, q4 * 4:(q4 + 1) * 4, :], pm[:].rearrange("p (a n) -> p a n", a=4)), reads=[s_pm], writes=[s_ssb])
                            for hc in range(16):
                                S.op("dve", lambda v, hc=hc: v.max(out=m16[:, hc, 0:8], in_=ssb[:, sub, hc, :]), reads=[s_ssb], writes=[s_m16])
                                S.op("dve", lambda v, hc=hc: v.match_replace(out=tmp[:, 0:128], in_to_replace=m16[:, hc, 0:8], in_values=ssb[:, sub, hc, :], imm_value=-1e30), reads=[s_ssb, s_m16], writes=[s_tmp])
                                S.op("dve", lambda v, hc=hc: v.max(out=m16[:, hc, 8:16], in_=tmp[:, 0:128]), reads=[s_tmp], writes=[s_m16])
                            m16v = m16[:].rearrange("p (h c) k -> p h c k", c=2)
                            S.op("dve", lambda v: v.tensor_tensor(cand[:], m16v[:, :, 0, :].unsqueeze(3).to_broadcast([128, 8, 16, 16]),
                                                                  m16v[:, :, 1, :].unsqueeze(2).to_broadcast([128, 8, 16, 16]), ALU.add), reads=[s_m16], writes=[s_cand])
                            for h in range(8):
                                cv = cand[:, h].rearrange("p a b -> p (a b)")
                                S.op("dve", lambda v, h=h, cv=cv: v.max(out=t16[:, h, 0:8], in_=cv), reads=[s_cand], writes=[s_t16])
                                S.op("dve", lambda v, h=h, cv=cv: v.match_replace(out=tmp[:], in_to_replace=t16[:, h, 0:8], in_values=cv, imm_value=-1e30), reads=[s_cand, s_t16], writes=[s_tmp])
                                S.op("dve", lambda v, h=h: v.max(out=t16[:, h, 8:16], in_=tmp[:]), reads=[s_tmp], writes=[s_t16])
                            S.op("dve", lambda v: v.tensor_tensor(cand[:, 0:8, 0, :], t16[:], t16[:, :, 0:1].to_broadcast([128, 8, 16]), ALU.subtract), reads=[s_t16], writes=[s_cand])
                            S.op("act", lambda a: a.activation(out=cand[:, 0:8, 1, :], in_=cand[:, 0:8, 0, :], func=AF.Exp), reads=[s_cand], writes=[s_cand])
                            S.op("dve", lambda v: v.reduce_sum(sm[:, :, 0], cand[:, 0:8, 1, :], axis=AX.X), reads=[s_cand], writes=[s_sm])
                            S.op("act", lambda a: a.activation(out=sm[:, :, 1], in_=sm[:, :, 0], func=AF.Ln), reads=[s_sm], writes=[s_sm])
                            S.op("dve", lambda v: v.tensor_tensor(sm[:, :, 2], t16[:, :, 0], sm[:, :, 1], ALU.add), reads=[s_sm, s_t16], writes=[s_sm])
                            S.op("dve", lambda v: v.tensor_scalar(sm[:, :, 2], sm[:, :, 2], -1.0, None, ALU.mult), reads=[s_sm], writes=[s_sm])
                            S.op("dve", lambda v: v.scalar_tensor_tensor(thr[:, sub, :], t16[:, :, 15], -1e-4, sm[:, :, 2], ALU.add, ALU.add), reads=[s_sm, s_t16], writes=[s_thr])
                            s1v = ssb[:, sub].rearrange("p (h c) n -> p h c n", c=2)[:, :, 0, :]
                            S.op("dve", lambda v: v.tensor_tensor(s1v, s1v, sm[:, :, 2:3].to_broadcast([128, 8, 128]), ALU.add), reads=[s_ssb, s_sm], writes=[s_ssb])
                            s2v = ssb[:, sub].rearrange("p (h c) n -> p h c n", c=2)[:, :, 1, :]
                            S.op("act", lambda a: a.copy(out=s1b[:, sub], in_=s1v), reads=[s_ssb], writes=[s_e12])
                            S.op("act", lambda a: a.activation(out=e1s[:, sub], in_=s1v[:, 6:8, :], func=AF.Exp), reads=[s_ssb], writes=[s_e67])
                            S.op("act", lambda a: a.activation(out=e2s[:, sub], in_=s2v[:, 6:8, :], func=AF.Exp), reads=[s_ssb], writes=[s_e67])
                            S.op("dve", lambda v: v.tensor_tensor(s1v, thr[:, sub, :].unsqueeze(2).to_broadcast([128, 8, 128]), s1v, ALU.subtract), reads=[s_ssb, s_thr, s_e12], writes=[s_ssb])
                        barrier()
                    es2 = ExitStack()
                    with es2:
                        s2a = lambda n, sh, dt: es2.enter_context(nc.sbuf_tensor(n + '_%d' % t, sh, dt))
                        vbr = Ring([(s2a("vb%d" % i, [128, 4, 2048], BF16), Slot()) for i in range(2)])
                        vbq = [S.dma_sem() for _ in range(2)]
                        mkr = Ring([(s2a("mk%d" % i, [128, 2048], BF16), Slot()) for i in range(2)])
                        ewr = Ring([(s2a("ew%d" % i, [128, 2048], BF16), Slot()) for i in range(2)])
                        whr = Ring([(s2a("wh%d" % i, [128, 2, 512], BF16), Slot()) for i in range(2)])
                        Wb = s2a("Wb", [128, 4, 512], BF16)
                        s_Wb = [Slot() for _ in range(4)]
                        glr = Ring([(s2a("gl%d" % i, [128, 4, 512], BF16), Slot()) for i in range(2)])
                        AT = s2a("AT", [128, 4, 512], BF16)
                        s_AT = Slot()
                        stt_ = {}

                        def S1_load(eb):
                            u_, s_u = ub.next()
                            S.dma("pool", u_[:], e_uT_v[:, :, eb * 512:(eb + 1) * 512], ubq[(ub.i - 1) % 2], writes=[s_u])
                            vb, s_vb = vbr.next()
                            S.dma("pool", vb[:], e_v[eb * 512:(eb + 1) * 512, :].rearrange("(c p) d -> p c d", p=128), vbq[(vbr.i - 1) % 2], writes=[s_vb])
                            gl, s_gl = glr.next()
                            stt_[eb] = (vb, s_vb, gl, s_gl, u_, s_u)

                        def S1_score(eb, ecs):
                            vb, s_vb, gl, s_gl, u_, s_u = stt_[eb]
                            for ec in ecs:
                                pm, s_pm = pq.next()
                                for kc in range(16):
                                    S.op("pe", lambda t_, kc=kc: t_.matmul(pm[:], u_[:, kc, ec * 128:(ec + 1) * 128], hn[:, kc, :], start=(kc == 0), stop=(kc == 15)),
                                         reads=[s_u, s_hn], writes=[s_pm], inc=(kc == 15))
                                S.op("act", lambda a: a.activation(out=gl[:, ec, :], in_=pm[:], func=AF.Gelu), reads=[s_pm], writes=[s_gl])

                        cur_wh = {}

                        bst = {}

                        def step_of(g):
                            return g // 8, (g % 8) // 2, g % 2

                        def Sa(g):
                            eb, sub, hh = step_of(g)
                            mk, s_mk = mkr.next()
                            ew, s_ew = ewr.next()
                            bst[g] = (mk, s_mk, ew, s_ew)
                            sv = ssb[:, sub].rearrange("p (h c) n -> p h c n", c=2)
                            hs = slice(4 * hh, 4 * hh + 4)
                            isl = slice(4 * eb, 4 * eb + 4)
                            mk4 = mk[:].rearrange("p (h i j) -> p h i j", h=4, i=4)
                            ew4 = ew[:].rearrange("p (h i j) -> p h i j", h=4, i=4)
                            B4 = [128, 4, 4, 128]
                            for h_ in range(4):
                                for i_ in range(4):
                                    hh_, ii_ = 4 * hh + h_, 4 * eb + i_
                                    edge = (h_ == 0 and i_ == 0) or (h_ == 3 and i_ == 3)
                                    S.op("act", lambda a, h_=h_, i_=i_, hh_=hh_, ii_=ii_: a.activation(out=ew4[:, h_, i_, :], in_=sv[:, hh_, 1, :], func=AF.Exp, bias=s1b[:, sub, hh_, ii_:ii_ + 1]),
                                         reads=[s_ssb, s_e12] if edge else [], writes=[s_ew] if edge else [])
                            S.op("dve", lambda v: v.tensor_tensor(mk4, sv[:, hs, 1, :].unsqueeze(2).to_broadcast(B4), sv[:, hs, 0, isl].unsqueeze(3).to_broadcast(B4), ALU.is_ge),
                                 reads=[s_ssb], writes=[s_mk])

                        def Sc_mult(g):
                            eb, sub, hh = step_of(g)
                            mk, s_mk, ew, s_ew = bst[g]
                            if hh == 0:
                                cur_wh[sub] = whr.next()
                            if True:
                                S.op("dve", lambda g_: g_.tensor_tensor(mk[:], mk[:], ew[:], ALU.mult), reads=[s_mk, s_ew], writes=[s_mk])
                            else:
                                isl = slice(4 * eb, 4 * eb + 4)
                                mk4 = mk[:].rearrange("p (h i j) -> p h i j", h=4, i=4)
                                B2 = [128, 2, 4, 128]
                                S.op("dve", lambda g_: g_.tensor_tensor(mk[:, 0:1024], mk[:, 0:1024], ew[:, 0:1024], ALU.mult), reads=[s_mk, s_ew], writes=[s_mk])
                                S.op("dve", lambda g_: g_.tensor_tensor(mk4[:, 2:4], mk4[:, 2:4], e2s[:, sub].unsqueeze(2).to_broadcast(B2), ALU.mult), reads=[s_mk, s_e67], writes=[s_mk])
                                S.op("dve", lambda g_: g_.tensor_tensor(mk4[:, 2:4], mk4[:, 2:4], e1s[:, sub, :, isl].unsqueeze(3).to_broadcast(B2), ALU.mult), reads=[s_mk, s_e67], writes=[s_mk])

                        def Sc_add1(g):
                            mk, s_mk, ew, s_ew = bst[g]
                            mk2 = mk[:].rearrange("p (a e) -> p a e", a=2)
                            S.op("dve", lambda v: v.tensor_tensor(mk2[:, 0, :], mk2[:, 0, :], mk2[:, 1, :], ALU.add), reads=[s_mk], writes=[s_mk])

                        def Sc_add2(g):
                            eb, sub, hh = step_of(g)
                            mk, s_mk, ew, s_ew = bst.pop(g)
                            wh, s_wh = cur_wh[sub]
                            S.op("dve", lambda v: v.tensor_tensor(wh[:, hh, :], mk[:, 0:512], mk[:, 512:1024], ALU.add), reads=[s_mk], writes=[s_wh])
                            if hh == 1:
                                S2_fin(eb, sub)

                        def Sc(g):
                            Sc_mult(g)
                            Sc_add1(g)
                            Sc_add2(g)

                        GMAX = 32 * 8

                        def build_step(n):
                            if 0 <= n + 1 < GMAX:
                                Sa(n + 1)
                            if 0 <= n < GMAX:
                                Sc(n)

                        def S2_fin(eb, sub):
                            wh, s_wh = cur_wh[sub]
                            S.op("dve", lambda g_: g_.tensor_tensor(Wb[:, sub, :], wh[:, 0, :], wh[:, 1, :], ALU.add), reads=[s_wh], writes=[s_Wb[sub]])
                            for ec in range(4):
                                pt_, s_pt = pT[ec // 2]
                                dst = pt_[:].bitcast(BF16)[:, (ec % 2) * 512 + sub * 128:(ec % 2) * 512 + (sub + 1) * 128]
                                S.op("pe", lambda t_, dst=dst, ec=ec: t_.transpose(dst, Wb[:, sub, ec * 128:(ec + 1) * 128], ident[:]), reads=[s_Wb[sub], s_id], writes=[s_pt])

                        def S3_at(eb):
                            vb, s_vb, gl, s_gl, u_, s_u = stt_[eb]
                            for ec in range(4):
                                pt_, s_pt = pT[ec // 2]
                                src = pt_[:].bitcast(BF16)[:, (ec % 2) * 512:(ec % 2 + 1) * 512]
                                S.op("dve", lambda v, src=src, ec=ec: v.tensor_tensor(AT[:, ec, :], src, gl[:, ec, :], ALU.mult), reads=[s_pt, s_gl], writes=[s_AT])

                        def S3_v_pe(eb, dcs):
                            vb, s_vb, gl, s_gl, u_, s_u = stt_[eb]
                            outs = []
                            for dc in dcs:
                                po, s_po = pv.next()
                                for ec in range(4):
                                    S.op("pe", lambda t_, ec=ec: t_.matmul(po[:], vb[:, ec, dc * 128:(dc + 1) * 128], AT[:, ec, :], start=(ec == 0), stop=(ec == 3)),
                                         reads=[s_vb, s_AT], writes=[s_po], inc=(ec == 3))
                                outs.append((dc, po, s_po))
                            return outs

                        def S3_v_add(item):
                            dc, po, s_po = item
                            S.op("dve", lambda v: v.tensor_tensor(acc[:, dc, :], po[:], acc[:, dc, :], ALU.add), reads=[s_po, s_acc], writes=[s_acc])

                        NEB = 32
                        S1_load(0)
                        S1_score(0, range(4))
                        for n in range(-1, 8):
                            build_step(n)
                        for eb in range(NEB):
                            nxt = eb + 1 < NEB
                            if nxt:
                                S1_load(eb + 1)
                            S3_at(eb)
                            for k2 in range(8):
                                n = (eb + 1) * 8 + k2
                                if nxt and n + 1 < GMAX:
                                    Sa(n + 1)
                                items = S3_v_pe(eb, [2 * k2, 2 * k2 + 1])
                                if nxt:
                                    Sc_mult(n)
                                S3_v_add(items[0])
                                if nxt:
                                    Sc_add1(n)
                                S3_v_add(items[1])
                                if nxt:
                                    Sc_add2(n)
                                if nxt and k2 == 2:
                                    S1_score(eb + 1, [0, 1])
                                if nxt and k2 == 5:
                                    S1_score(eb + 1, [2, 3])
                            del stt_[eb]
                        sl = Slot()
                        out_slots.append(sl)
                        S.dma("sp", outT_v[:, :, ts_], acc[:], outq, reads=[s_acc], writes=[sl])
                        barrier()
                S.wait_all("sp", out_slots)
                barrier()

        if stage >= 1:
            phaseA()
        if stage >= 2:
            phaseC()
        if stage >= 3:
            phaseB()
        if stage >= 4:
            phaseD()
        if stage >= 5:
            phaseE()
        if stage < 5:
            fin = sbt("fin", [128, 16], F32)
            s_fin = Slot()
            S.op("dve", lambda v: v.memset(fin[:], 0.0), writes=[s_fin])
            dqo = S.dma_sem()
            so = Slot()
            S.dma("sp", outT[0:128, 0:16], fin[:], dqo, reads=[s_fin], writes=[so])
            S.wait_all("sp", [so])
    return nc


_CONST = {}


def _const_tables(j):
    key = ("t", j)
    if key in _CONST:
        return _CONST[key]
    r = np.arange(S_LEN)
    s_act = (r + OWN * j) % S_LEN
    k_act = (np.arange(OWN) + OWN * j)
    prod = (s_act[:, None].astype(np.int64) * k_act[None, :].astype(np.int64)) % S_LEN
    ang = prod.astype(np.float64) * (2.0 * np.pi / S_LEN)
    tabC = (np.cos(ang) / math.sqrt(S_LEN)).astype(np.float32).astype(ml_dtypes.bfloat16)
    tabS = (-np.sin(ang) / math.sqrt(S_LEN)).astype(np.float32).astype(ml_dtypes.bfloat16)
    _CONST[key] = (tabC, tabS)
    return _CONST[key]


def _shared_consts():
    if "s" in _CONST:
        return _CONST["s"]
    jj = np.arange(256)
    ang = (jj[:, None] * jj[None, :] % 256).astype(np.float64) * (2.0 * np.pi / 256)
    csc = np.concatenate([np.cos(ang), np.sin(ang)], axis=1).astype(np.float32) / 16.0
    p = np.arange(128)[:, None]
    mlin = (np.arange(512)[None, :] - p).astype(np.float32)
    mdiag = np.abs(np.arange(896)[None, :] - p - 384).astype(np.float32)
    _CONST["s"] = (csc, mlin, mdiag)
    return _CONST["s"]


def _prep_inputs(inp):
    x = np.asarray(inp["x"], np.float32)
    csc, mlin, mdiag = _shared_consts()
    vecs = np.zeros((128, 64), np.float32)
    vecs[:, 0:16] = np.asarray(inp["norm1_g"], np.float32).reshape(16, 128).T
    vecs[:, 16:32] = np.asarray(inp["norm2_g"], np.float32).reshape(16, 128).T
    vecs[:, 32] = np.asarray(inp["q_norm_g"], np.float32).reshape(128)
    vecs[:, 33] = np.asarray(inp["k_norm_g"], np.float32).reshape(128)
    vecs[:, 34:36] = np.asarray(inp["subln_g"], np.float32).reshape(2, 128).T
    vecs[:, 36] = np.asarray(inp["lambda_q1"], np.float32).reshape(128)
    vecs[:, 37] = np.asarray(inp["lambda_k1"], np.float32).reshape(128)
    vecs[:, 38] = np.asarray(inp["lambda_q2"], np.float32).reshape(128)
    vecs[:, 39] = np.asarray(inp["lambda_k2"], np.float32).reshape(128)
    w_in = np.ascontiguousarray(np.asarray(inp["w_in"], np.float32)[0])
    w_f = np.ascontiguousarray(np.asarray(inp["w_fourier"], np.float32)[0])
    w_a = np.ascontiguousarray(np.asarray(inp["w_attn"], np.float32)[0])
    w_o = np.ascontiguousarray(np.asarray(inp["w_out"], np.float32)[0])
    w_q = np.ascontiguousarray(np.asarray(inp["w_query"], np.float32)[0])
    sk = np.asarray(inp["sub_keys"], np.float32)[0]
    skT = np.ascontiguousarray(sk.reshape(16, 128, 128).transpose(2, 0, 1))
    e_uT = np.ascontiguousarray(np.asarray(inp["expert_u"], np.float32)[0].T)
    e_v = np.ascontiguousarray(np.asarray(inp["expert_v"], np.float32)[0])
    xTb = [np.ascontiguousarray(x[b].T) for b in range(2)]
    maps = []
    for c in range(8):
        b, j = c // 4, c % 4
        tabC, tabS = _const_tables(j)
        xT = np.ascontiguousarray(np.roll(xTb[b], -OWN * j, axis=1))
        atab = np.zeros((NH, 4, 64, 2), np.float32)
        for qb in range(4):
            q0 = OWN * j + qb * 512
            for kc in range(64):
                k0 = (kc * 128 + OWN * j) % S_LEN
                A = q0 - k0
                for h in range(NH):
                    atab[h, qb, kc, 0] = 1.0 if A > 0 else -1.0
                    atab[h, qb, kc, 1] = -SLOPES[h] * abs(A)
        atab = np.ascontiguousarray(np.broadcast_to(atab.reshape(1, 4096), (128, 4096)))
        maps.append({
            "atab": atab,
            "xT": xT, "w_in": w_in, "vecs": vecs, "csc": csc, "tabC": tabC, "tabS": tabS,
            "mlin": mlin, "mdiag": mdiag, "w_f": w_f, "w_a": w_a, "w_o": w_o, "w_q": w_q,
            "skT": skT, "e_uT": e_uT, "e_v": e_v,
        })
    return maps


def kernel(**inputs):
    maps = _prep_inputs(inputs)
    nc = build()
    res = run_bass_kernel_spmd(nc, maps, core_ids=list(range(8)), trace=True)
    out = np.empty((2, S_LEN, D), np.float32)
    for c in range(8):
        b, j = c // 4, c % 4
        out[b, j * OWN:(j + 1) * OWN, :] = res.results[c]["outT"].T
    return out
```

```python
import math
from contextlib import ExitStack
import numpy as np
import ml_dtypes
import concourse.bass as bass
import concourse.mybir as mybir
from concourse.bass_utils import run_bass_kernel_spmd

F32 = mybir.dt.float32
BF16 = mybir.dt.bfloat16
AF = mybir.ActivationFunctionType
ALU = mybir.AluOpType
AX = mybir.AxisListType

D = 2048
S_LEN = 8192
OWN = 2048
NH = 8
EPS = 1e-6
LAM_INIT = 0.8 - 0.6 * math.exp(-0.3 * 0)
SLOPES = [2.0 ** (-8.0 * (h + 1) / NH) for h in range(NH)]
NEXP = 16384


class Slot:
    __slots__ = ("w", "r")

    def __init__(self):
        self.w = None
        self.r = []


class Sched:
    def __init__(self, nc, es):
        self.nc = nc
        self.eng = {"pe": nc.tensor, "act": nc.scalar, "dve": nc.vector, "pool": nc.gpsimd, "sp": nc.sync}
        self.sem = {}
        self.cnt = {}
        for e in ("pe", "act", "dve", "pool"):
            self.sem[e] = es.enter_context(nc.semaphore("s_" + e))
            self.cnt[e] = 0
        self.seen = {e: {} for e in self.eng}
        self.es = es
        self.ndma = 0

    def dma_sem(self):
        self.ndma += 1
        s = self.es.enter_context(self.nc.semaphore("dq%d" % self.ndma))
        return [s, 0]

    def _wait(self, e, tok):
        if tok is None:
            return
        key, val = tok
        if key == "pe" and e == "pe":
            return
        if isinstance(key, str):
            sem, kid = self.sem[key], key
        else:
            sem, kid = key, id(key)
        if self.seen[e].get(kid, 0) >= val:
            return
        self.eng[e].wait_ge(sem, val)
        self.seen[e][kid] = val

    def _deps(self, e, reads, writes):
        for s in reads:
            self._wait(e, s.w)
        for s in writes:
            self._wait(e, s.w)
            for t in s.r:
                self._wait(e, t)

    def _commit(self, tok, reads, writes):
        for s in reads:
            s.r.append(tok)
            if len(s.r) > 40:
                best = {}
                for k, v in s.r:
                    kk = k if isinstance(k, str) else id(k)
                    if kk not in best or best[kk][1] < v:
                        best[kk] = (k, v)
                s.r = list(best.values())
        for s in writes:
            s.w = tok
            s.r = []

    def op(self, e, fn, reads=(), writes=(), inc=True):
        self._deps(e, reads, writes)
        ins = fn(self.eng[e])
        if inc:
            self.cnt[e] += 1
            ins.then_inc(self.sem[e], 1)
            tok = (e, self.cnt[e])
        else:
            tok = (e, self.cnt[e] + 1)
        self._commit(tok, reads, writes)
        return tok

    def dma(self, q, out, in_, dsem, reads=(), writes=()):
        self._deps(q, reads, writes)
        dsem[1] += 16
        self.eng[q].dma_start(out=out, in_=in_).then_inc(dsem[0], 16)
        tok = (dsem[0], dsem[1])
        self._commit(tok, reads, writes)
        return tok

    def wait_all(self, e, slots):
        for s in slots:
            self._wait(e, s.w)
            for t in s.r:
                self._wait(e, t)


class Ring:
    def __init__(self, items):
        self.items = items
        self.i = 0

    def next(self):
        it = self.items[self.i % len(self.items)]
        self.i += 1
        return it


def build(stage=99):
    nc = bass.Bass("TRN2", target_bir_lowering=False)
    dt_in = lambda n, sh, dt=F32: nc.dram_tensor(n, sh, dt, kind="ExternalInput").ap()
    xT = dt_in("xT", [D, S_LEN])
    import os as _os
    WEXP = bool(_os.environ.get("WBF16"))
    w_in = dt_in("w_in", [D, 11264], BF16 if WEXP else F32)
    vecs = dt_in("vecs", [128, 64])
    csc = dt_in("csc", [256, 512])
    if stage >= 2:
        tabC = dt_in("tabC", [S_LEN, OWN], BF16)
        tabS = dt_in("tabS", [S_LEN, OWN], BF16)
    if stage >= 3:
        mlin_d = dt_in("mlin", [128, 512])
        mdiag_d = dt_in("mdiag", [128, 896])
        atab_d = dt_in("atab", [128, 4096])
    if stage >= 4:
        w_f = dt_in("w_f", [1024, D])
        w_a = dt_in("w_a", [D, D])
        w_o = dt_in("w_o", [D, D])
    if stage >= 5:
        w_q = dt_in("w_q", [D, D])
        skT = dt_in("skT", [128, 16, 128])
        e_uT = dt_in("e_uT", [D, NEXP])
        e_v = dt_in("e_v", [NEXP, D])
    outT = nc.dram_tensor("outT", [D, OWN], F32, kind="ExternalOutput").ap()
    dbg = stage < 99
    dkind = "ExternalOutput" if dbg else "Internal"
    scr = lambda n, sh, dt=BF16: nc.dram_tensor(n, sh, dt, kind=dkind).ap()
    KT = scr("KT", [16, 128, S_LEN])
    QT = scr("QT", [16, 128, OWN])
    Vs = scr("Vs", [NH, S_LEN, 256])
    Zcs = scr("Zcs", [4, S_LEN, 512])
    G = scr("G", [2, D, OWN])
    oT = scr("oT", [D, OWN])
    YfT = scr("YfT", [1024, OWN])
    hTs = scr("hTs", [D, OWN], F32)

    es = ExitStack()
    with es:
        S = Sched(nc, es)
        sbt = lambda n, sh, dt: es.enter_context(nc.sbuf_tensor(n, sh, dt))
        PS = []
        for i in range(8):
            PS.append((es.enter_context(nc.psum_tensor("ps%d" % i, [128, 512], F32)), Slot()))
        pmain = Ring(PS[0:4])
        paux = Ring(PS[4:8])

        vec = sbt("vec", [128, 64], F32)
        s_vec = Slot()
        dq_c = S.dma_sem()
        S.dma("sp", vec[:], vecs[:, :], dq_c, writes=[s_vec])
        ones_b = sbt("ones_b", [128, 128], BF16)
        s_ones = Slot()
        S.op("dve", lambda v: v.memset(ones_b[:], 1.0), writes=[s_ones])
        g1 = vec[:, 0:16]
        gqk = sbt("gqk", [128, 2], F32)
        s_gqk = Slot()
        S.op("dve", lambda v: v.tensor_scalar(gqk[:, 0:1], vec[:, 32:33], 128.0 ** -0.5, None, ALU.mult), reads=[s_vec], writes=[s_gqk])
        S.op("dve", lambda v: v.tensor_copy(gqk[:, 1:2], vec[:, 33:34]), reads=[s_vec], writes=[s_gqk])

        cscb = sbt("cscb", [128, 2, 512], BF16)
        s_csc = Slot()
        dq_p = S.dma_sem()
        S.dma("pool", cscb[:], csc.rearrange("(c p) n -> p c n", p=128), dq_p, writes=[s_csc])

        def phaseA():
            esA = ExitStack()
            with esA:
                sa = lambda n, sh, dt: esA.enter_context(nc.sbuf_tensor(n, sh, dt))
                NT = 256
                xin = Ring([(sa("xin%d" % i, [128, 16, NT], F32), Slot()) for i in range(2)])
                sqb = Ring([(sa("sqb%d" % i, [128, 16, NT], BF16), Slot()) for i in range(1)])
                rst = Ring([(sa("rst%d" % i, [128, NT], F32), Slot()) for i in range(2)])
                xn = sa("xn", [128, 16, 2048], BF16)
                s_xn = [Slot() for _ in range(8)]
                wb = Ring([(sa("wb%d" % i, [128, 16, 512], BF16), Slot()) for i in range(3)])
                wdq = [S.dma_sem() for _ in range(3)]
                xdq = [S.dma_sem() for _ in range(2)]
                zT = Ring([(sa("zT%d" % i, [128, 2, 512], BF16), Slot()) for i in range(2)])
                zst = Ring([(sa("zst%d" % i, [128, 4, 512], BF16), Slot()) for i in range(2)])
                zdq = [S.dma_sem() for _ in range(2)]
                raw = Ring([(sa("raw%d" % i, [128, 512], F32), Slot()) for i in range(3)])
                sq2 = Ring([(sa("sq2%d" % i, [128, 512], BF16), Slot()) for i in range(3)])
                sd2 = Ring([(sa("sd2%d" % i, [128, 512], F32), Slot()) for i in range(3)])
                st4 = Ring([(sa("st4%d" % i, [128, 4, 512], BF16), Slot()) for i in range(3)])
                st4dq = [S.dma_sem() for _ in range(3)]
                vst = Ring([(sa("vst%d" % i, [128, 512], BF16), Slot()) for i in range(3)])
                vdq = [S.dma_sem() for _ in range(3)]
                w_in_v = w_in.rearrange("(c p) n -> p c n", p=128)
                xT_v = xT.rearrange("(c p) t -> p c t", p=128)
                scr_slots = []

                def xn_slots(t0, n):
                    return s_xn[t0 // NT:(t0 + n + NT - 1) // NT]

                import os
                for st in range(int(os.environ.get('PH_A_ST0', '0')), int(os.environ.get('PH_A_ST', '4'))):
                    own = None
                    is_own = (st == 0)
                    tok0 = st * 2048
                    for nt in range(8):
                        xi, s_xi = xin.next()
                        xq = xdq[(xin.i - 1) % 2]
                        S.dma("sp", xi[:], xT_v[:, :, tok0 + nt * NT: tok0 + (nt + 1) * NT], xq, writes=[s_xi])
                        sq, s_sq = sqb.next()
                        S.op("act", lambda a: a.activation(out=sq[:], in_=xi[:], func=AF.Square), reads=[s_xi], writes=[s_sq])
                        pa, s_pa = paux.next()
                        for c in range(16):
                            S.op("pe", lambda t, c=c: t.matmul(pa[:, 0:NT], ones_b[:], sq[:, c, :], start=(c == 0), stop=(c == 15)),
                                 reads=[s_ones, s_sq], writes=[s_pa], inc=(c == 15))
                        rs, s_rs = rst.next()
                        S.op("act", lambda a: a.activation(out=rs[:], in_=pa[:, 0:NT], func=AF.Sqrt, bias=EPS, scale=1.0 / D), reads=[s_pa], writes=[s_rs])
                        S.op("dve", lambda v: v.reciprocal(rs[:], rs[:]), reads=[s_rs], writes=[s_rs])
                        for c in range(16):
                            edge = c in (0, 15)
                            S.op("dve", lambda v, c=c: v.scalar_tensor_tensor(xn[:, c, nt * NT:(nt + 1) * NT], xi[:, c, :], g1[:, c:c + 1], rs[:], ALU.mult, ALU.mult),
                                 reads=[s_xi, s_rs, s_vec] if edge else [], writes=[s_xn[nt]] if edge else [])
                    blocks = [("z", 0), ("z", 1)]
                    if is_own:
                        blocks += [("q", i) for i in range(4)]
                    blocks += [("k", i) for i in range(4)] + [("v", i) for i in range(4)]
                    if is_own:
                        blocks += [("gf", i) for i in range(4)] + [("ga", i) for i in range(4)]
                    col_base = {"z": 0, "q": 1024, "k": 3072, "v": 5120, "gf": 7168, "ga": 9216}
                    kk = os.environ.get('PH_A_KINDS')
                    if kk is not None:
                        blocks = [b_ for b_ in blocks if b_[0] in kk.split(',')]
                    for kind, bi in blocks:
                        col0 = col_base[kind] + bi * 512
                        w, s_w = wb.next()
                        wq = wdq[(wb.i - 1) % 3]
                        S.dma("sp" if WEXP else "pool", w[:], w_in_v[:, :, col0:col0 + 512], wq, writes=[s_w])
                        if kind == "v":
                            for sub in range(16):
                                pm, s_pm = pmain.next()
                                for kc in range(16):
                                    S.op("pe", lambda t, kc=kc: t.matmul(pm[:], xn[:, kc, sub * 128:(sub + 1) * 128], w[:, kc, :], start=(kc == 0), stop=(kc == 15)),
                                         reads=[s_w] + xn_slots(sub * 128, 128), writes=[s_pm], inc=(kc == 15))
                                vs_, s_vs = vst.next()
                                vq = vdq[(vst.i - 1) % 3]
                                S.op("dve", lambda v: v.tensor_copy(vs_[:], pm[:]), reads=[s_pm], writes=[s_vs])
                                dst = Vs[2 * bi:2 * bi + 2, tok0 + sub * 128: tok0 + (sub + 1) * 128, :].rearrange("h t d -> t h d")
                                sl = Slot()
                                scr_slots.append(sl)
                                S.dma("sp", dst, vs_[:].rearrange("p (h d) -> p h d", h=2), vq, reads=[s_vs], writes=[sl])
                            continue
                        if kind == "z":
                            for gi in range(2):
                                g = 2 * bi + gi
                                for t in range(4):
                                    z, s_z = zT.next()
                                    for c2 in range(2):
                                        cc = gi * 2 + c2
                                        pm, s_pm = pmain.next()
                                        for kc in range(16):
                                            S.op("pe", lambda t_, kc=kc: t_.matmul(pm[:], w[:, kc, cc * 128:(cc + 1) * 128], xn[:, kc, t * 512:(t + 1) * 512], start=(kc == 0), stop=(kc == 15)),
                                                 reads=[s_w] + xn_slots(t * 512, 512), writes=[s_pm], inc=(kc == 15))
                                        S.op("dve", lambda v: v.tensor_copy(z[:, c2, :], pm[:]), reads=[s_pm], writes=[s_z])
                                    zs, s_zs = zst.next()
                                    zq = zdq[(zst.i - 1) % 2]
                                    for sub in range(4):
                                        pa, s_pa = paux.next()
                                        for c2 in range(2):
                                            S.op("pe", lambda t_, c2=c2: t_.matmul(pa[:], z[:, c2, sub * 128:(sub + 1) * 128], cscb[:, c2, :], start=(c2 == 0), stop=(c2 == 1)),
                                                 reads=[s_z, s_csc], writes=[s_pa], inc=(c2 == 1))
                                        S.op("dve", lambda v: v.tensor_copy(zs[:, sub, :], pa[:]), reads=[s_pa], writes=[s_zs])
                                    dst = Zcs[g, tok0 + t * 512: tok0 + (t + 1) * 512, :].rearrange("(s p) n -> p s n", p=128)
                                    sl = Slot()
                                    scr_slots.append(sl)
                                    S.dma("sp", dst, zs[:], zq, reads=[s_zs], writes=[sl])
                            continue
                        pend = [None]

                        def qk_tail(info):
                            (kind_, ch_, stg_, s_stg_, sq__, t_i, r, s_r, q2, s_q2, d2, s_d2, is_last) = info
                            pa, s_pa = paux.next()
                            S.op("pe", lambda t_: t_.matmul(pa[:], ones_b[:], q2[:], start=True, stop=True), reads=[s_ones, s_q2], writes=[s_pa])
                            S.op("act", lambda a: a.activation(out=d2[:], in_=pa[:], func=AF.Sqrt, bias=EPS, scale=1.0 / 128), reads=[s_pa], writes=[s_d2])
                            S.op("dve", lambda v: v.reciprocal(d2[:], d2[:]), reads=[s_d2], writes=[s_d2])
                            gcol = gqk[:, 0:1] if kind_ == "q" else gqk[:, 1:2]
                            S.op("dve", lambda v: v.scalar_tensor_tensor(stg_[:, t_i, :], r[:], gcol, d2[:], ALU.mult, ALU.mult),
                                 reads=[s_r, s_d2, s_gqk], writes=[s_stg_])
                            if is_last:
                                store_stg(kind_, ch_, stg_, s_stg_, sq__)

                        def store_stg(kind_, ch_, stg_, s_stg_, sq__):
                            if kind_ == "q":
                                dst = QT[ch_, :, :].rearrange("p (t n) -> p t n", t=4)
                            elif kind_ == "k":
                                dst = KT[ch_, :, tok0:tok0 + 2048].rearrange("p (t n) -> p t n", t=4)
                            else:
                                dst = G[0 if kind_ == "gf" else 1, ch_ * 128:(ch_ + 1) * 128, :].rearrange("p (t n) -> p t n", t=4)
                            sl = Slot()
                            scr_slots.append(sl)
                            S.dma("sp", dst, stg_[:], sq__, reads=[s_stg_], writes=[sl])

                        for cc in range(4):
                            stg, s_stg = st4.next()
                            sq_ = st4dq[(st4.i - 1) % 3]
                            ch = bi * 4 + cc
                            for t in range(4):
                                pm, s_pm = pmain.next()
                                for kc in range(16):
                                    S.op("pe", lambda t_, kc=kc: t_.matmul(pm[:], w[:, kc, cc * 128:(cc + 1) * 128], xn[:, kc, t * 512:(t + 1) * 512], start=(kc == 0), stop=(kc == 15)),
                                         reads=[s_w] + xn_slots(t * 512, 512), writes=[s_pm], inc=(kc == 15))
                                if kind in ("gf", "ga"):
                                    S.op("act", lambda a: a.activation(out=stg[:, t, :], in_=pm[:], func=AF.Sigmoid), reads=[s_pm], writes=[s_stg])
                                    if t == 3:
                                        store_stg(kind, ch, stg, s_stg, sq_)
                                else:
                                    r, s_r = raw.next()
                                    q2, s_q2 = sq2.next()
                                    d2, s_d2 = sd2.next()
                                    S.op("dve", lambda v: v.tensor_copy(r[:], pm[:]), reads=[s_pm], writes=[s_r])
                                    S.op("act", lambda a: a.activation(out=q2[:], in_=r[:], func=AF.Square), reads=[s_r], writes=[s_q2])
                                    if pend[0] is not None:
                                        qk_tail(pend[0])
                                    pend[0] = (kind, ch, stg, s_stg, sq_, t, r, s_r, q2, s_q2, d2, s_d2, t == 3)
                        if pend[0] is not None:
                            qk_tail(pend[0])
                            pend[0] = None
                for e in ("sp", "pool"):
                    S.wait_all(e, scr_slots)
                barrier()

        def barrier():
            for e in ("pe", "act", "dve", "pool", "sp"):
                for o in ("pe", "act", "dve", "pool"):
                    if S.cnt[o] > 0:
                        S._wait(e, (o, S.cnt[o]))

        def phaseC():
            esC = ExitStack()
            with esC:
                sa = lambda n, sh, dt: esC.enter_context(nc.sbuf_tensor(n, sh, dt))
                TC = sa("TC", [128, 64, 512], BF16)
                TS = sa("TS", [128, 64, 512], BF16)
                s_TC, s_TS = Slot(), Slot()
                tq = [S.dma_sem(), S.dma_sem()]
                zp = Ring([(sa("zp%d" % i, [128, 8, 512], BF16), Slot()) for i in range(3)])
                zq = [S.dma_sem() for _ in range(3)]
                yst = Ring([(sa("yst%d" % i, [128, 512], BF16), Slot()) for i in range(2)])
                yq = [S.dma_sem() for _ in range(2)]
                tabC_v = tabC.rearrange("(c p) k -> p c k", p=128)
                tabS_v = tabS.rearrange("(c p) k -> p c k", p=128)
                scr_slots = []
                for kb in range(4):
                    S.dma("sp", TC[:], tabC_v[:, :, kb * 512:(kb + 1) * 512], tq[0], writes=[s_TC])
                    S.dma("sp", TS[:], tabS_v[:, :, kb * 512:(kb + 1) * 512], tq[1], writes=[s_TS])
                    for g in range(4):
                        acc = [pmain.next(), pmain.next()]
                        for pi in range(8):
                            z, s_z = zp.next()
                            q_ = zq[(zp.i - 1) % 3]
                            S.dma("sp", z[:], Zcs[g, pi * 1024:(pi + 1) * 1024, :].rearrange("(c p) n -> p c n", p=128), q_, writes=[s_z])
                            for c in range(8):
                                sc = pi * 8 + c
                                for half in range(2):
                                    pa_, s_pa_ = acc[half]
                                    S.op("pe", lambda t_: t_.matmul(pa_[:], z[:, c, half * 128:(half + 1) * 128], TC[:, sc, :], start=(sc == 0), stop=False),
                                         reads=[s_z, s_TC], writes=[s_pa_], inc=False)
                                    S.op("pe", lambda t_: t_.matmul(pa_[:], z[:, c, 256 + half * 128:256 + (half + 1) * 128], TS[:, sc, :], start=False, stop=(sc == 63)),
                                         reads=[s_z, s_TS], writes=[s_pa_], inc=(c == 7))
                        for half in range(2):
                            pa_, s_pa_ = acc[half]
                            ys, s_ys = yst.next()
                            q_ = yq[(yst.i - 1) % 2]
                            S.op("dve", lambda v: v.tensor_copy(ys[:], pa_[:]), reads=[s_pa_], writes=[s_ys])
                            sl = Slot()
                            scr_slots.append(sl)
                            ch = g * 2 + half
                            S.dma("sp", YfT[ch * 128:(ch + 1) * 128, kb * 512:(kb + 1) * 512], ys[:], q_, reads=[s_ys], writes=[sl])
                for e in ("sp", "pool"):
                    S.wait_all(e, scr_slots)
                barrier()

        def phaseB():
            esB = ExitStack()
            with esB:
                sa = lambda n, sh, dt: esB.enter_context(nc.sbuf_tensor(n, sh, dt))
                mlin = sa("mlin_sb", [128, 512], F32)
                mdiag = sa("mdiag_sb", [128, 896], F32)
                at = sa("atab_sb", [128, 4096], F32)
                s_cst = Slot()
                cq = S.dma_sem()
                S.dma("sp", mlin[:], mlin_d[:, :], cq, writes=[s_cst])
                S.dma("sp", mdiag[:], mdiag_d[:, :], cq, writes=[s_cst])
                S.dma("sp", at[:], atab_d[:, :], cq, writes=[s_cst])
                ones_f = sa("ones_f", [128, 128], F32)
                s_of = Slot()
                S.op("dve", lambda v: v.memset(ones_f[:], 1.0), writes=[s_of])
                lam = sa("lam", [128, 8], F32)
                s_lam = Slot()
                S.op("dve", lambda v: v.tensor_tensor(lam[:, 0:1], vec[:, 36:37], vec[:, 37:38], ALU.mult), reads=[s_vec], writes=[s_lam])
                S.op("dve", lambda v: v.tensor_tensor(lam[:, 1:2], vec[:, 38:39], vec[:, 39:40], ALU.mult), reads=[s_vec], writes=[s_lam])
                pmx, s_pmx = PS[4]
                S.op("pe", lambda t_: t_.matmul(pmx[:, 0:2], ones_f[:], lam[:, 0:2], start=True, stop=True), reads=[s_of, s_lam], writes=[s_pmx])
                S.op("act", lambda a: a.activation(out=lam[:, 2:4], in_=pmx[:, 0:2], func=AF.Exp), reads=[s_pmx], writes=[s_lam])
                S.op("dve", lambda v: v.tensor_tensor(lam[:, 4:5], lam[:, 2:3], lam[:, 3:4], ALU.subtract), reads=[s_lam], writes=[s_lam])
                S.op("dve", lambda v: v.tensor_scalar(lam[:, 5:6], lam[:, 4:5], LAM_INIT, -1.0, ALU.add, ALU.mult), reads=[s_lam], writes=[s_lam])
                nlam = lam[:, 5:6]
                gsub = sa("gsub", [128, 2], F32)
                s_gsub = Slot()
                S.op("dve", lambda v: v.tensor_scalar(gsub[:], vec[:, 34:36], 1.0 - LAM_INIT, None, ALU.mult), reads=[s_vec], writes=[s_gsub])

                kt = Ring([(sa("kt%d" % i, [128, 2, S_LEN], BF16), Slot()) for i in range(2)])
                vt = Ring([(sa("vt%d" % i, [128, 64, 256], BF16), Slot()) for i in range(2)])
                qt = Ring([(sa("qt%d" % i, [128, 2, OWN], BF16), Slot()) for i in range(2)])
                hq = [S.dma_sem() for _ in range(2)]
                baseL = sa("baseL", [128, 512], F32)
                baseD = sa("baseD", [128, 896], F32)
                s_base = Slot()
                tt = Ring([(sa("tt%d" % i, [128, 512], F32), Slot()) for i in range(4)])
                pp = Ring([(sa("pp%d" % i, [128, 512], BF16), Slot()) for i in range(4)])
                ev = [sa("ev%d" % i, [128, 512], F32) for i in range(3)]
                s_ev = Slot()
                acc = sa("acc", [128, 2, 512], F32)
                s_acc = Slot()
                sqo = sa("sqo", [128, 2, 512], BF16)
                s_sqo = Slot()
                sdo = sa("sdo", [128, 512], F32)
                s_sdo = Slot()
                ost = Ring([(sa("ost%d" % i, [128, 2, 512], BF16), Slot()) for i in range(2)])
                oq = [S.dma_sem() for _ in range(2)]
                scr_slots = []
                ps_s = Ring(PS[0:4])
                (pO0, s_pO0), (pO1, s_pO1), (pZ, s_pZ) = PS[5], PS[6], PS[7]
                LAG = 3

                def needed_chunks(h, qb):
                    dmin = (2 * 16.0 + 22.0) / SLOPES[h]
                    out = []
                    for kc in range(64):
                        best = 1 << 30
                        for j in range(4):
                            k0 = (kc * 128 + OWN * j) % S_LEN
                            q0 = OWN * j + qb * 512
                            if k0 >= q0 + 512:
                                d = k0 - (q0 + 511)
                            elif k0 + 128 <= q0:
                                d = q0 - (k0 + 127)
                            else:
                                d = 0
                            best = min(best, d)
                        if best < dmin:
                            out.append(kc)
                    return out

                hbuf = {}

                def load_head(h):
                    k_, s_k = kt.next()
                    v_, s_v = vt.next()
                    q_, s_q = qt.next()
                    dq = hq[h % 2]
                    S.dma("sp", k_[:], KT[2 * h:2 * h + 2, :, :].rearrange("m p t -> p m t"), dq, writes=[s_k])
                    S.dma("sp", v_[:], Vs[h, :, :].rearrange("(c p) d -> p c d", p=128), dq, writes=[s_v])
                    S.dma("sp", q_[:], QT[2 * h:2 * h + 2, :, :].rearrange("m p t -> p m t"), dq, writes=[s_q])
                    hbuf[h] = (k_, s_k, v_, s_v, q_, s_q)

                load_head(0)
                for h in range(NH):
                    if h + 1 < NH:
                        load_head(h + 1)
                    k_, s_k, v_, s_v, q_, s_q = hbuf.pop(h)
                    S.op("pool", lambda g_: g_.tensor_scalar(baseL[:], mlin[:], -SLOPES[h], None, ALU.mult), reads=[s_cst], writes=[s_base])
                    S.op("pool", lambda g_: g_.tensor_scalar(baseD[:], mdiag[:], -SLOPES[h], None, ALU.mult), reads=[s_cst], writes=[s_base])
                    for qb in range(4):
                        for m in range(2):
                            tiles = {}

                            def issue_s(kc):
                                ps_, s_ps = ps_s.next()
                                S.op("pe", lambda t_: t_.matmul(ps_[:], k_[:, m, kc * 128:(kc + 1) * 128], q_[:, m, qb * 512:(qb + 1) * 512], start=True, stop=True),
                                     reads=[s_k, s_q], writes=[s_ps])
                                t, s_t = tt.next()
                                col = ((h * 4 + qb) * 64 + kc) * 2
                                diag = (qb * 4 <= kc < qb * 4 + 4)
                                if diag:
                                    delta = (kc - qb * 4) * 128
                                    bs = baseD[:, 384 - delta:384 - delta + 512]
                                    S.op("dve", lambda v: v.tensor_tensor(t[:], ps_[:], bs, ALU.add), reads=[s_ps, s_base], writes=[s_t])
                                else:
                                    S.op("dve", lambda v: v.scalar_tensor_tensor(t[:], baseL[:], at[:, col:col + 1], ps_[:], ALU.mult, ALU.add),
                                         reads=[s_ps, s_base, s_cst], writes=[s_t])
                                p, s_p = pp.next()
                                if diag:
                                    S.op("act", lambda a: a.activation(out=p[:], in_=t[:], func=AF.Exp), reads=[s_t], writes=[s_p])
                                else:
                                    S.op("act", lambda a: a.activation(out=p[:], in_=t[:], func=AF.Exp, bias=at[:, col + 1:col + 2]), reads=[s_t, s_cst], writes=[s_p])
                                tiles[kc] = (p, s_p)

                            def issue_pv(kc, st_, sp_):
                                p, s_p = tiles.pop(kc)
                                S.op("pe", lambda t_: t_.matmul(pO0[:], v_[:, kc, 0:128], p[:], start=st_, stop=sp_), reads=[s_v, s_p], writes=[s_pO0], inc=False)
                                S.op("pe", lambda t_: t_.matmul(pO1[:], v_[:, kc, 128:256], p[:], start=st_, stop=sp_), reads=[s_v, s_p], writes=[s_pO1], inc=False)
                                S.op("pe", lambda t_: t_.matmul(pZ[:], ones_b[:], p[:], start=st_, stop=sp_), reads=[s_ones, s_p], writes=[s_pZ], inc=True)

                            chunks = needed_chunks(h, qb)
                            nch = len(chunks)
                            for ix in range(nch + LAG):
                                if ix < nch:
                                    issue_s(chunks[ix])
                                if ix >= LAG:
                                    issue_pv(chunks[ix - LAG], ix - LAG == 0, ix - LAG == nch - 1)
                            S.op("dve", lambda v: v.reciprocal(ev[2][:], pZ[:]), reads=[s_pZ], writes=[s_ev])
                            if m == 0:
                                S.op("dve", lambda v: v.tensor_tensor(acc[:, 0, :], pO0[:], ev[2][:], ALU.mult), reads=[s_pO0, s_ev], writes=[s_acc])
                                S.op("dve", lambda v: v.tensor_tensor(acc[:, 1, :], pO1[:], ev[2][:], ALU.mult), reads=[s_pO1, s_ev], writes=[s_acc])
                            else:
                                S.op("dve", lambda v: v.tensor_tensor(ev[0][:], pO0[:], ev[2][:], ALU.mult), reads=[s_pO0, s_ev], writes=[s_ev])
                                S.op("dve", lambda v: v.tensor_tensor(ev[1][:], pO1[:], ev[2][:], ALU.mult), reads=[s_pO1, s_ev], writes=[s_ev])
                                for dv in range(2):
                                    S.op("dve", lambda v, dv=dv: v.scalar_tensor_tensor(acc[:, dv, :], ev[dv][:], nlam, acc[:, dv, :], ALU.mult, ALU.add),
                                         reads=[s_ev, s_lam, s_acc], writes=[s_acc])
                        S.op("act", lambda a: a.activation(out=sqo[:], in_=acc[:], func=AF.Square), reads=[s_acc], writes=[s_sqo])
                        for dv in range(2):
                            S.op("pe", lambda t_, dv=dv: t_.matmul(pmx[:], ones_b[:], sqo[:, dv, :], start=(dv == 0), stop=(dv == 1)), reads=[s_ones, s_sqo], writes=[s_pmx], inc=(dv == 1))
                        S.op("act", lambda a: a.activation(out=sdo[:], in_=pmx[:], func=AF.Sqrt, bias=EPS, scale=1.0 / 256), reads=[s_pmx], writes=[s_sdo])
                        S.op("dve", lambda v: v.reciprocal(sdo[:], sdo[:]), reads=[s_sdo], writes=[s_sdo])
                        os_, s_os = ost.next()
                        oq_ = oq[(ost.i - 1) % 2]
                        for dv in range(2):
                            S.op("dve", lambda v, dv=dv: v.scalar_tensor_tensor(os_[:, dv, :], acc[:, dv, :], gsub[:, dv:dv + 1], sdo[:], ALU.mult, ALU.mult),
                                 reads=[s_acc, s_gsub, s_sdo], writes=[s_os])
                        sl = Slot()
                        scr_slots.append(sl)
                        S.dma("sp", oT[h * 256:(h + 1) * 256, qb * 512:(qb + 1) * 512].rearrange("(c p) t -> p c t", p=128), os_[:], oq_, reads=[s_os], writes=[sl])
                for e in ("sp", "pool"):
                    S.wait_all(e, scr_slots)
                barrier()

        def phaseD():
            esD = ExitStack()
            with esD:
                sa = lambda n, sh, dt: esD.enter_context(nc.sbuf_tensor(n, sh, dt))
                yf = sa("yf", [128, 8, 512], BF16)
                ot = sa("ot", [128, 16, 512], BF16)
                gf = sa("gf", [128, 16, 512], BF16)
                ga = sa("ga", [128, 16, 512], BF16)
                xt = sa("xt", [128, 16, 512], F32)
                mixed = sa("mixed", [128, 16, 512], BF16)
                s_in, s_xt, s_mixed = Slot(), Slot(), Slot()
                inq = S.dma_sem()
                xq = S.dma_sem()
                hq_ = S.dma_sem()
                wf = Ring([(sa("wf%d" % i, [128, 8, 512], BF16), Slot()) for i in range(2)])
                wa = Ring([(sa("wa%d" % i, [128, 16, 512], BF16), Slot()) for i in range(2)])
                wo = Ring([(sa("wo%d" % i, [128, 16, 512], BF16), Slot()) for i in range(2)])
                wfq = [S.dma_sem() for _ in range(2)]
                waq = [S.dma_sem() for _ in range(2)]
                woq = [S.dma_sem() for _ in range(2)]
                t1 = Ring([(sa("t1_%d" % i, [128, 512], F32), Slot()) for i in range(2)])
                t2 = Ring([(sa("t2_%d" % i, [128, 512], F32), Slot()) for i in range(2)])
                YfT_v = YfT.rearrange("(c p) k -> p c k", p=128)
                oT_v = oT.rearrange("(c p) k -> p c k", p=128)
                Gf_v = G[0].rearrange("(c p) k -> p c k", p=128)
                Ga_v = G[1].rearrange("(c p) k -> p c k", p=128)
                xT_v = xT.rearrange("(c p) t -> p c t", p=128)
                hT_v = hTs.rearrange("(c p) t -> p c t", p=128)
                w_f_v = w_f.rearrange("(c p) n -> p c n", p=128)
                w_a_v = w_a.rearrange("(c p) n -> p c n", p=128)
                w_o_v = w_o.rearrange("(c p) n -> p c n", p=128)
                scr_slots = []
                for t in range(4):
                    ts_ = slice(t * 512, (t + 1) * 512)
                    S.dma("sp", yf[:], YfT_v[:, :, ts_], inq, writes=[s_in])
                    S.dma("sp", ot[:], oT_v[:, :, ts_], inq, writes=[s_in])
                    S.dma("sp", gf[:], Gf_v[:, :, ts_], inq, writes=[s_in])
                    S.dma("sp", ga[:], Ga_v[:, :, ts_], inq, writes=[s_in])
                    S.dma("sp", xt[:], xT_v[:, :, ts_], xq, writes=[s_xt])
                    for ob in range(4):
                        wf_, s_wf = wf.next()
                        wa_, s_wa = wa.next()
                        S.dma("pool", wf_[:], w_f_v[:, :, ob * 512:(ob + 1) * 512], wfq[(wf.i - 1) % 2], writes=[s_wf])
                        S.dma("pool", wa_[:], w_a_v[:, :, ob * 512:(ob + 1) * 512], waq[(wa.i - 1) % 2], writes=[s_wa])
                        for cc in range(4):
                            oc = ob * 4 + cc
                            pf, s_pf = pmain.next()
                            for kc in range(8):
                                S.op("pe", lambda t_, kc=kc: t_.matmul(pf[:], wf_[:, kc, cc * 128:(cc + 1) * 128], yf[:, kc, :], start=(kc == 0), stop=(kc == 7)),
                                     reads=[s_wf, s_in], writes=[s_pf], inc=(kc == 7))
                            pa_, s_pa_ = pmain.next()
                            for kc in range(16):
                                S.op("pe", lambda t_, kc=kc: t_.matmul(pa_[:], wa_[:, kc, cc * 128:(cc + 1) * 128], ot[:, kc, :], start=(kc == 0), stop=(kc == 15)),
                                     reads=[s_wa, s_in], writes=[s_pa_], inc=(kc == 15))
                            a1, s_a1 = t1.next()
                            a2, s_a2 = t2.next()
                            S.op("dve", lambda v: v.tensor_tensor(a1[:], pf[:], gf[:, oc, :], ALU.mult), reads=[s_pf, s_in], writes=[s_a1])
                            S.op("dve", lambda v: v.tensor_tensor(a2[:], pa_[:], ga[:, oc, :], ALU.mult), reads=[s_pa_, s_in], writes=[s_a2])
                            S.op("pool", lambda g_: g_.tensor_tensor(mixed[:, oc, :], a1[:], a2[:], ALU.add), reads=[s_a1, s_a2], writes=[s_mixed])
                    for ob in range(4):
                        wo_, s_wo = wo.next()
                        S.dma("pool", wo_[:], w_o_v[:, :, ob * 512:(ob + 1) * 512], woq[(wo.i - 1) % 2], writes=[s_wo])
                        for cc in range(4):
                            oc = ob * 4 + cc
                            ph, s_ph = pmain.next()
                            for kc in range(16):
                                S.op("pe", lambda t_, kc=kc: t_.matmul(ph[:], wo_[:, kc, cc * 128:(cc + 1) * 128], mixed[:, kc, :], start=(kc == 0), stop=(kc == 15)),
                                     reads=[s_wo, s_mixed], writes=[s_ph], inc=(kc == 15))
                            S.op("dve", lambda v: v.tensor_tensor(xt[:, oc, :], ph[:], xt[:, oc, :], ALU.add), reads=[s_ph, s_xt], writes=[s_xt])
                    sl = Slot()
                    scr_slots.append(sl)
                    S.dma("sp", hT_v[:, :, ts_], xt[:], hq_, reads=[s_xt], writes=[sl])
                for e in ("sp", "pool"):
                    S.wait_all(e, scr_slots)
                barrier()

        def phaseE():
            esE = ExitStack()
            with esE:
                sa = lambda n, sh, dt: esE.enter_context(nc.sbuf_tensor(n, sh, dt))
                ident = sa("ident", [128, 128], BF16)
                s_id = Slot()
                S.op("pool", lambda g_: g_.memset(ident[:], 0.0), writes=[s_id])
                S.op("pool", lambda g_: g_.affine_select(out=ident[:], in_=ident[:], pattern=[[-1, 128]], compare_op=ALU.not_equal, fill=1.0, base=0, channel_multiplier=1), reads=[s_id], writes=[s_id])
                skb = sa("skb", [128, 16, 128], BF16)
                s_skb = Slot()
                kq_ = S.dma_sem()
                S.dma("pool", skb[:], skT[:, :, :], kq_, writes=[s_skb])
                acc = sa("pacc", [128, 16, 512], F32)
                s_acc = Slot()
                hn = sa("hn", [128, 16, 512], BF16)
                s_hn = Slot()
                ssb = sa("ssb", [128, 4, 16, 128], F32)
                s_ssb = Slot()
                thr = sa("thr", [128, 4, 8], F32)
                s_thr = Slot()
                s1b = sa("s1b", [128, 4, 8, 128], F32)
                s_e12 = Slot()
                e1s = sa("e1s", [128, 4, 2, 128], BF16)
                e2s = sa("e2s", [128, 4, 2, 128], BF16)
                s_e67 = Slot()
                accq = S.dma_sem()
                outq = S.dma_sem()
                ub = Ring([(sa("ub%d" % i, [128, 16, 512], BF16), Slot()) for i in range(2)])
                ubq = [S.dma_sem() for _ in range(2)]
                hT_v = hTs.rearrange("(c p) t -> p c t", p=128)
                outT_v = outT.rearrange("(c p) t -> p c t", p=128)
                w_q_v = w_q.rearrange("(c p) n -> p c n", p=128)
                e_uT_v = e_uT.rearrange("(c p) e -> p c e", p=128)
                g2 = vec[:, 16:32]
                out_slots = []
                pq = Ring(PS[0:3])
                pT = [PS[3], PS[4]]
                pv = Ring(PS[5:8])
                for t in range(4):
                    ts_ = slice(t * 512, (t + 1) * 512)
                    S.dma("sp", acc[:], hT_v[:, :, ts_], accq, writes=[s_acc])
                    es1 = ExitStack()
                    with es1:
                        s1a = lambda n, sh, dt: es1.enter_context(nc.sbuf_tensor(n + '_%d' % t, sh, dt))
                        sq = s1a("esq", [128, 16, 512], BF16)
                        s_sq = Slot()
                        rs = s1a("ers", [128, 512], F32)
                        s_rs = Slot()
                        qT_ = s1a("eqT", [128, 16, 512], BF16)
                        s_qT = Slot()
                        m16 = s1a("m16", [128, 16, 16], F32)
                        s_m16 = Slot()
                        tmpk = s1a("etmpk", [128, 16, 128], F32)
                        s_tmpk = [Slot() for _ in range(16)]
                        tmpc = s1a("etmpc", [128, 8, 256], F32)
                        s_tmpc = [Slot() for _ in range(8)]
                        s_m16h = [Slot() for _ in range(16)]
                        s_t16h = [Slot() for _ in range(8)]
                        cand = s1a("cand", [128, 8, 16, 16], F32)
                        s_cand = Slot()
                        t16 = s1a("t16", [128, 8, 16], F32)
                        s_t16 = Slot()
                        sm = s1a("sm", [128, 8, 8], F32)
                        s_sm = Slot()
                        S.op("act", lambda a: a.activation(out=sq[:], in_=acc[:], func=AF.Square), reads=[s_acc], writes=[s_sq])
                        pa_, s_pa_ = pq.next()
                        for c in range(16):
                            S.op("pe", lambda t_, c=c: t_.matmul(pa_[:], ones_b[:], sq[:, c, :], start=(c == 0), stop=(c == 15)), reads=[s_ones, s_sq], writes=[s_pa_], inc=(c == 15))
                        S.op("act", lambda a: a.activation(out=rs[:], in_=pa_[:], func=AF.Sqrt, bias=EPS, scale=1.0 / D), reads=[s_pa_], writes=[s_rs])
                        S.op("dve", lambda v: v.reciprocal(rs[:], rs[:]), reads=[s_rs], writes=[s_rs])
                        for c in range(16):
                            S.op("dve", lambda v, c=c: v.scalar_tensor_tensor(hn[:, c, :], acc[:, c, :], g2[:, c:c + 1], rs[:], ALU.mult, ALU.mult),
                                 reads=[s_acc, s_rs, s_vec], writes=[s_hn])
                        for ob in range(4):
                            u_, s_u = ub.next()
                            S.dma("pool", u_[:], w_q_v[:, :, ob * 512:(ob + 1) * 512], ubq[(ub.i - 1) % 2], writes=[s_u])
                            for cc in range(4):
                                pm, s_pm = pq.next()
                                for kc in range(16):
                                    S.op("pe", lambda t_, kc=kc: t_.matmul(pm[:], u_[:, kc, cc * 128:(cc + 1) * 128], hn[:, kc, :], start=(kc == 0), stop=(kc == 15)),
                                         reads=[s_u, s_hn], writes=[s_pm], inc=(kc == 15))
                                S.op("dve", lambda v: v.tensor_copy(qT_[:, ob * 4 + cc, :], pm[:]), reads=[s_pm], writes=[s_qT])
                        for sub in range(4):
                            for q4 in range(4):
                                pm, s_pm = pq.next()
                                for i4 in range(4):
                                    hc = q4 * 4 + i4
                                    S.op("pe", lambda t_, hc=hc, i4=i4: t_.matmul(pm[:, i4 * 128:(i4 + 1) * 128], qT_[:, hc, sub * 128:(sub + 1) * 128], skb[:, hc, :], start=True, stop=True),
                                         reads=[s_qT, s_skb], writes=[s_pm], inc=(i4 == 3))
                                S.op("dve", lambda v: v.tensor_copy(ssb[:, sub, q4 * 4:(q4 + 1) * 4, :], pm[:].rearrange("p (a n) -> p a n", a=4)), reads=[s_pm], writes=[s_ssb])
                            for hc in range(16):
                                S.op("dve", lambda v, hc=hc: v.max(out=m16[:, hc, 0:8], in_=ssb[:, sub, hc, :]), reads=[s_ssb], writes=[s_m16h[hc]])
                            for hc in range(16):
                                S.op("dve", lambda v, hc=hc: v.match_replace(out=tmpk[:, hc, :], in_to_replace=m16[:, hc, 0:8], in_values=ssb[:, sub, hc, :], imm_value=-1e30), reads=[s_ssb, s_m16h[hc]], writes=[s_tmpk[hc]])
                            for hc in range(16):
                                S.op("dve", lambda v, hc=hc: v.max(out=m16[:, hc, 8:16], in_=tmpk[:, hc, :]), reads=[s_tmpk[hc]], writes=[s_m16h[hc]])
                            m16v = m16[:].rearrange("p (h c) k -> p h c k", c=2)
                            S.op("dve", lambda v: v.tensor_tensor(cand[:], m16v[:, :, 0, :].unsqueeze(3).to_broadcast([128, 8, 16, 16]),
                                                                  m16v[:, :, 1, :].unsqueeze(2).to_broadcast([128, 8, 16, 16]), ALU.add), reads=s_m16h, writes=[s_cand])
                            cvs = [cand[:, h].rearrange("p a b -> p (a b)") for h in range(8)]
                            for h in range(8):
                                S.op("dve", lambda v, h=h: v.max(out=t16[:, h, 0:8], in_=cvs[h]), reads=[s_cand], writes=[s_t16h[h]])
                            for h in range(8):
                                S.op("dve", lambda v, h=h: v.match_replace(out=tmpc[:, h, :], in_to_replace=t16[:, h, 0:8], in_values=cvs[h], imm_value=-1e30), reads=[s_cand, s_t16h[h]], writes=[s_tmpc[h]])
                            for h in range(8):
                                S.op("dve", lambda v, h=h: v.max(out=t16[:, h, 8:16], in_=tmpc[:, h, :]), reads=[s_tmpc[h]], writes=[s_t16h[h]])
                            s_t16 = Slot()
                            S.op("dve", lambda v: v.tensor_copy(sm[:, :, 3], t16[:, :, 15]), reads=s_t16h, writes=[s_sm, s_t16])
                            S.op("dve", lambda v: v.tensor_tensor(cand[:, 0:8, 0, :], t16[:], t16[:, :, 0:1].to_broadcast([128, 8, 16]), ALU.subtract), reads=[s_t16], writes=[s_cand])
                            S.op("act", lambda a: a.activation(out=cand[:, 0:8, 1, :], in_=cand[:, 0:8, 0, :], func=AF.Exp), reads=[s_cand], writes=[s_cand])
                            S.op("dve", lambda v: v.reduce_sum(sm[:, :, 0], cand[:, 0:8, 1, :], axis=AX.X), reads=[s_cand], writes=[s_sm])
                            S.op("act", lambda a: a.activation(out=sm[:, :, 1], in_=sm[:, :, 0], func=AF.Ln), reads=[s_sm], writes=[s_sm])
                            S.op("dve", lambda v: v.tensor_tensor(sm[:, :, 2], t16[:, :, 0], sm[:, :, 1], ALU.add), reads=[s_sm, s_t16], writes=[s_sm])
                            S.op("dve", lambda v: v.tensor_scalar(sm[:, :, 2], sm[:, :, 2], -1.0, None, ALU.mult), reads=[s_sm], writes=[s_sm])
                            S.op("dve", lambda v: v.scalar_tensor_tensor(thr[:, sub, :], t16[:, :, 15], -1e-4, sm[:, :, 2], ALU.add, ALU.add), reads=[s_sm, s_t16], writes=[s_thr])
                            s1v = ssb[:, sub].rearrange("p (h c) n -> p h c n", c=2)[:, :, 0, :]
                            S.op("dve", lambda v: v.tensor_tensor(s1v, s1v, sm[:, :, 2:3].to_broadcast([128, 8, 128]), ALU.add), reads=[s_ssb, s_sm], writes=[s_ssb])
                            s2v = ssb[:, sub].rearrange("p (h c) n -> p h c n", c=2)[:, :, 1, :]
                            S.op("act", lambda a: a.copy(out=s1b[:, sub], in_=s1v), reads=[s_ssb], writes=[s_e12])
                            S.op("act", lambda a: a.activation(out=e1s[:, sub], in_=s1v[:, 6:8, :], func=AF.Exp), reads=[s_ssb], writes=[s_e67])
                            S.op("act", lambda a: a.activation(out=e2s[:, sub], in_=s2v[:, 6:8, :], func=AF.Exp), reads=[s_ssb], writes=[s_e67])
                            S.op("dve", lambda v: v.tensor_tensor(s1v, thr[:, sub, :].unsqueeze(2).to_broadcast([128, 8, 128]), s1v, ALU.subtract), reads=[s_ssb, s_thr, s_e12], writes=[s_ssb])
                        barrier()
                    es2 = ExitStack()
                    with es2:
                        s2a = lambda n, sh, dt: es2.enter_context(nc.sbuf_tensor(n + '_%d' % t, sh, dt))
                        vbr = Ring([(s2a("vb%d" % i, [128, 4, 2048], BF16), Slot()) for i in range(2)])
                        vbq = [S.dma_sem() for _ in range(2)]
                        mkr = Ring([(s2a("mk%d" % i, [128, 2048], BF16), Slot()) for i in range(2)])
                        ewr = Ring([(s2a("ew%d" % i, [128, 2048], BF16), Slot()) for i in range(2)])
                        whr = Ring([(s2a("wh%d" % i, [128, 2, 512], BF16), Slot()) for i in range(2)])
                        Wb = s2a("Wb", [128, 4, 512], BF16)
                        s_Wb = [Slot() for _ in range(4)]
                        glr = Ring([(s2a("gl%d" % i, [128, 4, 512], BF16), Slot()) for i in range(2)])
                        AT = s2a("AT", [128, 4, 512], BF16)
                        s_AT = Slot()
                        stt_ = {}

                        def S1_load(eb):
                            u_, s_u = ub.next()
                            S.dma("pool", u_[:], e_uT_v[:, :, eb * 512:(eb + 1) * 512], ubq[(ub.i - 1) % 2], writes=[s_u])
                            vb, s_vb = vbr.next()
                            S.dma("pool", vb[:], e_v[eb * 512:(eb + 1) * 512, :].rearrange("(c p) d -> p c d", p=128), vbq[(vbr.i - 1) % 2], writes=[s_vb])
                            gl, s_gl = glr.next()
                            stt_[eb] = (vb, s_vb, gl, s_gl, u_, s_u)

                        def S1_score(eb, ecs):
                            vb, s_vb, gl, s_gl, u_, s_u = stt_[eb]
                            for ec in ecs:
                                pm, s_pm = pq.next()
                                for kc in range(16):
                                    S.op("pe", lambda t_, kc=kc: t_.matmul(pm[:], u_[:, kc, ec * 128:(ec + 1) * 128], hn[:, kc, :], start=(kc == 0), stop=(kc == 15)),
                                         reads=[s_u, s_hn], writes=[s_pm], inc=(kc == 15))
                                S.op("act", lambda a: a.activation(out=gl[:, ec, :], in_=pm[:], func=AF.Gelu), reads=[s_pm], writes=[s_gl])

                        cur_wh = {}

                        bst = {}

                        def step_of(g):
                            return g // 8, (g % 8) // 2, g % 2

                        def Sa(g):
                            eb, sub, hh = step_of(g)
                            mk, s_mk = mkr.next()
                            ew, s_ew = ewr.next()
                            bst[g] = (mk, s_mk, ew, s_ew)
                            sv = ssb[:, sub].rearrange("p (h c) n -> p h c n", c=2)
                            hs = slice(4 * hh, 4 * hh + 4)
                            isl = slice(4 * eb, 4 * eb + 4)
                            mk4 = mk[:].rearrange("p (h i j) -> p h i j", h=4, i=4)
                            ew4 = ew[:].rearrange("p (h i j) -> p h i j", h=4, i=4)
                            B4 = [128, 4, 4, 128]
                            for h_ in range(4):
                                for i_ in range(4):
                                    hh_, ii_ = 4 * hh + h_, 4 * eb + i_
                                    edge = (h_ == 0 and i_ == 0) or (h_ == 3 and i_ == 3)
                                    S.op("act", lambda a, h_=h_, i_=i_, hh_=hh_, ii_=ii_: a.activation(out=ew4[:, h_, i_, :], in_=sv[:, hh_, 1, :], func=AF.Exp, bias=s1b[:, sub, hh_, ii_:ii_ + 1]),
                                         reads=[s_ssb, s_e12] if edge else [], writes=[s_ew] if edge else [])
                            S.op("dve", lambda v: v.tensor_tensor(mk4, sv[:, hs, 1, :].unsqueeze(2).to_broadcast(B4), sv[:, hs, 0, isl].unsqueeze(3).to_broadcast(B4), ALU.is_ge),
                                 reads=[s_ssb], writes=[s_mk])

                        def Sc_mult(g):
                            eb, sub, hh = step_of(g)
                            mk, s_mk, ew, s_ew = bst[g]
                            if hh == 0:
                                cur_wh[sub] = whr.next()
                            if True:
                                S.op("dve", lambda g_: g_.tensor_tensor(mk[:], mk[:], ew[:], ALU.mult), reads=[s_mk, s_ew], writes=[s_mk])
                            else:
                                isl = slice(4 * eb, 4 * eb + 4)
                                mk4 = mk[:].rearrange("p (h i j) -> p h i j", h=4, i=4)
                                B2 = [128, 2, 4, 128]
                                S.op("dve", lambda g_: g_.tensor_tensor(mk[:, 0:1024], mk[:, 0:1024], ew[:, 0:1024], ALU.mult), reads=[s_mk, s_ew], writes=[s_mk])
                                S.op("dve", lambda g_: g_.tensor_tensor(mk4[:, 2:4], mk4[:, 2:4], e2s[:, sub].unsqueeze(2).to_broadcast(B2), ALU.mult), reads=[s_mk, s_e67], writes=[s_mk])
                                S.op("dve", lambda g_: g_.tensor_tensor(mk4[:, 2:4], mk4[:, 2:4], e1s[:, sub, :, isl].unsqueeze(3).to_broadcast(B2), ALU.mult), reads=[s_mk, s_e67], writes=[s_mk])

                        def Sc_add1(g):
                            mk, s_mk, ew, s_ew = bst[g]
                            mk2 = mk[:].rearrange("p (a e) -> p a e", a=2)
                            S.op("dve", lambda v: v.tensor_tensor(mk2[:, 0, :], mk2[:, 0, :], mk2[:, 1, :], ALU.add), reads=[s_mk], writes=[s_mk])

                        def Sc_add2(g):
                            eb, sub, hh = step_of(g)
                            mk, s_mk, ew, s_ew = bst.pop(g)
                            wh, s_wh = cur_wh[sub]
                            S.op("dve", lambda v: v.tensor_tensor(wh[:, hh, :], mk[:, 0:512], mk[:, 512:1024], ALU.add), reads=[s_mk], writes=[s_wh])
                            if hh == 1:
                                S2_fin(eb, sub)

                        def Sc(g):
                            Sc_mult(g)
                            Sc_add1(g)
                            Sc_add2(g)

                        GMAX = 32 * 8

                        def build_step(n):
                            if 0 <= n + 1 < GMAX:
                                Sa(n + 1)
                            if 0 <= n < GMAX:
                                Sc(n)

                        def S2_fin(eb, sub):
                            wh, s_wh = cur_wh[sub]
                            S.op("dve", lambda g_: g_.tensor_tensor(Wb[:, sub, :], wh[:, 0, :], wh[:, 1, :], ALU.add), reads=[s_wh], writes=[s_Wb[sub]])
                            for ec in range(4):
                                pt_, s_pt = pT[ec // 2]
                                dst = pt_[:].bitcast(BF16)[:, (ec % 2) * 512 + sub * 128:(ec % 2) * 512 + (sub + 1) * 128]
                                S.op("pe", lambda t_, dst=dst, ec=ec: t_.transpose(dst, Wb[:, sub, ec * 128:(ec + 1) * 128], ident[:]), reads=[s_Wb[sub], s_id], writes=[s_pt])

                        def S3_at(eb):
                            vb, s_vb, gl, s_gl, u_, s_u = stt_[eb]
                            for ec in range(4):
                                pt_, s_pt = pT[ec // 2]
                                src = pt_[:].bitcast(BF16)[:, (ec % 2) * 512:(ec % 2 + 1) * 512]
                                S.op("dve", lambda v, src=src, ec=ec: v.tensor_tensor(AT[:, ec, :], src, gl[:, ec, :], ALU.mult), reads=[s_pt, s_gl], writes=[s_AT] if ec in (0, 3) else [])

                        def S3_v_pe(eb, dcs):
                            vb, s_vb, gl, s_gl, u_, s_u = stt_[eb]
                            outs = []
                            for dc in dcs:
                                po, s_po = pv.next()
                                for ec in range(4):
                                    S.op("pe", lambda t_, ec=ec: t_.matmul(po[:], vb[:, ec, dc * 128:(dc + 1) * 128], AT[:, ec, :], start=(ec == 0), stop=(ec == 3)),
                                         reads=[s_vb, s_AT], writes=[s_po], inc=(ec == 3))
                                outs.append((dc, po, s_po))
                            return outs

                        def S3_v_add(item):
                            dc, po, s_po = item
                            S.op("dve", lambda v: v.tensor_tensor(acc[:, dc, :], po[:], acc[:, dc, :], ALU.add), reads=[s_po, s_acc], writes=[s_acc])

                        NEB = 32
                        S1_load(0)
                        S1_score(0, range(4))
                        for n in range(-1, 8):
                            build_step(n)
                        for eb in range(NEB):
                            nxt = eb + 1 < NEB
                            if nxt:
                                S1_load(eb + 1)
                            S3_at(eb)
                            for k2 in range(8):
                                n = (eb + 1) * 8 + k2
                                if nxt and n + 1 < GMAX:
                                    Sa(n + 1)
                                items = S3_v_pe(eb, [2 * k2, 2 * k2 + 1])
                                if nxt:
                                    Sc_mult(n)
                                S3_v_add(items[0])
                                if nxt:
                                    Sc_add1(n)
                                S3_v_add(items[1])
                                if nxt:
                                    Sc_add2(n)
                                if nxt and k2 == 2:
                                    S1_score(eb + 1, [0, 1])
                                if nxt and k2 == 5:
                                    S1_score(eb + 1, [2, 3])
                            del stt_[eb]
                        sl = Slot()
                        out_slots.append(sl)
                        S.dma("sp", outT_v[:, :, ts_], acc[:], outq, reads=[s_acc], writes=[sl])
                        barrier()
                S.wait_all("sp", out_slots)
                barrier()

        if stage >= 1:
            phaseA()
        if stage >= 2:
            phaseC()
        if stage >= 3:
            phaseB()
        if stage >= 4:
            phaseD()
        if stage >= 5:
            phaseE()
        if stage < 5:
            fin = sbt("fin", [128, 16], F32)
            s_fin = Slot()
            S.op("dve", lambda v: v.memset(fin[:], 0.0), writes=[s_fin])
            dqo = S.dma_sem()
            so = Slot()
            S.dma("sp", outT[0:128, 0:16], fin[:], dqo, reads=[s_fin], writes=[so])
            S.wait_all("sp", [so])
    return nc


_CONST = {}


def _const_tables(j):
    key = ("t", j)
    if key in _CONST:
        return _CONST[key]
    r = np.arange(S_LEN)
    s_act = (r + OWN * j) % S_LEN
    k_act = (np.arange(OWN) + OWN * j)
    prod = (s_act[:, None].astype(np.int64) * k_act[None, :].astype(np.int64)) % S_LEN
    ang = prod.astype(np.float64) * (2.0 * np.pi / S_LEN)
    tabC = (np.cos(ang) / math.sqrt(S_LEN)).astype(np.float32).astype(ml_dtypes.bfloat16)
    tabS = (-np.sin(ang) / math.sqrt(S_LEN)).astype(np.float32).astype(ml_dtypes.bfloat16)
    _CONST[key] = (tabC, tabS)
    return _CONST[key]


def _shared_consts():
    if "s" in _CONST:
        return _CONST["s"]
    jj = np.arange(256)
    ang = (jj[:, None] * jj[None, :] % 256).astype(np.float64) * (2.0 * np.pi / 256)
    csc = np.concatenate([np.cos(ang), np.sin(ang)], axis=1).astype(np.float32) / 16.0
    p = np.arange(128)[:, None]
    mlin = (np.arange(512)[None, :] - p).astype(np.float32)
    mdiag = np.abs(np.arange(896)[None, :] - p - 384).astype(np.float32)
    _CONST["s"] = (csc, mlin, mdiag)
    return _CONST["s"]


def _prep_inputs(inp):
    x = np.asarray(inp["x"], np.float32)
    csc, mlin, mdiag = _shared_consts()
    vecs = np.zeros((128, 64), np.float32)
    vecs[:, 0:16] = np.asarray(inp["norm1_g"], np.float32).reshape(16, 128).T
    vecs[:, 16:32] = np.asarray(inp["norm2_g"], np.float32).reshape(16, 128).T
    vecs[:, 32] = np.asarray(inp["q_norm_g"], np.float32).reshape(128)
    vecs[:, 33] = np.asarray(inp["k_norm_g"], np.float32).reshape(128)
    vecs[:, 34:36] = np.asarray(inp["subln_g"], np.float32).reshape(2, 128).T
    vecs[:, 36] = np.asarray(inp["lambda_q1"], np.float32).reshape(128)
    vecs[:, 37] = np.asarray(inp["lambda_k1"], np.float32).reshape(128)
    vecs[:, 38] = np.asarray(inp["lambda_q2"], np.float32).reshape(128)
    vecs[:, 39] = np.asarray(inp["lambda_k2"], np.float32).reshape(128)
    w_in = np.ascontiguousarray(np.asarray(inp["w_in"], np.float32)[0])
    w_f = np.ascontiguousarray(np.asarray(inp["w_fourier"], np.float32)[0])
    w_a = np.ascontiguousarray(np.asarray(inp["w_attn"], np.float32)[0])
    w_o = np.ascontiguousarray(np.asarray(inp["w_out"], np.float32)[0])
    w_q = np.ascontiguousarray(np.asarray(inp["w_query"], np.float32)[0])
    sk = np.asarray(inp["sub_keys"], np.float32)[0]
    skT = np.ascontiguousarray(sk.reshape(16, 128, 128).transpose(2, 0, 1))
    e_uT = np.ascontiguousarray(np.asarray(inp["expert_u"], np.float32)[0].T)
    e_v = np.ascontiguousarray(np.asarray(inp["expert_v"], np.float32)[0])
    xTb = [np.ascontiguousarray(x[b].T) for b in range(2)]
    maps = []
    for c in range(8):
        b, j = c // 4, c % 4
        tabC, tabS = _const_tables(j)
        xT = np.ascontiguousarray(np.roll(xTb[b], -OWN * j, axis=1))
        atab = np.zeros((NH, 4, 64, 2), np.float32)
        for qb in range(4):
            q0 = OWN * j + qb * 512
            for kc in range(64):
                k0 = (kc * 128 + OWN * j) % S_LEN
                A = q0 - k0
                for h in range(NH):
                    atab[h, qb, kc, 0] = 1.0 if A > 0 else -1.0
                    atab[h, qb, kc, 1] = -SLOPES[h] * abs(A)
        atab = np.ascontiguousarray(np.broadcast_to(atab.reshape(1, 4096), (128, 4096)))
        maps.append({
            "atab": atab,
            "xT": xT, "w_in": w_in, "vecs": vecs, "csc": csc, "tabC": tabC, "tabS": tabS,
            "mlin": mlin, "mdiag": mdiag, "w_f": w_f, "w_a": w_a, "w_o": w_o, "w_q": w_q,
            "skT": skT, "e_uT": e_uT, "e_v": e_v,
        })
    return maps


def kernel(**inputs):
    maps = _prep_inputs(inputs)
    nc = build()
    res = run_bass_kernel_spmd(nc, maps, core_ids=list(range(8)), trace=True)
    out = np.empty((2, S_LEN, D), np.float32)
    for c in range(8):
        b, j = c // 4, c % 4
        out[b, j * OWN:(j + 1) * OWN, :] = res.results[c]["outT"].T
    return out
```
